# Optimizing a Trainium2 kernel written in Bass

```python
import jax, jax.numpy as jnp
from jax import lax
import numpy as np

D_MODEL = 2048
BATCH = 8
SEQ = 4096
DEPTH = 1

N_HEADS_ATTN = 8
HEAD_DIM = 128
D_ATTN = N_HEADS_ATTN * HEAD_DIM
MOBA_BLOCK = 256
MOBA_TOPK = 3
Q_CHUNK = 16
ROPE_THETA = 500000.0
ROPE_DIM = HEAD_DIM // 4
N_GROUPS_SGU = 8
SGU_GROUP_DIM = 128
D_SGU = N_GROUPS_SGU * SGU_GROUP_DIM
SGU_CHUNK = 128
N_EXPERT_GROUPS = 4
EXPERTS_PER_GROUP = 8
N_EXPERTS = N_EXPERT_GROUPS * EXPERTS_PER_GROUP
D_FF_EXPERT = 1024
TOP_K_INNER = 2
DISPATCH_BLOCK = 256
EPS = 1e-6
D_IN_PROJ = 3 * D_ATTN + 2 * D_SGU + 2 * D_MODEL
SPLIT_POINTS = (D_ATTN, 2 * D_ATTN, 3 * D_ATTN, 3 * D_ATTN + D_SGU,
                3 * D_ATTN + 2 * D_SGU, 3 * D_ATTN + 2 * D_SGU + D_MODEL)

kernel_name = "hybrid_moba_sgu_hiermoe_block"


def rms_norm(x, g):
    xf = x.astype(jnp.float32)
    y = xf * lax.rsqrt(jnp.mean(xf * xf, axis=-1, keepdims=True) + EPS)
    return (y * g.astype(jnp.float32)).astype(x.dtype)


def layer_norm(x, g, b):
    xf = x.astype(jnp.float32)
    mu = jnp.mean(xf, axis=-1, keepdims=True)
    var = jnp.mean(jnp.square(xf - mu), axis=-1, keepdims=True)
    y = (xf - mu) * lax.rsqrt(var + EPS)
    return (y * g.astype(jnp.float32) + b.astype(jnp.float32)).astype(x.dtype)


def partial_rope(x, pos):
    half = ROPE_DIM // 2
    inv = ROPE_THETA ** (-jnp.arange(half, dtype=jnp.float32) * 2.0 / ROPE_DIM)
    ang = pos.astype(jnp.float32)[:, None] * inv[None, :]
    cos = jnp.cos(ang).astype(x.dtype)
    sin = jnp.sin(ang).astype(x.dtype)
    x1 = x[..., :half]
    x2 = x[..., half:ROPE_DIM]
    return jnp.concatenate([x1 * cos - x2 * sin, x2 * cos + x1 * sin, x[..., ROPE_DIM:]], axis=-1)


def moba_attention(q, k, v):
    B, S, H, dh = q.shape
    pos = jnp.arange(S)
    q = partial_rope(q.transpose(0, 2, 1, 3), pos)
    k = partial_rope(k.transpose(0, 2, 1, 3), pos)
    v = v.transpose(0, 2, 1, 3)
    n_blk = -(-S // MOBA_BLOCK)
    s_pad = n_blk * MOBA_BLOCK
    pad = ((0, 0), (0, 0), (0, s_pad - S), (0, 0))
    kb = jnp.pad(k, pad).reshape(B, H, n_blk, MOBA_BLOCK, dh)
    vb = jnp.pad(v, pad).reshape(B, H, n_blk, MOBA_BLOCK, dh)
    k_mean = jnp.mean(kb.astype(jnp.float32), axis=3).astype(k.dtype)
    k_eff = min(MOBA_TOPK, n_blk)
    n_sel = k_eff * MOBA_BLOCK
    scale = HEAD_DIM ** -0.5
    n_qc = S // Q_CHUNK
    q_chunks = q.reshape(B, H, n_qc, Q_CHUNK, dh).transpose(2, 0, 1, 3, 4)
    b_idx = jnp.arange(B)[:, None, None, None]
    h_idx = jnp.arange(H)[None, :, None, None]
    blk_ids = jnp.arange(n_blk)

    def chunk(args):
        qc, c = args
        q0 = c * Q_CHUNK
        blk = q0 // MOBA_BLOCK
        qpos = q0 + jnp.arange(Q_CHUNK)
        gate = jnp.einsum('bhqd,bhnd->bhqn', qc, k_mean).astype(jnp.float32)
        gate = jnp.where(blk_ids < blk, gate, -jnp.inf)
        _, sel = lax.top_k(gate, k_eff)
        valid = sel < blk
        k_sel = kb[b_idx, h_idx, sel]
        v_sel = vb[b_idx, h_idx, sel]
        s_sel = jnp.einsum('bhqd,bhqjkd->bhqjk', qc, k_sel).astype(jnp.float32) * scale
        s_sel = jnp.where(valid[..., None], s_sel, -jnp.inf).reshape(B, H, Q_CHUNK, n_sel)
        k_own = lax.dynamic_index_in_dim(kb, blk, axis=2, keepdims=False)
        v_own = lax.dynamic_index_in_dim(vb, blk, axis=2, keepdims=False)
        s_own = jnp.einsum('bhqd,bhkd->bhqk', qc, k_own).astype(jnp.float32) * scale
        kpos = blk * MOBA_BLOCK + jnp.arange(MOBA_BLOCK)
        s_own = jnp.where(kpos[None, :] <= qpos[:, None], s_own, -jnp.inf)
        p = jax.nn.softmax(jnp.concatenate([s_sel, s_own], axis=-1), axis=-1).astype(v.dtype)
        p_sel = p[..., :n_sel].reshape(B, H, Q_CHUNK, k_eff, MOBA_BLOCK)
        o = (jnp.einsum('bhqjk,bhqjkd->bhqd', p_sel, v_sel)
             + jnp.einsum('bhqk,bhkd->bhqd', p[..., n_sel:], v_own))
        return o

    out = lax.map(chunk, (q_chunks, jnp.arange(n_qc)))
    return out.transpose(1, 0, 3, 2, 4).reshape(B, S, H * dh)


def spatial_gating(u, vg, sgu_w, sgu_b, ln_g, ln_b):
    B, S, _ = u.shape
    u = jax.nn.gelu(u)
    vg = layer_norm(jax.nn.gelu(vg), ln_g, ln_b)
    n_ch = S // SGU_CHUNK
    vr = vg.reshape(B, n_ch, SGU_CHUNK, N_GROUPS_SGU, SGU_GROUP_DIM)
    causal = jnp.tril(jnp.ones((SGU_CHUNK, SGU_CHUNK), dtype=bool))
    w = jnp.where(causal[None], sgu_w, jnp.zeros_like(sgu_w))
    mixed = jnp.einsum('gts,bnsgc->bntgc', w, vr) + sgu_b.T[None, None, :, :, None]
    return u * mixed.reshape(B, S, D_SGU)


def hier_moe(h, w_rg, b_rg, w_re, b_re, w_gate, w_up, w_down):
    B, S, D = h.shape
    T = B * S
    xt = h.reshape(T, D)
    g_logits = (xt @ w_rg + b_rg).astype(jnp.float32)
    grp = jnp.argmax(g_logits, axis=-1)
    p_grp = jnp.take_along_axis(jax.nn.softmax(g_logits, axis=-1), grp[:, None], axis=-1)
    e_logits = (xt @ w_re + b_re).astype(jnp.float32).reshape(T, N_EXPERT_GROUPS, EXPERTS_PER_GROUP)
    e_logits = jnp.take_along_axis(e_logits, grp[:, None, None], axis=1)[:, 0]
    top_w, top_i = lax.top_k(jax.nn.softmax(e_logits, axis=-1), TOP_K_INNER)
    top_w = top_w / jnp.sum(top_w, axis=-1, keepdims=True)
    weights = p_grp * top_w
    experts = grp[:, None].astype(jnp.int32) * EXPERTS_PER_GROUP + top_i.astype(jnp.int32)

    n_assign = T * TOP_K_INNER
    flat_e = experts.reshape(n_assign)
    flat_w = weights.reshape(n_assign)
    flat_tok = jnp.repeat(jnp.arange(T, dtype=jnp.int32), TOP_K_INNER)
    order = jnp.argsort(flat_e)
    se = flat_e[order]
    counts = jax.ops.segment_sum(jnp.ones_like(flat_e), flat_e, num_segments=N_EXPERTS)
    padded = (counts + DISPATCH_BLOCK - 1) // DISPATCH_BLOCK * DISPATCH_BLOCK
    pad_end = jnp.cumsum(padded)
    pad_start = pad_end - padded
    start = jnp.cumsum(counts) - counts
    dest = pad_start[se] + jnp.arange(n_assign, dtype=jnp.int32) - start[se]
    n_rows = -(-n_assign // DISPATCH_BLOCK) * DISPATCH_BLOCK + N_EXPERTS * DISPATCH_BLOCK
    n_blocks = n_rows // DISPATCH_BLOCK
    row_tok = jnp.full((n_rows,), T, dtype=jnp.int32).at[dest].set(flat_tok[order])
    row_w = jnp.zeros((n_rows,), jnp.float32).at[dest].set(flat_w[order])
    blk_e = jnp.minimum(jnp.searchsorted(pad_end, jnp.arange(n_blocks) * DISPATCH_BLOCK, side='right'),
                        N_EXPERTS - 1).astype(jnp.int32)
    x_pad = jnp.concatenate([xt, jnp.zeros((1, D), xt.dtype)], axis=0)

    def expert_block(args):
        tok, e = args
        xb = x_pad[tok]
        hid = jax.nn.silu(xb @ w_gate[e]) * (xb @ w_up[e])
        return hid @ w_down[e]

    ys = lax.map(expert_block, (row_tok.reshape(n_blocks, DISPATCH_BLOCK), blk_e))
    ys = ys.reshape(n_rows, D) * row_w[:, None].astype(xt.dtype)
    out = jnp.zeros((T + 1, D), xt.dtype).at[row_tok].add(ys)
    return out[:T].reshape(B, S, D)


def hybrid_layer(x, norm_mix_g, w_in, sgu_w, sgu_b, sgu_ln_g, sgu_ln_b, w_proj_attn, w_proj_sgu,
                 w_out, norm_ffn_g, w_router_group, b_router_group, w_router_expert, b_router_expert,
                 w_exp_gate, w_exp_up, w_exp_down):
    B, S, _ = x.shape
    h = rms_norm(x, norm_mix_g)
    proj = h @ w_in
    q, k, v, u, vg, gate_attn, gate_sgu = jnp.split(proj, SPLIT_POINTS, axis=-1)
    shp = (B, S, N_HEADS_ATTN, HEAD_DIM)
    attn = moba_attention(q.reshape(shp), k.reshape(shp), v.reshape(shp))
    sgu = spatial_gating(u, vg, sgu_w, sgu_b, sgu_ln_g, sgu_ln_b)
    merged = (jax.nn.sigmoid(gate_attn) * (attn @ w_proj_attn)
              + jax.nn.sigmoid(gate_sgu) * (sgu @ w_proj_sgu))
    x = x + merged @ w_out
    x = x + hier_moe(rms_norm(x, norm_ffn_g), w_router_group, b_router_group, w_router_expert,
                     b_router_expert, w_exp_gate, w_exp_up, w_exp_down)
    return x


def setup_inputs(seed: int = 0) -> dict:
    key = jax.random.key(seed)
    ks = jax.random.split(key, 20)
    L = DEPTH

    def nrm(k, shape, scale):
        return jax.random.normal(k, shape, jnp.float32) * scale

    return {
        "x": nrm(ks[0], (BATCH, SEQ, D_MODEL), 1.0),
        "norm_mix_g": 1.0 + nrm(ks[1], (L, D_MODEL), 0.01),
        "w_in": nrm(ks[2], (L, D_MODEL, D_IN_PROJ), D_MODEL ** -0.5),
        "sgu_w": nrm(ks[3], (L, N_GROUPS_SGU, SGU_CHUNK, SGU_CHUNK), SGU_CHUNK ** -0.5),
        "sgu_b": 1.0 + nrm(ks[4], (L, N_GROUPS_SGU, SGU_CHUNK), 0.01),
        "sgu_ln_g": 1.0 + nrm(ks[5], (L, D_SGU), 0.01),
        "sgu_ln_b": nrm(ks[6], (L, D_SGU), 0.01),
        "w_proj_attn": nrm(ks[7], (L, D_ATTN, D_MODEL), D_ATTN ** -0.5),
        "w_proj_sgu": nrm(ks[8], (L, D_SGU, D_MODEL), D_SGU ** -0.5),
        "w_out": nrm(ks[9], (L, D_MODEL, D_MODEL), D_MODEL ** -0.5),
        "norm_ffn_g": 1.0 + nrm(ks[10], (L, D_MODEL), 0.01),
        "w_router_group": nrm(ks[11], (L, D_MODEL, N_EXPERT_GROUPS), D_MODEL ** -0.5),
        "b_router_group": nrm(ks[12], (L, N_EXPERT_GROUPS), 0.01),
        "w_router_expert": nrm(ks[13], (L, D_MODEL, N_EXPERTS), D_MODEL ** -0.5),
        "b_router_expert": nrm(ks[14], (L, N_EXPERTS), 0.01),
        "w_exp_gate": nrm(ks[15], (L, N_EXPERTS, D_MODEL, D_FF_EXPERT), D_MODEL ** -0.5),
        "w_exp_up": nrm(ks[16], (L, N_EXPERTS, D_MODEL, D_FF_EXPERT), D_MODEL ** -0.5),
        "w_exp_down": nrm(ks[17], (L, N_EXPERTS, D_FF_EXPERT, D_MODEL), D_FF_EXPERT ** -0.5),
        "norm_final_g": 1.0 + nrm(ks[18], (D_MODEL,), 0.01),
    }


def reference(x, norm_mix_g, w_in, sgu_w, sgu_b, sgu_ln_g, sgu_ln_b, w_proj_attn, w_proj_sgu,
              w_out, norm_ffn_g, w_router_group, b_router_group, w_router_expert, b_router_expert,
              w_exp_gate, w_exp_up, w_exp_down, norm_final_g):
    for l in range(DEPTH):
        x = hybrid_layer(x, norm_mix_g[l], w_in[l], sgu_w[l], sgu_b[l], sgu_ln_g[l], sgu_ln_b[l],
                         w_proj_attn[l], w_proj_sgu[l], w_out[l], norm_ffn_g[l],
                         w_router_group[l], b_router_group[l], w_router_expert[l], b_router_expert[l],
                         w_exp_gate[l], w_exp_up[l], w_exp_down[l])
    return rms_norm(x, norm_final_g)
```

```python
import numpy as np
from contextlib import ExitStack
import concourse.bass as bass
import concourse.mybir as mybir
from concourse.bass_utils import run_bass_kernel_spmd

F32 = mybir.dt.float32
BF16 = mybir.dt.bfloat16
I32 = mybir.dt.int32
AF = mybir.ActivationFunctionType
ALU = mybir.AluOpType
AX = mybir.AxisListType

S = 4096
D = 2048
NH = 8
DH = 128
DIN = 9216
NE = 32
DFF = 1024
CAP = 512
NT = CAP // 128
NROW = NE * CAP + 128
DUMP = NE * CAP
EPS = 1e-6
NEG = -30000.0
ENG = ['tensor', 'vector', 'scalar', 'gpsimd', 'sync']
import os
OPTS = set(os.environ.get('KOPTS', 'zero,p2,h1').split(','))


class Prog:
    def __init__(self, nc, es):
        self.nc = nc
        self.es = es
        self.q = {e: [] for e in ENG}
        self.sems = {}
        self.count = {}
        self.seen = {e: {} for e in ENG}
        for e in ENG:
            self._sem('E_' + e)

    def _sem(self, key):
        if key not in self.sems:
            self.sems[key] = self.es.enter_context(self.nc.semaphore(key))
            self.count[key] = 0
        return self.sems[key]

    def _waits(self, eng, deps):
        waits = {}
        for d in deps:
            if d is None:
                continue
            k, n = d
            if n > self.seen[eng].get(k, 0):
                waits[k] = max(waits.get(k, 0), n)
        for k, n in waits.items():
            self.seen[eng][k] = n
        return list(waits.items())

    def op(self, eng, fn, deps=()):
        w = self._waits(eng, deps)
        key = 'E_' + eng
        self.count[key] += 1
        self.q[eng].append((fn, w, key, 1))
        return (key, self.count[key])

    def dma(self, eng, semkey, fn, deps=()):
        semkey = eng[0] + '_' + semkey
        self._sem(semkey)
        w = self._waits(eng, deps)
        self.count[semkey] += 16
        self.q[eng].append((fn, w, semkey, 16))
        return (semkey, self.count[semkey])

    def barrier(self):
        toks = [(k, c) for k, c in self.count.items() if c > 0]
        for e in ENG:
            w = self._waits(e, toks)
            if w:
                self.q[e].append((None, w, None, 0))

    def emit(self, block):
        sems = self.sems
        q = self.q
        self.q = {e: [] for e in ENG}

        def run(engname):
            def body(e):
                for fn, w, key, inc in q[engname]:
                    for k, n in w:
                        e.wait_ge(sems[k], n)
                    if fn is not None:
                        ins = fn(e)
                        ins.then_inc(sems[key], inc)
            return body
        block.tensor(run('tensor'))
        block.vector(run('vector'))
        block.scalar(run('scalar'))
        block.gpsimd(run('gpsimd'))
        block.sync(run('sync'))


class Buf:
    def __init__(self):
        self.w = None
        self.r = []


def _deps(reads, writes):
    deps = []
    for b in reads:
        deps.append(b.w)
    for b in writes:
        deps.append(b.w)
        deps.extend(b.r)
    return deps


def _upd(tok, reads, writes):
    for b in reads:
        b.r.append(tok)
    for b in writes:
        b.w = tok
        b.r = []


class K:
    def __init__(self, nc, P):
        self.nc = nc
        self.P = P

    def X(self, eng, fn, reads=(), writes=()):
        deps = _deps(reads, writes)
        if eng == 'tensor':
            deps = [d for d in deps if d is not None and d[0] != 'E_tensor']
        tok = self.P.op(eng, fn, deps)
        _upd(tok, reads, writes)
        return tok

    def DMA(self, eng, semkey, out, in_, reads=(), writes=()):
        deps = _deps(reads, writes)
        tok = self.P.dma(eng, semkey, lambda e, out=out, in_=in_: e.dma_start(out=out, in_=in_), deps)
        _upd(tok, reads, writes)
        return tok


def bufs(n):
    return [Buf() for _ in range(n)]


def build(debug=False, upto='G'):
    nc = bass.Bass("TRN2", target_bir_lowering=False)

    def din(name, shape, dt=F32):
        return nc.dram_tensor(name, list(shape), dt, kind="ExternalInput").ap()

    def dscr(name, shape, dt):
        kind = "ExternalOutput" if (debug and name in debug) else "Internal"
        return nc.dram_tensor(name, list(shape), dt, kind=kind).ap()

    x = din("x", [S, D])
    w_in = din("w_in", [D, DIN])
    sgu_w = din("sgu_w", [8, 128, 128])
    w_pa = din("w_pa", [1024, D])
    w_pb = din("w_pb", [1024, D])
    w_out = din("w_out", [D, D])
    w_r = din("w_r", [D, 36])
    wg = din("wg", [NE, D, DFF])
    wu = din("wu", [NE, D, DFF])
    wd = din("wd", [NE, DFF, D])
    gmix = din("gmix", [128, 16])
    ln_g_bc = din("ln_g_bc", [128, 1024])
    ln_b_bc = din("ln_b_bc", [128, 1024])
    sgub_bc = din("sgub_bc", [128, 1024])
    g2_bc = din("g2_bc", [128, D])
    gf_bc = din("gf_bc", [128, D])
    rb_bc = din("rb_bc", [128, 36])
    c_ident = din("c_ident", [128, 128])
    c_ltri = din("c_ltri", [128, 128])
    c_ones = din("c_ones", [128, 128])
    c_cos = din("c_cos", [128, S])
    c_sin = din("c_sin", [128, S])
    c_rot = din("c_rot", [128, 128])
    c_mneg = din("c_mneg", [128, 512])
    c_valid = din("c_valid", [128, 512])
    c_own = din("c_own", [128, 512])
    c_esel = din("c_esel", [128, 16 * 128])
    c_caus = din("c_caus", [128, 4 * 512])
    c_tril = din("c_tril", [128, 128])
    c_ecap = din("c_ecap", [128, 32])

    out = nc.dram_tensor("out", [S, D], F32, kind="ExternalOutput").ap()

    qT = dscr("qT", [NH * 128, S], BF16)
    kT = dscr("kT", [NH * 128, S], BF16)
    Vd = dscr("Vd", [S, 1024], BF16)
    uT = dscr("uT", [1024, S], BF16)
    vgn = dscr("vgn", [S, 1024], BF16)
    sga = dscr("sga", [D, S], BF16)
    sgs = dscr("sgs", [D, S], BF16)
    attnT = dscr("attnT", [1024, S], BF16)
    GTd = dscr("GTd", [1024, S], BF16)
    mTd = dscr("mTd", [D, S], BF16)
    x1d = dscr("x1d", [S, D], F32)
    h2d = dscr("h2d", [S, D], BF16)
    disp = dscr("disp", [NROW, D], BF16)
    ydisp = dscr("ydisp", [NROW, D], BF16)
    rtab = dscr("rtab", [128, 128], F32)

    with ExitStack() as es:
        P = Prog(nc, es)
        k = K(nc, P)
        X, DMA = k.X, k.DMA
        ps = [es.enter_context(nc.psum_tensor(f"ps{i}", [128, 512], F32)) for i in range(8)]
        psb = bufs(8)

        uniq = [0]

        def sb(st, name, shape, dt):
            uniq[0] += 1
            return st.enter_context(nc.sbuf_tensor(f"{name}_{uniq[0]}", list(shape), dt))

        ident_b = sb(es, "ident_b", [128, 128], BF16)
        ident_f = sb(es, "ident_f", [128, 128], F32)
        ones_b = sb(es, "ones_b", [128, 128], BF16)
        eps_t = sb(es, "eps_t", [128, 1], F32)
        R0 = sb(es, "R0", [128, 32], I32)
        R1 = sb(es, "R1", [128, 32], I32)
        W0 = sb(es, "W0", [128, 32], F32)
        W1 = sb(es, "W1", [128, 32], F32)
        b_const = Buf()
        b_rt = Buf()
        rt_t = bufs(32)

        def phase(fn):
            with ExitStack() as st:
                block = st.enter_context(nc.Block())
                fn(st)
                P.barrier()
                P.emit(block)

        def gelu_tanh(src_ps, srcbuf, tmp, tmpbuf, dst, dstbuf, extra_reads=()):
            X('scalar', lambda e: e.activation(out=tmp, in_=src_ps, func=AF.Square),
              reads=[srcbuf], writes=[tmpbuf])
            X('vector', lambda e: e.tensor_scalar(out=tmp, in0=tmp, scalar1=0.044715, scalar2=1.0,
                                                  op0=ALU.mult, op1=ALU.add),
              reads=[], writes=[tmpbuf])
            X('vector', lambda e: e.tensor_tensor(out=tmp, in0=tmp, in1=src_ps, op=ALU.mult),
              reads=[srcbuf], writes=[tmpbuf])
            X('scalar', lambda e: e.activation(out=tmp, in_=tmp, func=AF.Sigmoid, scale=1.5957691216057308),
              reads=[], writes=[tmpbuf])
            return X('vector', lambda e: e.tensor_tensor(out=dst, in0=tmp, in1=src_ps, op=ALU.mult),
                     reads=[srcbuf, tmpbuf] + list(extra_reads), writes=[dstbuf])

        def rstd_from_ss(ss, ssb, tmpb):
            X('scalar', lambda e: e.activation(out=ss, in_=ss, func=AF.Ln, bias=eps_t[:, 0:1], scale=1.0 / D),
              reads=[b_const], writes=[ssb])
            X('scalar', lambda e: e.activation(out=ss, in_=ss, func=AF.Exp, scale=-0.5), writes=[ssb])

        def phaseA(st):
            hT = sb(st, "hT", [128, 16, 2048], BF16)
            cosT = sb(st, "cosT", [128, 2048], F32)
            sinT = sb(st, "sinT", [128, 2048], F32)
            rot = sb(st, "rot", [128, 128], BF16)
            gm = sb(st, "gm", [128, 16], F32)
            lng = sb(st, "lng", [128, 1024], F32)
            lnb = sb(st, "lnb", [128, 1024], F32)
            xs = [sb(st, f"xs{i}", [128, D], F32) for i in range(2)]
            xsb = bufs(2)
            xn2 = [sb(st, f"xn{i}", [128, D], BF16) for i in range(2)]
            xn2b = bufs(2)
            junkA = sb(st, "junkA", [128, D], BF16)
            junkAb = Buf()
            ssA2 = sb(st, "ssA2", [128, 2], F32)
            ssA2b = bufs(2)
            ssb = Buf()
            wr = [sb(st, f"wr{i}", [128, 16, 512], BF16) for i in range(3)]
            wrb = bufs(3)
            stg = [sb(st, f"stg{i}", [128, 4, 512], BF16) for i in range(2)]
            stgb = bufs(2)
            tmp = [sb(st, f"tmpA{i}", [128, 512], F32) for i in range(2)]
            tmpb = bufs(2)
            gv2 = [sb(st, f"gv{i}", [128, 1024], F32) for i in range(2)]
            gv2b = bufs(2)
            bst2 = [sb(st, f"bst{i}", [128, 2, 6], F32) for i in range(2)]
            mv2 = [sb(st, f"mv{i}", [128, 2], F32) for i in range(2)]
            mvb2 = bufs(2)
            vst = [sb(st, f"vst{i}", [128, 1024], BF16) for i in range(2)]
            vstb = bufs(2)
            hTb = Buf()
            cb = Buf()

            DMA('gpsimd', 'c0', ident_b[:], c_ident, writes=[b_const])
            DMA('sync', 'c1', ident_f[:], c_ident, writes=[b_const])
            DMA('gpsimd', 'c2', ones_b[:], c_ones, writes=[b_const])
            X('vector', lambda e: e.memset(eps_t[:], EPS), writes=[b_const])
            DMA('gpsimd', 'c3', rot[:], c_rot, writes=[cb])
            DMA('sync', 'c4', gm[:], gmix, writes=[cb])
            DMA('sync', 'c5', lng[:], ln_g_bc, writes=[cb])
            DMA('sync', 'c6', lnb[:], ln_b_bc, writes=[cb])
            ztile = sb(st, "ztile", [128, D], BF16)
            zt = ztile[:]
            zb = Buf()
            X('vector', lambda e: e.memset(zt, 0.0), writes=[zb])

            def zero_fill():
                for r in range(NROW // 128):
                    DMA('sync', f'z{r % 4}', disp[r * 128:(r + 1) * 128, :], zt, reads=[zb])
                DMA('sync', 'z4', ydisp[DUMP:DUMP + 128, :], zt, reads=[zb])

            units = [('k', 1024 + 512 * i) for i in range(2)] + [('q', 512 * i) for i in range(2)] + \
                    [('v', 2048 + 512 * i) for i in range(2)] + [('u', 3072 + 512 * i) for i in range(2)] + \
                    [('vg', 4096)] + [('ga', 5120 + 512 * i) for i in range(4)] + \
                    [('gs', 7168 + 512 * i) for i in range(4)]
            wslot = [0]
            pend_rope = []
            psrr = [0]
            stgrr = [0]

            def load_w(col0):
                s_ = wslot[0] % 3
                wslot[0] += 1
                DMA('gpsimd', f'w{s_}', wr[s_][:], w_in[:, col0:col0 + 512].rearrange("(k p) c -> p k c", p=128),
                    writes=[wrb[s_]])
                return s_

            for half in range(2 if 'h1' in OPTS else 1):
                T0 = half * 2048
                DMA('sync', 'c7', cosT[:], c_cos[:, T0:T0 + 2048], writes=[cb])
                DMA('sync', 'c8', sinT[:], c_sin[:, T0:T0 + 2048], writes=[cb])
                for i in range(16):
                    t0 = T0 + i * 128
                    xi = i % 2
                    DMA('sync', f'x{xi}', xs[xi][:], x[t0:t0 + 128, :], writes=[xsb[xi]])
                    xn = xn2[xi]
                    xnb = xn2b[xi]
                    X('scalar', lambda e, xi=xi: e.activation(out=junkA[:], in_=xs[xi][:], func=AF.Square,
                                                              accum_out=ssA2[:, xi:xi + 1]),
                      reads=[xsb[xi]], writes=[junkAb, ssA2b[xi]])
                    rstd_from_ss(ssA2[:, xi:xi + 1], ssA2b[xi], None)
                    X('scalar', lambda e, xi=xi, xn=xn: e.activation(out=xn[:], in_=xs[xi][:], func=AF.Copy,
                                                                     scale=ssA2[:, xi:xi + 1]),
                      reads=[xsb[xi], ssA2b[xi]], writes=[xnb])
                    for g8 in range(2):
                        pb_ = 6 + g8
                        pv = ps[pb_][:].bitcast(BF16).rearrange("p (a b) -> p a b", a=8)

                        def tr(e, g8=g8, pv=pv, xn=xn):
                            ins = None
                            for j in range(8):
                                kc = g8 * 8 + j
                                ins = e.transpose(out=pv[:, j, :], in_=xn[:, kc * 128:(kc + 1) * 128],
                                                  identity=ident_b[:])
                            return ins
                        X('tensor', tr, reads=[xnb, b_const], writes=[psb[pb_]])
                        X('vector', lambda e, g8=g8, pv=pv, i=i: e.tensor_tensor(
                            out=hT[:, g8 * 8:(g8 + 1) * 8, i * 128:(i + 1) * 128], in0=pv,
                            in1=gm[:, g8 * 8:(g8 + 1) * 8].unsqueeze(2).to_broadcast([128, 8, 128]),
                            op=ALU.mult), reads=[psb[pb_], cb], writes=[hTb])
                if half == 0:
                    zero_fill()
                for kind, col0 in units:
                    if 'p2' not in OPTS or ('only' in OPTS and kind not in OPTS):
                        continue
                    if kind == 'vg':
                        sl = [load_w(col0), load_w(col0 + 512)]
                        for i in range(16):
                            t0 = T0 + i * 128
                            pbs = []
                            for hh in range(2):
                                pb_ = psrr[0] % 4
                                psrr[0] += 1
                                pbs.append(pb_)

                                def mm(e, i=i, pb_=pb_, ws=sl[hh]):
                                    ins = None
                                    for kc in range(16):
                                        ins = e.matmul(ps[pb_][:], lhsT=hT[:, kc, i * 128:(i + 1) * 128],
                                                       rhs=wr[ws][:, kc, :], start=(kc == 0), stop=(kc == 15))
                                    return ins
                                X('tensor', mm, reads=[hTb, wrb[sl[hh]]], writes=[psb[pb_]])
                            gi_ = i % 2
                            gv, gvb, bst, mv, mvb = gv2[gi_], gv2b[gi_], bst2[gi_], mv2[gi_], mvb2[gi_]
                            for hh in range(2):
                                gelu_tanh(ps[pbs[hh]][:], psb[pbs[hh]], tmp[hh][:], tmpb[hh],
                                          gv[:, hh * 512:(hh + 1) * 512], gvb)
                            for hh in range(2):
                                X('vector', lambda e, hh=hh, bst=bst, gv=gv: e.bn_stats(out=bst[:, hh, :],
                                                                                        in_=gv[:, hh * 512:(hh + 1) * 512]),
                                  reads=[gvb], writes=[mvb])
                            X('vector', lambda e, bst=bst, mv=mv: e.bn_aggr(out=mv[:], in_=bst[:].rearrange("p a b -> p (a b)")),
                              writes=[mvb])
                            X('vector', lambda e, mv=mv: e.tensor_scalar(out=mv[:, 1:2], in0=mv[:, 1:2], scalar1=EPS,
                                                                         scalar2=None, op0=ALU.add), writes=[mvb])
                            X('scalar', lambda e, mv=mv: e.activation(out=mv[:, 1:2], in_=mv[:, 1:2], func=AF.Sqrt),
                              writes=[mvb])
                            X('vector', lambda e, mv=mv: e.reciprocal(out=mv[:, 1:2], in_=mv[:, 1:2]), writes=[mvb])
                            X('vector', lambda e, mv=mv, gv=gv: e.tensor_scalar(out=gv[:], in0=gv[:], scalar1=mv[:, 0:1],
                                                                                scalar2=mv[:, 1:2], op0=ALU.subtract,
                                                                                op1=ALU.mult), reads=[mvb], writes=[gvb])
                            X('gpsimd', lambda e, gv=gv: e.tensor_tensor(out=gv[:], in0=gv[:], in1=lng[:], op=ALU.mult),
                              reads=[cb], writes=[gvb])
                            vi = i % 2
                            X('gpsimd', lambda e, vi=vi, gv=gv: e.tensor_tensor(out=vst[vi][:], in0=gv[:], in1=lnb[:],
                                                                                op=ALU.add),
                              reads=[gvb, cb], writes=[vstb[vi]])
                            DMA('sync', f'vs{vi}', vgn[t0:t0 + 128, :], vst[vi][:], reads=[vstb[vi]])
                        continue
                    s_ = load_w(col0)
                    if kind == 'v':
                        vc0 = col0 - 2048
                        for i in range(16):
                            t0 = T0 + i * 128
                            pb_ = psrr[0] % 4
                            psrr[0] += 1

                            def mm(e, i=i, pb_=pb_, s_=s_):
                                ins = None
                                for kc in range(16):
                                    ins = e.matmul(ps[pb_][:], lhsT=hT[:, kc, i * 128:(i + 1) * 128],
                                                   rhs=wr[s_][:, kc, :], start=(kc == 0), stop=(kc == 15))
                                return ins
                            X('tensor', mm, reads=[hTb, wrb[s_]], writes=[psb[pb_]])
                            vi = i % 2
                            X('scalar', lambda e, vi=vi, pb_=pb_: e.activation(out=vst[vi][:, 0:512], in_=ps[pb_][:],
                                                                               func=AF.Copy),
                              reads=[psb[pb_]], writes=[vstb[vi]])
                            DMA('sync', f'vs{vi}', Vd[t0:t0 + 128, vc0:vc0 + 512], vst[vi][:, 0:512],
                                reads=[vstb[vi]])
                        continue
                    for tt in range(4):
                        t0 = T0 + tt * 512
                        si = stgrr[0] % 2
                        stgrr[0] += 1
                        for cc in range(4):
                            pb_ = psrr[0] % 4
                            psrr[0] += 1

                            def mm(e, tt=tt, cc=cc, pb_=pb_, s_=s_):
                                ins = None
                                for kc in range(16):
                                    ins = e.matmul(ps[pb_][:], lhsT=wr[s_][:, kc, cc * 128:(cc + 1) * 128],
                                                   rhs=hT[:, kc, tt * 512:(tt + 1) * 512],
                                                   start=(kc == 0), stop=(kc == 15))
                                return ins
                            X('tensor', mm, reads=[hTb, wrb[s_]], writes=[psb[pb_]])
                            dst = stg[si][:, cc, :]
                            if kind in ('q', 'k'):
                                X('scalar', lambda e, dst=dst, pb_=pb_: e.activation(out=dst, in_=ps[pb_][:],
                                                                                     func=AF.Copy),
                                  reads=[psb[pb_]], writes=[stgb[si]])
                                cs = cosT[:, tt * 512:(tt + 1) * 512]
                                sn = sinT[:, tt * 512:(tt + 1) * 512]

                                def rope_tail(dst=dst, cs=cs, sn=sn, pb_=pb_, si=si):
                                    X('tensor', lambda e, dst=dst: e.matmul(ps[4][:], lhsT=rot[:], rhs=dst,
                                                                            start=True, stop=True),
                                      reads=[stgb[si], cb], writes=[psb[4]])
                                    X('vector', lambda e, cs=cs, pb_=pb_: e.tensor_tensor(out=tmp[0][:], in0=cs,
                                                                                          in1=ps[pb_][:], op=ALU.mult),
                                      reads=[psb[pb_], cb, stgb[si]], writes=[tmpb[0]])
                                    X('vector', lambda e, sn=sn: e.tensor_tensor(out=tmp[1][:], in0=sn,
                                                                                 in1=ps[4][:], op=ALU.mult),
                                      reads=[psb[4], cb], writes=[tmpb[1]])
                                    X('vector', lambda e, dst=dst: e.tensor_tensor(out=dst, in0=tmp[0][:],
                                                                                   in1=tmp[1][:], op=ALU.add),
                                      reads=[tmpb[0], tmpb[1]], writes=[stgb[si]])
                                if pend_rope:
                                    pend_rope.pop()()
                                pend_rope.append(rope_tail)
                            elif kind == 'u':
                                ti = cc % 2
                                gelu_tanh(ps[pb_][:], psb[pb_], tmp[ti][:], tmpb[ti], dst, stgb[si])
                            else:
                                X('scalar', lambda e, dst=dst, pb_=pb_: e.activation(out=dst, in_=ps[pb_][:],
                                                                                     func=AF.Sigmoid),
                                  reads=[psb[pb_]], writes=[stgb[si]])
                        if pend_rope:
                            pend_rope.pop()()
                        if kind == 'q':
                            dd, r0_ = qT, col0
                        elif kind == 'k':
                            dd, r0_ = kT, col0 - 1024
                        elif kind == 'u':
                            dd, r0_ = uT, col0 - 3072
                        elif kind == 'ga':
                            dd, r0_ = sga, col0 - 5120
                        else:
                            dd, r0_ = sgs, col0 - 7168
                        DMA('sync', f'st{si}', dd[r0_:r0_ + 512, t0:t0 + 512].rearrange("(c p) t -> p c t", p=128),
                            stg[si][:], reads=[stgb[si]])

        if upto >= 'A':
            phase(phaseA)

        def phaseB(st):
            qs = [sb(st, f"qs{i}", [128, S], BF16) for i in range(3)]
            ks = [sb(st, f"ks{i}", [128, S], BF16) for i in range(3)]
            vs = [sb(st, f"vs{i}", [128, 32, 128], BF16) for i in range(3)]
            qb_, kb_, vb_ = bufs(3), bufs(3), bufs(3)
            kmf = sb(st, "kmf", [128, 16], F32)
            kmb = sb(st, "kmb", [128, 16], BF16)
            kmB = Buf()
            mneg = sb(st, "mneg", [128, 32, 16], F32)
            valid = sb(st, "valid", [128, 32, 16], F32)
            own = sb(st, "own", [128, 32, 16], F32)
            esel = sb(st, "esel", [128, 16, 128], BF16)
            caus = sb(st, "caus", [128, 4, 512], BF16)
            cb = Buf()
            gmt = sb(st, "gmt", [128, 32, 16], F32)
            gmtb = Buf()
            m8 = sb(st, "m8", [128, 32, 8], F32)
            m8b = Buf()
            alw = sb(st, "alw", [128, 32, 16], F32)
            alwb = Buf()
            Mb2 = [sb(st, f"Mb{i}", [128, 32, 16], BF16) for i in range(2)]
            Mbb = bufs(2)
            MT2 = [sb(st, f"MT{i}", [128, S], BF16) for i in range(2)]
            MT2b = bufs(2)
            for i_ in range(2):
                X('vector', lambda e, i_=i_: e.memset(MT2[i_][:], 0.0), writes=[MT2b[i_]])
            pT = [sb(st, f"pT{i}", [128, 512], BF16) for i in range(4)]
            pTb = bufs(4)
            rec = sb(st, "rec", [128, 512], F32)
            recb = Buf()
            dacc = [sb(st, f"dacc{i}", [128, 512], F32) for i in range(4)]
            daccb = bufs(4)
            ones_f = sb(st, "ones_f", [128, 128], F32)
            DMA('sync', 'c5', ones_f[:], c_ones, writes=[cb])
            ot = [sb(st, f"ot{i}", [128, 512], BF16) for i in range(2)]
            otb = bufs(2)
            DMA('sync', 'c0', mneg[:].rearrange("p a b -> p (a b)"), c_mneg, writes=[cb])
            DMA('sync', 'c1', valid[:].rearrange("p a b -> p (a b)"), c_valid, writes=[cb])
            DMA('sync', 'c2', own[:].rearrange("p a b -> p (a b)"), c_own, writes=[cb])
            DMA('gpsimd', 'c3', esel[:].rearrange("p a b -> p (a b)"), c_esel, writes=[cb])
            DMA('gpsimd', 'c4', caus[:].rearrange("p a b -> p (a b)"), c_caus, writes=[cb])
            scale = DH ** -0.5
            prr = [0]
            orr = [0]

            def load_head(h):
                s_ = h % 3
                DMA('sync', f'q{s_}', qs[s_][:], qT[h * 128:(h + 1) * 128, :], writes=[qb_[s_]])
                DMA('sync', f'k{s_}', ks[s_][:], kT[h * 128:(h + 1) * 128, :], writes=[kb_[s_]])
                DMA('sync', f'v{s_}', vs[s_][:], Vd[:, h * 128:(h + 1) * 128].rearrange("(n p) c -> p n c", p=128),
                    writes=[vb_[s_]])

            def G1(h):
                s_ = h % 3
                q_, k_ = qs[s_], ks[s_]
                mi = h % 2
                X('vector', lambda e, k_=k_: e.tensor_reduce(out=kmf[:], in_=k_[:].rearrange("p (n t) -> p n t", t=256),
                                                             axis=AX.X, op=ALU.add),
                  reads=[kb_[s_]], writes=[kmB])
                X('vector', lambda e: e.tensor_scalar(out=kmb[:], in0=kmf[:], scalar1=1.0 / 256, scalar2=None,
                                                      op0=ALU.mult), writes=[kmB])

                def gmm(e, q_=q_):
                    ins = None
                    for qi in range(32):
                        ins = e.matmul(ps[4][:, qi * 16:(qi + 1) * 16], lhsT=q_[:, qi * 128:(qi + 1) * 128],
                                       rhs=kmb[:], start=True, stop=True)
                    return ins
                X('tensor', gmm, reads=[qb_[s_], kmB], writes=[psb[4]])
                X('vector', lambda e: e.tensor_tensor(out=gmt[:], in0=mneg[:],
                                                      in1=ps[4][:].rearrange("p (a b) -> p a b", b=16), op=ALU.add),
                  reads=[psb[4], cb], writes=[gmtb])
                for qi in range(32):
                    X('vector', lambda e, qi=qi: e.max(out=m8[:, qi, :], in_=gmt[:, qi, :]),
                      reads=[gmtb], writes=[m8b])
                X('vector', lambda e: e.tensor_tensor(out=alw[:], in0=gmt[:],
                                                      in1=m8[:, :, 2:3].to_broadcast([128, 32, 16]), op=ALU.is_ge),
                  reads=[gmtb, m8b], writes=[alwb])
                X('vector', lambda e: e.tensor_tensor(out=alw[:], in0=alw[:], in1=valid[:], op=ALU.mult),
                  reads=[cb], writes=[alwb])
                X('vector', lambda e: e.tensor_tensor(out=alw[:], in0=alw[:], in1=own[:], op=ALU.add),
                  reads=[cb], writes=[alwb])
                X('vector', lambda e, mi=mi: e.tensor_scalar(out=Mb2[mi][:], in0=alw[:], scalar1=-1.0, scalar2=-NEG,
                                                             op0=ALU.add, op1=ALU.mult), reads=[alwb], writes=[Mbb[mi]])

            def G2(h):
                mi = h % 2
                for g4 in range(4):
                    pb_ = 4
                    pv = ps[pb_][0:16, :].bitcast(BF16)

                    def tr(e, g4=g4, pv=pv, mi=mi):
                        ins = None
                        for j in range(8):
                            qi = g4 * 8 + j
                            ins = e.transpose(out=pv[:, j * 128:(j + 1) * 128], in_=Mb2[mi][:, qi, :], identity=ident_b[:])
                        return ins
                    X('tensor', tr, reads=[Mbb[mi], b_const], writes=[psb[pb_]])
                    X('scalar', lambda e, g4=g4, pv=pv, mi=mi: e.activation(out=MT2[mi][0:16, g4 * 1024:(g4 + 1) * 1024],
                                                                            in_=pv, func=AF.Copy),
                      reads=[psb[pb_]], writes=[MT2b[mi]])

            load_head(0)
            load_head(1)
            G1(0)
            G2(0)
            for h in range(NH):
                s_ = h % 3
                if h + 2 < NH:
                    load_head(h + 2)
                q_, k_, v_ = qs[s_], ks[s_], vs[s_]
                MT = MT2[h % 2]
                MTb = MT2b[h % 2]
                for J in range(8):
                    if h + 1 < NH and J == 2:
                        G1(h + 1)
                    if h + 1 < NH and J == 5:
                        G2(h + 1)
                    nkt = 4 * J + 4
                    PO, PD = (2, 3) if J % 2 == 0 else (6, 7)

                    SB = (0, 1, 5)

                    def smm(kt, J=J, k_=k_, q_=q_, MT=MT, MTb=MTb):
                        pb_ = SB[kt % 3]

                        def f(e):
                            e.matmul(ps[pb_][:], lhsT=k_[:, kt * 128:(kt + 1) * 128], rhs=q_[:, J * 512:(J + 1) * 512],
                                     start=True, stop=False)
                            return e.matmul(ps[pb_][:], lhsT=esel[:, kt // 2, :], rhs=MT[:, J * 512:(J + 1) * 512],
                                            start=False, stop=True)
                        X('tensor', f, reads=[kb_[s_], qb_[s_], MTb, cb], writes=[psb[pb_]])
                    smm(0)
                    smm(1)
                    for kt in range(nkt):
                        if kt + 2 < nkt:
                            smm(kt + 2)
                        pi = prr[0] % 4
                        prr[0] += 1
                        pb_ = SB[kt % 3]
                        X('scalar', lambda e, pi=pi, pb_=pb_: e.activation(out=pT[pi][:], in_=ps[pb_][:], func=AF.Exp,
                                                                           scale=scale),
                          reads=[psb[pb_]], writes=[pTb[pi]])
                        r = kt - 4 * J
                        if r >= 0:
                            X('vector', lambda e, pi=pi, r=r: e.tensor_tensor(out=pT[pi][:], in0=pT[pi][:],
                                                                              in1=caus[:, r, :], op=ALU.mult),
                              reads=[cb], writes=[pTb[pi]])

                        if kt % 2 == 1:
                            def pv_(e, kt=kt, pi=pi, v_=v_, nkt=nkt, PO=PO, PD=PD):
                                e.matmul(ps[PO][:], lhsT=v_[:, kt, :], rhs=pT[pi][:], start=(kt == 0), stop=(kt == nkt - 1))
                                return e.matmul(ps[PD][:], lhsT=ones_b[:], rhs=pT[pi][:], start=(kt == 1), stop=False)
                            X('tensor', pv_, reads=[pTb[pi], vb_[s_], b_const], writes=[psb[PO], psb[PD]])
                        else:
                            def pv_(e, kt=kt, pi=pi, v_=v_, nkt=nkt, PO=PO):
                                return e.matmul(ps[PO][:], lhsT=v_[:, kt, :], rhs=pT[pi][:], start=(kt == 0),
                                                stop=(kt == nkt - 1))
                            X('tensor', pv_, reads=[pTb[pi], vb_[s_]], writes=[psb[PO]])
                            ai = J % 2
                            if kt == 0:
                                X('gpsimd', lambda e, pi=pi, ai=ai: e.tensor_copy(out=dacc[ai][:], in_=pT[pi][:]),
                                  reads=[pTb[pi]], writes=[daccb[ai]])
                            else:
                                X('gpsimd', lambda e, pi=pi, ai=ai: e.tensor_tensor(out=dacc[ai][:], in0=dacc[ai][:],
                                                                                    in1=pT[pi][:], op=ALU.add),
                                  reads=[pTb[pi]], writes=[daccb[ai]])
                    a0_ = J % 2
                    X('tensor', lambda e, a0_=a0_, PD=PD: e.matmul(ps[PD][:], lhsT=ones_f[:], rhs=dacc[a0_][:], start=False, stop=True),
                      reads=[daccb[a0_], cb], writes=[psb[PD]])
                    X('vector', lambda e, PD=PD: e.reciprocal(out=rec[:], in_=ps[PD][:]), reads=[psb[PD]], writes=[recb])
                    oi = orr[0] % 2
                    orr[0] += 1
                    X('vector', lambda e, oi=oi, PO=PO: e.tensor_tensor(out=ot[oi][:], in0=rec[:], in1=ps[PO][:], op=ALU.mult),
                      reads=[psb[PO], recb], writes=[otb[oi]])
                    DMA('sync', f'o{oi}', attnT[h * 128:(h + 1) * 128, J * 512:(J + 1) * 512], ot[oi][:],
                        reads=[otb[oi]])

        if upto >= 'B':
            phase(phaseB)

        def phaseC(st):
            wraw = sb(st, "wraw", [128, 8, 128], F32)
            tril = sb(st, "tril", [128, 128], F32)
            wmb = sb(st, "wmb", [128, 8, 128], BF16)
            wT = sb(st, "wT", [128, 8, 128], BF16)
            bbc = sb(st, "bbc", [128, 8, 128], F32)
            cb = Buf()
            wTb = Buf()
            vg_ = [sb(st, f"vg{i}", [128, 1024], BF16) for i in range(2)]
            u_ = [sb(st, f"u{i}", [128, 8, 128], BF16) for i in range(2)]
            vgb, ub = bufs(2), bufs(2)
            tm = sb(st, "tmC", [128, 8, 128], F32)
            tmb = Buf()
            go = [sb(st, f"go{i}", [128, 8, 128], BF16) for i in range(2)]
            gob = bufs(2)
            DMA('sync', 'c0', wraw[:], sgu_w.rearrange("g t s -> t g s"), writes=[cb])
            DMA('sync', 'c1', tril[:], c_tril, writes=[cb])
            DMA('sync', 'c2', bbc[:].rearrange("p a b -> p (a b)"), sgub_bc, writes=[cb])
            X('vector', lambda e: e.tensor_tensor(out=wmb[:], in0=wraw[:],
                                                  in1=tril[:].unsqueeze(1).to_broadcast([128, 8, 128]), op=ALU.mult),
              reads=[cb], writes=[wTb])
            pv = ps[6][:].bitcast(BF16).rearrange("p (a b) -> p a b", a=8)

            def tr(e):
                ins = None
                for g in range(8):
                    ins = e.transpose(out=pv[:, g, :], in_=wmb[:, g, :], identity=ident_b[:])
                return ins
            X('tensor', tr, reads=[wTb, b_const], writes=[psb[6]])
            X('vector', lambda e: e.tensor_copy(out=wT[:], in_=pv), reads=[psb[6]], writes=[wTb])

            def load(i):
                s_ = i % 2
                DMA('sync', f'a{s_}', vg_[s_][:], vgn[i * 128:(i + 1) * 128, :], writes=[vgb[s_]])
                DMA('sync', f'b{s_}', u_[s_][:], uT[:, i * 128:(i + 1) * 128].rearrange("(g c) t -> c g t", c=128),
                    writes=[ub[s_]])
            load(0)
            for i in range(32):
                s_ = i % 2
                if i + 1 < 32:
                    load(i + 1)
                pa, pb2 = (0, 1) if s_ == 0 else (2, 3)

                def mm(e, s_=s_, pa=pa, pb2=pb2):
                    ins = None
                    for g in range(8):
                        bank = pa if g < 4 else pb2
                        ins = e.matmul(ps[bank][:, (g % 4) * 128:(g % 4 + 1) * 128],
                                       lhsT=vg_[s_][:, g * 128:(g + 1) * 128], rhs=wT[:, g, :], start=True, stop=True)
                    return ins
                X('tensor', mm, reads=[vgb[s_], wTb], writes=[psb[pa], psb[pb2]])
                for hh, bank in enumerate((pa, pb2)):
                    X('vector', lambda e, hh=hh, bank=bank: e.tensor_tensor(
                        out=tm[:, hh * 4:(hh + 1) * 4, :], in0=bbc[:, hh * 4:(hh + 1) * 4, :],
                        in1=ps[bank][:].rearrange("p (a b) -> p a b", a=4), op=ALU.add),
                      reads=[psb[bank], cb], writes=[tmb])
                X('vector', lambda e, s_=s_: e.tensor_tensor(out=go[s_][:], in0=tm[:], in1=u_[s_][:], op=ALU.mult),
                  reads=[tmb, ub[s_]], writes=[gob[s_]])
                DMA('sync', f'g{s_}', GTd[:, i * 128:(i + 1) * 128].rearrange("(g c) t -> c g t", c=128), go[s_][:],
                    reads=[gob[s_]])

        if upto >= 'C':
            phase(phaseC)

        def phaseD(st):
            wpa = sb(st, "wpa", [128, 8, D], BF16)
            wpb = sb(st, "wpb", [128, 8, D], BF16)
            wb = Buf()
            at = [sb(st, f"at{i}", [128, 8, 512], BF16) for i in range(2)]
            gt = [sb(st, f"gt{i}", [128, 8, 512], BF16) for i in range(2)]
            atb, gtb = bufs(2), bufs(2)
            ga_ = [sb(st, f"ga{i}", [128, 512], BF16) for i in range(4)]
            gs_ = [sb(st, f"gs{i}", [128, 512], BF16) for i in range(4)]
            gab, gsb = bufs(4), bufs(4)
            m1 = sb(st, "m1", [128, 512], F32)
            m2 = sb(st, "m2", [128, 512], F32)
            m1b, m2b = Buf(), Buf()
            mo = [sb(st, f"mo{i}", [128, 16, 512], BF16) for i in range(2)]
            mob = bufs(2)
            for hh in range(2):
                DMA('gpsimd', f'c{hh}', wpa[:, :, hh * 1024:(hh + 1) * 1024],
                    w_pa[:, hh * 1024:(hh + 1) * 1024].rearrange("(k p) c -> p k c", p=128), writes=[wb])
                DMA('gpsimd', f'c{2 + hh}', wpb[:, :, hh * 1024:(hh + 1) * 1024],
                    w_pb[:, hh * 1024:(hh + 1) * 1024].rearrange("(k p) c -> p k c", p=128), writes=[wb])

            def load(J):
                s_ = J % 2
                DMA('sync', f'a{s_}', at[s_][:], attnT[:, J * 512:(J + 1) * 512].rearrange("(k p) t -> p k t", p=128),
                    writes=[atb[s_]])
                DMA('sync', f'b{s_}', gt[s_][:], GTd[:, J * 512:(J + 1) * 512].rearrange("(k p) t -> p k t", p=128),
                    writes=[gtb[s_]])
            grr = [0]

            def loadg(J, c):
                gi = grr[0] % 4
                grr[0] += 1
                DMA('sync', f'ga{gi}', ga_[gi][:], sga[c * 128:(c + 1) * 128, J * 512:(J + 1) * 512], writes=[gab[gi]])
                DMA('sync', f'gs{gi}', gs_[gi][:], sgs[c * 128:(c + 1) * 128, J * 512:(J + 1) * 512], writes=[gsb[gi]])
                return gi
            load(0)
            pend = [loadg(0, 0), loadg(0, 1)]
            for J in range(8):
                s_ = J % 2
                if J + 1 < 8:
                    load(J + 1)
                for c in range(16):
                    nxt = J * 16 + c + 2
                    gi = pend.pop(0)
                    if nxt < 128:
                        pend.append(loadg(nxt // 16, nxt % 16))
                    pa, pb2 = (0, 1) if c % 2 == 0 else (2, 3)

                    def mm(e, s_=s_, c=c, pa=pa, pb2=pb2):
                        for kc in range(8):
                            e.matmul(ps[pa][:], lhsT=wpa[:, kc, c * 128:(c + 1) * 128], rhs=at[s_][:, kc, :],
                                     start=(kc == 0), stop=(kc == 7))
                        ins = None
                        for kc in range(8):
                            ins = e.matmul(ps[pb2][:], lhsT=wpb[:, kc, c * 128:(c + 1) * 128], rhs=gt[s_][:, kc, :],
                                           start=(kc == 0), stop=(kc == 7))
                        return ins
                    X('tensor', mm, reads=[wb, atb[s_], gtb[s_]], writes=[psb[pa], psb[pb2]])
                    X('vector', lambda e, gi=gi, pa=pa: e.tensor_tensor(out=m1[:], in0=ga_[gi][:], in1=ps[pa][:],
                                                                        op=ALU.mult),
                      reads=[psb[pa], gab[gi]], writes=[m1b])
                    X('vector', lambda e, gi=gi, pb2=pb2: e.tensor_tensor(out=m2[:], in0=gs_[gi][:], in1=ps[pb2][:],
                                                                          op=ALU.mult),
                      reads=[psb[pb2], gsb[gi]], writes=[m2b])
                    X('vector', lambda e, s_=s_, c=c: e.tensor_tensor(out=mo[s_][:, c, :], in0=m1[:], in1=m2[:],
                                                                      op=ALU.add),
                      reads=[m1b, m2b], writes=[mob[s_]])
                DMA('sync', f'm{s_}', mTd[:, J * 512:(J + 1) * 512].rearrange("(c p) t -> p c t", p=128), mo[s_][:],
                    reads=[mob[s_]])

        if upto >= 'D':
            phase(phaseD)

        def phaseE(st):
            wo = sb(st, "wo", [128, 16, D], BF16)
            wob = Buf()
            wr_ = sb(st, "wrt", [128, 16, 36], F32)
            g2 = sb(st, "g2", [128, D], F32)
            rb = sb(st, "rb", [128, 36], F32)
            ltri = sb(st, "ltri", [128, 128], BF16)
            ecap = sb(st, "ecap", [128, 32], F32)
            cb = Buf()
            mt = [sb(st, f"mt{i}", [128, 16, 512], BF16) for i in range(2)]
            mtb = bufs(2)
            xs = [sb(st, f"xs{i}", [128, D], F32) for i in range(2)]
            xsb = bufs(2)
            x1s = [sb(st, f"x1s{i}", [128, D], F32) for i in range(2)]
            x1b = bufs(2)
            junk = sb(st, "junkE", [128, D], BF16)
            junkb = Buf()
            h2f = [sb(st, f"h2f{i}", [128, D], F32) for i in range(2)]
            h2fb = bufs(2)
            h2b = [sb(st, f"h2b{i}", [128, D], BF16) for i in range(3)]
            h2bb = bufs(3)
            ss2 = sb(st, "ss2", [128, 2], F32)
            ss2b = bufs(2)
            Lb = bufs(2)
            h2T = sb(st, "h2T", [128, 16, 128], F32)
            h2Tb = Buf()
            ss = sb(st, "ssE", [128, 1], F32)
            ssb = Buf()
            Lall = sb(st, "Lall", [128, 32, 36], F32)
            gmx = sb(st, "gmx", [128, 32], F32)
            Gh = sb(st, "Gh", [128, 32, 4], F32)
            dg = sb(st, "dg", [128, 32, 4], F32)
            pg = sb(st, "pg", [128, 32], F32)
            ed = sb(st, "ed", [128, 32], F32)
            den = sb(st, "den", [128, 32], F32)
            m8 = sb(st, "m8E", [128, 32, 8], F32)
            rf = sb(st, "rf", [128, 32], F32)
            h2db = bufs(32)
            rtb = Buf()
            acb = Buf()
            for q4 in range(4):
                DMA('gpsimd', f'c{q4}', wo[:, :, q4 * 512:(q4 + 1) * 512],
                    w_out[:, q4 * 512:(q4 + 1) * 512].rearrange("(k p) c -> p k c", p=128), writes=[wob])
            DMA('sync', 'c4', wr_[:], w_r.rearrange("(k p) c -> p k c", p=128), writes=[cb])
            DMA('sync', 'c5', g2[:], g2_bc, writes=[cb])
            DMA('sync', 'c6', rb[:], rb_bc, writes=[cb])
            DMA('gpsimd', 'c7', ltri[:], c_ltri, writes=[cb])
            DMA('sync', 'c8', ecap[:], c_ecap, writes=[cb])

            def load(J):
                s_ = J % 2
                DMA('sync', f'a{s_}', mt[s_][:], mTd[:, J * 512:(J + 1) * 512].rearrange("(k p) t -> p k t", p=128),
                    writes=[mtb[s_]])
            load(0)

            def S1(i):
                J, r = divmod(i, 4)
                s_ = J % 2
                t0 = i * 128
                xi = i % 2
                hi = i % 2
                bi = i % 3
                if r == 0 and J + 1 < 8:
                    load(J + 1)
                DMA('sync', f'x{xi}', xs[xi][:], x[t0:t0 + 128, :], writes=[xsb[xi]])
                for dt_ in range(4):
                    def mm(e, s_=s_, r=r, dt_=dt_):
                        ins = None
                        for c in range(16):
                            ins = e.matmul(ps[dt_][:], lhsT=mt[s_][:, c, r * 128:(r + 1) * 128],
                                           rhs=wo[:, c, dt_ * 512:(dt_ + 1) * 512], start=(c == 0), stop=(c == 15))
                        return ins
                    X('tensor', mm, reads=[mtb[s_], wob], writes=[psb[dt_]])
                    X('vector', lambda e, xi=xi, dt_=dt_: e.tensor_tensor(
                        out=x1s[xi][:, dt_ * 512:(dt_ + 1) * 512], in0=xs[xi][:, dt_ * 512:(dt_ + 1) * 512],
                        in1=ps[dt_][:], op=ALU.add),
                      reads=[psb[dt_], xsb[xi]], writes=[x1b[xi]])
                DMA('sync', f'y{xi}', x1d[t0:t0 + 128, :], x1s[xi][:], reads=[x1b[xi]])

            def S1b(i):
                t0 = i * 128
                xi = i % 2
                hi = i % 2
                bi = i % 3
                X('scalar', lambda e, xi=xi, hi=hi: e.activation(out=junk[:], in_=x1s[xi][:], func=AF.Square,
                                                                 accum_out=ss2[:, hi:hi + 1]),
                  reads=[x1b[xi]], writes=[junkb, ss2b[hi]])
                rstd_from_ss(ss2[:, hi:hi + 1], ss2b[hi], None)
                X('scalar', lambda e, xi=xi, hi=hi: e.activation(out=h2f[hi][:], in_=x1s[xi][:], func=AF.Copy,
                                                                 scale=ss2[:, hi:hi + 1]),
                  reads=[x1b[xi], ss2b[hi]], writes=[h2fb[hi]])
                X('gpsimd', lambda e, hi=hi: e.tensor_tensor(out=h2f[hi][:], in0=h2f[hi][:], in1=g2[:], op=ALU.mult),
                  reads=[cb], writes=[h2fb[hi]])
                X('scalar', lambda e, hi=hi, bi=bi: e.activation(out=h2b[bi][:], in_=h2f[hi][:], func=AF.Copy),
                  reads=[h2fb[hi]], writes=[h2bb[bi]])
                DMA('sync', f'hd{bi}', h2d[t0:t0 + 128, :], h2b[bi][:], reads=[h2bb[bi]], writes=[h2db[i]])

            def S2(i):
                hi = i % 2
                li = i % 2
                for g4 in range(4):
                    pb_ = 4 + (g4 % 2)

                    def tr(e, g4=g4, pb_=pb_, hi=hi):
                        ins = None
                        for j in range(4):
                            kc = g4 * 4 + j
                            ins = e.transpose(out=ps[pb_][:, j * 128:(j + 1) * 128],
                                              in_=h2f[hi][:, kc * 128:(kc + 1) * 128], identity=ident_f[:])
                        return ins
                    X('tensor', tr, reads=[h2fb[hi], b_const], writes=[psb[pb_]])
                    X('scalar', lambda e, g4=g4, pb_=pb_: e.activation(
                        out=h2T[:, g4 * 4:(g4 + 1) * 4, :], in_=ps[pb_][:].rearrange("p (a b) -> p a b", a=4),
                        func=AF.Copy), reads=[psb[pb_]], writes=[h2Tb])

                def lmm(e):
                    ins = None
                    for kc in range(16):
                        ins = e.matmul(ps[6][:, 0:36], lhsT=h2T[:, kc, :], rhs=wr_[:, kc, :], start=(kc == 0),
                                       stop=(kc == 15))
                    return ins
                X('tensor', lmm, reads=[h2Tb, cb], writes=[psb[6]])
                X('vector', lambda e, i=i: e.tensor_tensor(out=Lall[:, i, :], in0=rb[:], in1=ps[6][:, 0:36], op=ALU.add),
                  reads=[psb[6], cb], writes=[Lb[li]])

            S1(0)
            S1b(0)
            for i in range(32):
                if i + 1 < 32:
                    S1(i + 1)
                S2(i)
                if i + 1 < 32:
                    S1b(i + 1)
            v3 = lambda ap_: ap_.rearrange("p (t e) -> p t e", e=32)
            Lm = v3(xs[0][:, 0:1024])
            A0 = v3(xs[0][:, 1024:2048])
            A1 = v3(xs[1][:, 0:1024])
            posE = v3(xs[1][:, 1024:2048])
            okm = v3(x1s[0][:, 0:1024])
            t32 = v3(x1s[0][:, 1024:2048])
            Ab = v3(junk[:, 0:1024])
            X('vector', lambda e: e.memset(gmx[:], 0.0), writes=[rtb, xsb[0], xsb[1], x1b[0], x1b[1], junkb])
            V_ = lambda fn, reads=(), writes=(): X('vector', fn, reads=list(reads), writes=[rtb] + list(writes))
            Lg = Lall[:, :, 0:4]
            Le4 = Lall[:, :, 4:36].rearrange("p t (a b) -> p t a b", a=4)
            V_(lambda e: e.tensor_reduce(out=gmx[:], in_=Lg, axis=AX.X, op=ALU.max), reads=Lb)
            V_(lambda e: e.tensor_tensor(out=Gh[:], in0=Lg, in1=gmx[:].unsqueeze(2).to_broadcast([128, 32, 4]),
                                         op=ALU.is_ge))
            V_(lambda e: e.tensor_tensor(out=dg[:], in0=Lg, in1=gmx[:].unsqueeze(2).to_broadcast([128, 32, 4]),
                                         op=ALU.subtract))
            X('scalar', lambda e: e.activation(out=dg[:], in_=dg[:], func=AF.Exp), writes=[rtb])
            V_(lambda e: e.tensor_reduce(out=pg[:], in_=dg[:], axis=AX.X, op=ALU.add))
            V_(lambda e: e.reciprocal(out=pg[:], in_=pg[:]))
            V_(lambda e: e.tensor_scalar(out=Gh[:], in0=Gh[:], scalar1=-1.0, scalar2=1e30, op0=ALU.add, op1=ALU.mult))
            V_(lambda e: e.tensor_tensor(out=Lm.rearrange("p t (a b) -> p t a b", a=4), in0=Le4,
                                         in1=Gh[:].unsqueeze(3).to_broadcast([128, 32, 4, 8]), op=ALU.add))
            for i in range(32):
                V_(lambda e, i=i: e.max(out=m8[:, i, :], in_=Lm[:, i, :]))
            V_(lambda e: e.tensor_tensor(out=A0, in0=Lm, in1=m8[:, :, 0:1].to_broadcast([128, 32, 32]),
                                         op=ALU.is_equal))
            V_(lambda e: e.tensor_tensor(out=A1, in0=Lm, in1=m8[:, :, 1:2].to_broadcast([128, 32, 32]),
                                         op=ALU.is_equal))
            V_(lambda e: e.tensor_tensor(out=ed[:].unsqueeze(2), in0=m8[:, :, 1:2], in1=m8[:, :, 0:1], op=ALU.subtract))
            X('scalar', lambda e: e.activation(out=ed[:], in_=ed[:], func=AF.Exp), writes=[rtb])
            V_(lambda e: e.tensor_scalar(out=den[:], in0=ed[:], scalar1=1.0, scalar2=None, op0=ALU.add))
            V_(lambda e: e.reciprocal(out=den[:], in_=den[:]))
            V_(lambda e: e.tensor_tensor(out=W0[:], in0=pg[:], in1=den[:], op=ALU.mult), writes=[b_rt])
            V_(lambda e: e.tensor_tensor(out=W1[:], in0=W0[:], in1=ed[:], op=ALU.mult), writes=[b_rt])
            V_(lambda e: e.tensor_tensor(out=Ab, in0=A0, in1=A1, op=ALU.add))
            for hf in range(2):
                def pmm(e, hf=hf):
                    ins = None
                    for ii in range(16):
                        i = hf * 16 + ii
                        o_ = ps[6 + hf][:, ii * 32:(ii + 1) * 32]
                        ins = e.matmul(o_, lhsT=ltri[:], rhs=Ab[:, i, :], start=True, stop=(i == 0))
                        for j in range(i):
                            ins = e.matmul(o_, lhsT=ones_b[:], rhs=Ab[:, j, :], start=False, stop=(j == i - 1))
                    return ins
                X('tensor', pmm, reads=[rtb, cb, b_const], writes=[psb[6 + hf]])
                hs = slice(hf * 16, (hf + 1) * 16)
                pv3 = ps[6 + hf][:].rearrange("p (t e) -> p t e", e=32)
                V_(lambda e, hs=hs, pv3=pv3: e.tensor_scalar(out=okm[:, hs, :], in0=pv3, scalar1=float(CAP), scalar2=None,
                                                             op0=ALU.is_lt), reads=[psb[6 + hf]])
                V_(lambda e, hs=hs, pv3=pv3: e.tensor_tensor(out=posE[:, hs, :],
                                                             in0=ecap[:].unsqueeze(1).to_broadcast([128, 16, 32]),
                                                             in1=pv3, op=ALU.add), reads=[psb[6 + hf], cb])
            V_(lambda e: e.scalar_tensor_tensor(out=posE, in0=posE, scalar=-float(DUMP), in1=okm,
                                                op0=ALU.add, op1=ALU.mult))
            for sl_, A_, R_ in ((0, A0, R0), (1, A1, R1)):
                V_(lambda e, A_=A_: e.tensor_tensor(out=t32, in0=A_, in1=posE, op=ALU.mult))
                V_(lambda e: e.tensor_reduce(out=rf[:], in_=t32, axis=AX.X, op=ALU.add))
                V_(lambda e: e.tensor_scalar(out=rf[:], in0=rf[:], scalar1=float(DUMP), scalar2=None, op0=ALU.add))
                V_(lambda e, R_=R_: e.tensor_copy(out=R_[:], in_=rf[:]), writes=[b_rt])
            for i in range(32):
                bi = i % 3
                DMA('sync', f'hb{bi}', h2b[bi][:], h2d[i * 128:(i + 1) * 128, :], reads=[h2db[i]], writes=[h2bb[bi]])
                for sl_, R_ in ((0, R0), (1, R1)):
                    deps = _deps([h2bb[bi], b_rt], [])
                    tok = P.dma('gpsimd', f'sc{bi}{sl_}',
                                lambda e, R_=R_, i=i, bi=bi: e.indirect_dma_start(
                                    out=disp, out_offset=bass.IndirectOffsetOnAxis(ap=R_[:, i:i + 1], axis=0),
                                    in_=h2b[bi][:], in_offset=None), deps)
                    _upd(tok, [h2bb[bi], b_rt], [])
            if debug:
                X('vector', lambda e: e.tensor_copy(out=h2f[0][:, 0:32], in_=R0[:]), reads=[b_rt], writes=[h2fb[0]])
                X('vector', lambda e: e.tensor_copy(out=h2f[0][:, 32:64], in_=R1[:]), reads=[b_rt], writes=[h2fb[0]])
                X('vector', lambda e: e.tensor_copy(out=h2f[0][:, 64:96], in_=W0[:]), reads=[b_rt], writes=[h2fb[0]])
                X('vector', lambda e: e.tensor_copy(out=h2f[0][:, 96:128], in_=W1[:]), reads=[b_rt], writes=[h2fb[0]])
                DMA('sync', 'dbg', rtab, h2f[0][:, 0:128], reads=[h2fb[0]])

        if upto >= 'E':
            phase(phaseE)

        def phaseF(st):
            NR = 8
            ring = [sb(st, f"rg{i}", [128, 4096], BF16) for i in range(NR)]
            ringb = bufs(NR)
            xb = [sb(st, f"xb{i}", [128, NT, D], BF16) for i in range(2)]
            xbb = bufs(2)
            xbT = sb(st, "xbT", [128, 16, CAP], BF16)
            xbTb = Buf()
            hid = sb(st, "hid", [128, 8, CAP], BF16)
            hidb = bufs(8)
            sg = [sb(st, f"sg{i}", [128, CAP], F32) for i in range(2)]
            sgb = bufs(2)
            seq = []
            for e_ in range(NE):
                for j in range(4):
                    seq.append((e_, 'g', j))
                    seq.append((e_, 'u', j))
                for dt_ in range(4):
                    seq.append((e_, 'd', dt_))
            slot_of = {}
            nxt = [0]

            def issue_load():
                if nxt[0] >= len(seq):
                    return
                n = nxt[0]
                nxt[0] += 1
                e_, kind, j = seq[n]
                s_ = n % NR
                slot_of[(e_, kind, j)] = s_
                if kind == 'd':
                    src = wd[e_][:, j * 512:(j + 1) * 512].rearrange("(k p) c -> p k c", p=128)
                    dst = ring[s_][:].rearrange("p (k c) -> p k c", k=8)
                else:
                    wsrc = wg if kind == 'g' else wu
                    src = wsrc[e_][:, j * 256:(j + 1) * 256].rearrange("(k p) c -> p k c", p=128)
                    dst = ring[s_][:].rearrange("p (k c) -> p k c", k=16)
                DMA('gpsimd', f'r{s_}', dst, src, writes=[ringb[s_]])

            def load_x(e_):
                s_ = e_ % 2
                DMA('sync', f'x{s_}', xb[s_][:], disp[e_ * CAP:(e_ + 1) * CAP, :].rearrange("(n p) d -> p n d", p=128),
                    writes=[xbb[s_]])
            load_x(0)
            for _ in range(NR):
                issue_load()
            prr = [0]
            yrr = [0]
            for e_ in range(NE):
                xs_ = e_ % 2
                if e_ + 1 < NE:
                    load_x(e_ + 1)
                for k2 in range(8):
                    pb_ = 6 + (k2 % 2)
                    pv = ps[pb_][:].bitcast(BF16)[:, 0:2 * CAP].rearrange("p (a b) -> p a b", a=2)

                    def tr(e, k2=k2, pv=pv, xs_=xs_):
                        ins = None
                        for a in range(2):
                            kc = k2 * 2 + a
                            for n in range(NT):
                                ins = e.transpose(out=pv[:, a, n * 128:(n + 1) * 128],
                                                  in_=xb[xs_][:, n, kc * 128:(kc + 1) * 128], identity=ident_b[:])
                        return ins
                    X('tensor', tr, reads=[xbb[xs_], b_const], writes=[psb[pb_]])
                    X('vector', lambda e, k2=k2, pv=pv: e.tensor_copy(out=xbT[:, k2 * 2:(k2 + 1) * 2, :], in_=pv),
                      reads=[psb[pb_]], writes=[xbTb])
                for j in range(4):
                    sg_ = slot_of[(e_, 'g', j)]
                    su_ = slot_of[(e_, 'u', j)]
                    wgv = ring[sg_][:].rearrange("p (k c) -> p k c", k=16)
                    wuv = ring[su_][:].rearrange("p (k c) -> p k c", k=16)
                    for a in range(2):
                        fc = j * 2 + a
                        pg = (prr[0] % 2) * 2
                        prr[0] += 1
                        pu = pg + 1

                        def mm(e, a=a, wgv=wgv, wuv=wuv, pg=pg, pu=pu):
                            for kc in range(16):
                                e.matmul(ps[pg][:, 0:CAP], lhsT=wgv[:, kc, a * 128:(a + 1) * 128], rhs=xbT[:, kc, :],
                                         start=(kc == 0), stop=(kc == 15))
                            ins = None
                            for kc in range(16):
                                ins = e.matmul(ps[pu][:, 0:CAP], lhsT=wuv[:, kc, a * 128:(a + 1) * 128], rhs=xbT[:, kc, :],
                                               start=(kc == 0), stop=(kc == 15))
                            return ins
                        X('tensor', mm, reads=[xbTb, ringb[sg_], ringb[su_]], writes=[psb[pg], psb[pu]])
                        si = fc % 2
                        X('scalar', lambda e, si=si, pg=pg: e.activation(out=sg[si][:], in_=ps[pg][:, 0:CAP], func=AF.Silu),
                          reads=[psb[pg]], writes=[sgb[si]])
                        X('vector', lambda e, si=si, pu=pu, fc=fc: e.tensor_tensor(out=hid[:, fc, :], in0=sg[si][:],
                                                                                   in1=ps[pu][:, 0:CAP], op=ALU.mult),
                          reads=[sgb[si], psb[pu]], writes=[hidb[fc]])
                    issue_load()
                    issue_load()
                for dt_ in range(4):
                    sd_ = slot_of[(e_, 'd', dt_)]
                    wdv = ring[sd_][:].rearrange("p (k c) -> p k c", k=8)
                    for n in range(NT):
                        pb_ = 4 + (prr[0] % 2)
                        prr[0] += 1

                        def mm(e, n=n, wdv=wdv, pb_=pb_):
                            ins = None
                            for fc in range(8):
                                ins = e.matmul(ps[pb_][:], lhsT=hid[:, fc, n * 128:(n + 1) * 128], rhs=wdv[:, fc, :],
                                               start=(fc == 0), stop=(fc == 7))
                            return ins
                        X('tensor', mm, reads=hidb + [ringb[sd_]], writes=[psb[pb_]])
                        X('scalar' if (n % 2) else 'vector',
                          (lambda e, pb_=pb_, n=n, dt_=dt_: e.activation(out=yst[n][:, dt_ * 512:(dt_ + 1) * 512],
                                                                         in_=ps[pb_][:], func=AF.Copy)) if (n % 2) else
                          (lambda e, pb_=pb_, n=n, dt_=dt_: e.tensor_copy(out=yst[n][:, dt_ * 512:(dt_ + 1) * 512],
                                                                          in_=ps[pb_][:])),
                          reads=[psb[pb_]], writes=[ystb[n]])
                    issue_load()
                for n in range(NT):
                    r0_ = e_ * CAP + n * 128
                    DMA('sync', f'y{n}', ydisp[r0_:r0_ + 128, :], yst[n][:], reads=[ystb[n]])

        yst = None
        ystb = None

        def phaseF_wrap(st):
            nonlocal yst, ystb
            yst = [sb(st, f"yst{i}", [128, D], BF16) for i in range(NT)]
            ystb = bufs(NT)
            phaseF(st)

        if upto >= 'F':
            phase(phaseF_wrap)

        def phaseG(st):
            gf = sb(st, "gf", [128, D], F32)
            cb = Buf()
            y0 = [sb(st, f"y0{i}", [128, D], BF16) for i in range(3)]
            y1 = [sb(st, f"y1{i}", [128, D], BF16) for i in range(3)]
            x1t = [sb(st, f"x1t{i}", [128, D], F32) for i in range(3)]
            y0b, y1b, x1tb = bufs(3), bufs(3), bufs(3)
            acc2 = [sb(st, f"acc{i}", [128, D], F32) for i in range(2)]
            acc2b = bufs(2)
            ssg = sb(st, "ssg2", [128, 2], F32)
            ssgb = bufs(2)
            t0g = [sb(st, f"t0g{i}", [128, D], F32) for i in range(2)]
            t1g = [sb(st, f"t1g{i}", [128, D], F32) for i in range(2)]
            t0gb, t1gb = bufs(2), bufs(2)
            junk = sb(st, "junkG", [128, D], BF16)
            junkb = Buf()
            ss = sb(st, "ssG", [128, 1], F32)
            ssb = Buf()
            ob = [sb(st, f"ob{i}", [128, D], F32) for i in range(2)]
            obb = bufs(2)
            DMA('sync', 'c0', gf[:], gf_bc, writes=[cb])

            def load(i):
                s_ = i % 3
                for nm, yt, ytb, R_ in (('g0', y0, y0b, R0), ('g1', y1, y1b, R1)):
                    deps = _deps([b_rt], [ytb[s_]])
                    tok = P.dma('gpsimd', f'{nm}{s_}',
                                lambda e, yt=yt, R_=R_, i=i, s_=s_: e.indirect_dma_start(
                                    out=yt[s_][:], out_offset=None, in_=ydisp,
                                    in_offset=bass.IndirectOffsetOnAxis(ap=R_[:, i:i + 1], axis=0)), deps)
                    _upd(tok, [b_rt], [ytb[s_]])
                DMA('sync', f'x{s_}', x1t[s_][:], x1d[i * 128:(i + 1) * 128, :], writes=[x1tb[s_]])
            load(0)
            load(1)
            for i in range(32):
                s_ = i % 2
                l_ = i % 3
                if i + 2 < 32:
                    load(i + 2)
                X('scalar', lambda e, s_=s_, i=i, l_=l_: e.activation(out=t0g[s_][:], in_=y0[l_][:], func=AF.Copy,
                                                               scale=W0[:, i:i + 1]),
                  reads=[y0b[l_], b_rt], writes=[t0gb[s_]])
                X('scalar', lambda e, s_=s_, i=i, l_=l_: e.activation(out=t1g[s_][:], in_=y1[l_][:], func=AF.Copy,
                                                               scale=W1[:, i:i + 1]),
                  reads=[y1b[l_], b_rt], writes=[t1gb[s_]])
                X('vector', lambda e, s_=s_, l_=l_: e.tensor_tensor(out=acc2[s_][:], in0=t0g[s_][:], in1=x1t[l_][:], op=ALU.add),
                  reads=[t0gb[s_], x1tb[l_]], writes=[acc2b[s_]])
                X('vector', lambda e, s_=s_: e.tensor_tensor(out=acc2[s_][:], in0=acc2[s_][:], in1=t1g[s_][:], op=ALU.add),
                  reads=[t1gb[s_]], writes=[acc2b[s_]])
                X('scalar', lambda e, s_=s_: e.activation(out=junk[:], in_=acc2[s_][:], func=AF.Square,
                                                          accum_out=ssg[:, s_:s_ + 1]),
                  reads=[acc2b[s_]], writes=[junkb, ssgb[s_]])
                rstd_from_ss(ssg[:, s_:s_ + 1], ssgb[s_], None)
                X('scalar', lambda e, s_=s_: e.activation(out=acc2[s_][:], in_=acc2[s_][:], func=AF.Copy,
                                                          scale=ssg[:, s_:s_ + 1]),
                  reads=[ssgb[s_]], writes=[acc2b[s_]])
                X('vector', lambda e, s_=s_: e.tensor_tensor(out=ob[s_][:], in0=acc2[s_][:], in1=gf[:], op=ALU.mult),
                  reads=[acc2b[s_], cb], writes=[obb[s_]])
                DMA('sync', f'o{s_}', out[i * 128:(i + 1) * 128, :], ob[s_][:], reads=[obb[s_]])

        if upto >= 'G':
            phase(phaseG)
    return nc


def make_consts():
    c = {}
    c["c_ident"] = np.eye(128, dtype=np.float32)
    tp = np.arange(128)
    c["c_ltri"] = (tp[:, None] < tp[None, :]).astype(np.float32)
    c["c_ones"] = np.ones((128, 128), np.float32)
    half = 16
    inv = (500000.0 ** (-np.arange(half, dtype=np.float32) * 2.0 / 32)).astype(np.float32)
    ang = np.arange(S, dtype=np.float32)[None, :] * inv[:, None]
    c["c_cos"] = np.concatenate([np.cos(ang), np.cos(ang), np.ones((96, S))], 0).astype(np.float32)
    c["c_sin"] = np.concatenate([np.sin(ang), np.sin(ang), np.zeros((96, S))], 0).astype(np.float32)
    R = np.zeros((128, 128), np.float32)
    for j in range(16):
        R[j + 16, j] = -1.0
        R[j, j + 16] = 1.0
    c["c_rot"] = R
    n = np.arange(16)[None, :]
    blk = (np.arange(32) // 2)[:, None]
    mneg = np.where(n >= blk, -1e30, 0.0).astype(np.float32)
    valid = (n < blk).astype(np.float32)
    own = (n == blk).astype(np.float32)
    c["c_mneg"] = np.ascontiguousarray(np.broadcast_to(mneg.reshape(1, 512), (128, 512)))
    c["c_valid"] = np.ascontiguousarray(np.broadcast_to(valid.reshape(1, 512), (128, 512)))
    c["c_own"] = np.ascontiguousarray(np.broadcast_to(own.reshape(1, 512), (128, 512)))
    es = np.zeros((128, 16, 128), np.float32)
    for i in range(16):
        es[i, i, :] = 1.0
    c["c_esel"] = es.reshape(128, 2048)
    kl = np.arange(128)[:, None]
    ii = np.arange(512)[None, :]
    ca = np.ones((128, 4, 512), np.float32)
    ca[:, 0, :] = np.where(ii < 256, (kl <= ii), 1.0)
    ca[:, 1, :] = np.where(ii < 256, (128 + kl <= ii), 1.0)
    ca[:, 2, :] = np.where(ii >= 256, (kl <= ii - 256), 1.0)
    ca[:, 3, :] = np.where(ii >= 256, (128 + kl <= ii - 256), 1.0)
    c["c_caus"] = ca.reshape(128, 2048)
    c["c_tril"] = (tp[None, :] <= tp[:, None]).astype(np.float32)
    c["c_ecap"] = np.ascontiguousarray(np.broadcast_to((np.arange(32, dtype=np.float32) * CAP)[None, :], (128, 32)))
    return c


def make_shared(inp):
    f = lambda a: np.ascontiguousarray(np.asarray(a, dtype=np.float32))
    bc = lambda v, n: np.ascontiguousarray(np.broadcast_to(f(v).reshape(1, n), (128, n)))
    sh = {}
    sh["w_in"] = f(inp["w_in"][0])
    sh["sgu_w"] = f(inp["sgu_w"][0])
    sh["w_pa"] = f(inp["w_proj_attn"][0])
    sh["w_pb"] = f(inp["w_proj_sgu"][0])
    sh["w_out"] = f(inp["w_out"][0])
    sh["w_r"] = np.ascontiguousarray(np.concatenate([f(inp["w_router_group"][0]), f(inp["w_router_expert"][0])], axis=1))
    sh["wg"] = f(inp["w_exp_gate"][0])
    sh["wu"] = f(inp["w_exp_up"][0])
    sh["wd"] = f(inp["w_exp_down"][0])
    sh["gmix"] = np.ascontiguousarray(f(inp["norm_mix_g"][0]).reshape(16, 128).T)
    sh["ln_g_bc"] = bc(inp["sgu_ln_g"][0], 1024)
    sh["ln_b_bc"] = bc(inp["sgu_ln_b"][0], 1024)
    sh["sgub_bc"] = bc(f(inp["sgu_b"][0]).reshape(-1), 1024)
    sh["g2_bc"] = bc(inp["norm_ffn_g"][0], D)
    sh["gf_bc"] = bc(inp["norm_final_g"], D)
    sh["rb_bc"] = bc(np.concatenate([f(inp["b_router_group"][0]), f(inp["b_router_expert"][0])]), 36)
    sh.update(make_consts())
    return sh


_NC_CACHE = {}


def kernel(**inputs):
    xfull = np.asarray(inputs["x"], dtype=np.float32)
    B = xfull.shape[0]
    sh = make_shared(inputs)
    if "nc" not in _NC_CACHE:
        _NC_CACHE["nc"] = build()
    nc = _NC_CACHE["nc"]
    in_maps = []
    for c in range(B):
        m = dict(sh)
        m["x"] = np.ascontiguousarray(xfull[c])
        in_maps.append(m)
    res = run_bass_kernel_spmd(nc, in_maps, core_ids=list(range(B)))
    return np.stack([np.asarray(r["out"]) for r in res.results], axis=0).astype(np.float32)
```

```python
import numpy as np
from contextlib import ExitStack
import concourse.bass as bass
import concourse.mybir as mybir
from concourse.bass_utils import run_bass_kernel_spmd

F32 = mybir.dt.float32
BF16 = mybir.dt.bfloat16
I32 = mybir.dt.int32
AF = mybir.ActivationFunctionType
ALU = mybir.AluOpType
AX = mybir.AxisListType

S = 4096
D = 2048
NH = 8
DH = 128
DIN = 9216
NE = 32
DFF = 1024
CAP = 512
NT = CAP // 128
NROW = NE * CAP + 128
DUMP = NE * CAP
EPS = 1e-6
NEG = -30000.0
ENG = ['tensor', 'vector', 'scalar', 'gpsimd', 'sync']
import os
OPTS = set(os.environ.get('KOPTS', 'zero,p2,h1').split(','))


class Prog:
    def __init__(self, nc, es):
        self.nc = nc
        self.es = es
        self.q = {e: [] for e in ENG}
        self.sems = {}
        self.count = {}
        self.seen = {e: {} for e in ENG}
        for e in ENG:
            self._sem('E_' + e)

    def _sem(self, key):
        if key not in self.sems:
            self.sems[key] = self.es.enter_context(self.nc.semaphore(key))
            self.count[key] = 0
        return self.sems[key]

    def _waits(self, eng, deps):
        waits = {}
        for d in deps:
            if d is None:
                continue
            k, n = d
            if n > self.seen[eng].get(k, 0):
                waits[k] = max(waits.get(k, 0), n)
        for k, n in waits.items():
            self.seen[eng][k] = n
        return list(waits.items())

    def op(self, eng, fn, deps=()):
        w = self._waits(eng, deps)
        key = 'E_' + eng
        self.count[key] += 1
        self.q[eng].append((fn, w, key, 1))
        return (key, self.count[key])

    def dma(self, eng, semkey, fn, deps=()):
        semkey = eng[0] + '_' + semkey
        self._sem(semkey)
        w = self._waits(eng, deps)
        self.count[semkey] += 16
        self.q[eng].append((fn, w, semkey, 16))
        return (semkey, self.count[semkey])

    def barrier(self):
        toks = [(k, c) for k, c in self.count.items() if c > 0]
        for e in ENG:
            w = self._waits(e, toks)
            if w:
                self.q[e].append((None, w, None, 0))

    def emit(self, block):
        sems = self.sems
        q = self.q
        self.q = {e: [] for e in ENG}

        def run(engname):
            def body(e):
                for fn, w, key, inc in q[engname]:
                    for k, n in w:
                        e.wait_ge(sems[k], n)
                    if fn is not None:
                        ins = fn(e)
                        ins.then_inc(sems[key], inc)
            return body
        block.tensor(run('tensor'))
        block.vector(run('vector'))
        block.scalar(run('scalar'))
        block.gpsimd(run('gpsimd'))
        block.sync(run('sync'))


class Buf:
    def __init__(self):
        self.w = None
        self.r = []


def _deps(reads, writes):
    deps = []
    for b in reads:
        deps.append(b.w)
    for b in writes:
        deps.append(b.w)
        deps.extend(b.r)
    return deps


def _upd(tok, reads, writes):
    for b in reads:
        b.r.append(tok)
    for b in writes:
        b.w = tok
        b.r = []


class K:
    def __init__(self, nc, P):
        self.nc = nc
        self.P = P

    def X(self, eng, fn, reads=(), writes=()):
        deps = _deps(reads, writes)
        if eng == 'tensor':
            deps = [d for d in deps if d is not None and d[0] != 'E_tensor']
        tok = self.P.op(eng, fn, deps)
        _upd(tok, reads, writes)
        return tok

    def DMA(self, eng, semkey, out, in_, reads=(), writes=()):
        deps = _deps(reads, writes)
        tok = self.P.dma(eng, semkey, lambda e, out=out, in_=in_: e.dma_start(out=out, in_=in_), deps)
        _upd(tok, reads, writes)
        return tok


def bufs(n):
    return [Buf() for _ in range(n)]


def build(debug=False, upto='G'):
    nc = bass.Bass("TRN2", target_bir_lowering=False)

    def din(name, shape, dt=F32):
        return nc.dram_tensor(name, list(shape), dt, kind="ExternalInput").ap()

    def dscr(name, shape, dt):
        kind = "ExternalOutput" if (debug and name in debug) else "Internal"
        return nc.dram_tensor(name, list(shape), dt, kind=kind).ap()

    x = din("x", [S, D])
    w_in = din("w_in", [D, DIN])
    sgu_w = din("sgu_w", [8, 128, 128])
    w_pa = din("w_pa", [1024, D])
    w_pb = din("w_pb", [1024, D])
    w_out = din("w_out", [D, D])
    w_r = din("w_r", [D, 36])
    wg = din("wg", [NE, D, DFF])
    wu = din("wu", [NE, D, DFF])
    wd = din("wd", [NE, DFF, D])
    gmix = din("gmix", [128, 16])
    ln_g_bc = din("ln_g_bc", [128, 1024])
    ln_b_bc = din("ln_b_bc", [128, 1024])
    sgub_bc = din("sgub_bc", [128, 1024])
    g2_bc = din("g2_bc", [128, D])
    gf_bc = din("gf_bc", [128, D])
    rb_bc = din("rb_bc", [128, 36])
    c_ident = din("c_ident", [128, 128])
    c_ltri = din("c_ltri", [128, 128])
    c_ones = din("c_ones", [128, 128])
    c_cos = din("c_cos", [128, S])
    c_sin = din("c_sin", [128, S])
    c_rot = din("c_rot", [128, 128])
    c_mneg = din("c_mneg", [128, 512])
    c_valid = din("c_valid", [128, 512])
    c_own = din("c_own", [128, 512])
    c_esel = din("c_esel", [128, 16 * 128])
    c_caus = din("c_caus", [128, 4 * 512])
    c_tril = din("c_tril", [128, 128])
    c_ecap = din("c_ecap", [128, 32])

    out = nc.dram_tensor("out", [S, D], F32, kind="ExternalOutput").ap()

    qT = dscr("qT", [NH * 128, S], BF16)
    kT = dscr("kT", [NH * 128, S], BF16)
    Vd = dscr("Vd", [S, 1024], BF16)
    uT = dscr("uT", [1024, S], BF16)
    vgn = dscr("vgn", [S, 1024], BF16)
    sga = dscr("sga", [D, S], BF16)
    sgs = dscr("sgs", [D, S], BF16)
    attnT = dscr("attnT", [1024, S], BF16)
    GTd = dscr("GTd", [1024, S], BF16)
    mTd = dscr("mTd", [D, S], BF16)
    x1d = dscr("x1d", [S, D], F32)
    h2d = dscr("h2d", [S, D], BF16)
    disp = dscr("disp", [NROW, D], BF16)
    ydisp = dscr("ydisp", [NROW, D], BF16)
    rtab = dscr("rtab", [128, 128], F32)

    with ExitStack() as es:
        P = Prog(nc, es)
        k = K(nc, P)
        X, DMA = k.X, k.DMA
        ps = [es.enter_context(nc.psum_tensor(f"ps{i}", [128, 512], F32)) for i in range(8)]
        psb = bufs(8)

        uniq = [0]

        def sb(st, name, shape, dt):
            uniq[0] += 1
            return st.enter_context(nc.sbuf_tensor(f"{name}_{uniq[0]}", list(shape), dt))

        ident_b = sb(es, "ident_b", [128, 128], BF16)
        ident_f = sb(es, "ident_f", [128, 128], F32)
        ones_b = sb(es, "ones_b", [128, 128], BF16)
        eps_t = sb(es, "eps_t", [128, 1], F32)
        R0 = sb(es, "R0", [128, 32], I32)
        R1 = sb(es, "R1", [128, 32], I32)
        W0 = sb(es, "W0", [128, 32], F32)
        W1 = sb(es, "W1", [128, 32], F32)
        b_const = Buf()
        b_rt = Buf()
        rt_t = bufs(32)

        def phase(fn):
            with ExitStack() as st:
                block = st.enter_context(nc.Block())
                fn(st)
                P.barrier()
                P.emit(block)

        def gelu_tanh(src_ps, srcbuf, tmp, tmpbuf, dst, dstbuf, extra_reads=()):
            X('scalar', lambda e: e.activation(out=tmp, in_=src_ps, func=AF.Square),
              reads=[srcbuf], writes=[tmpbuf])
            X('vector', lambda e: e.tensor_scalar(out=tmp, in0=tmp, scalar1=0.044715, scalar2=1.0,
                                                  op0=ALU.mult, op1=ALU.add),
              reads=[], writes=[tmpbuf])
            X('vector', lambda e: e.tensor_tensor(out=tmp, in0=tmp, in1=src_ps, op=ALU.mult),
              reads=[srcbuf], writes=[tmpbuf])
            X('scalar', lambda e: e.activation(out=tmp, in_=tmp, func=AF.Sigmoid, scale=1.5957691216057308),
              reads=[], writes=[tmpbuf])
            return X('vector', lambda e: e.tensor_tensor(out=dst, in0=tmp, in1=src_ps, op=ALU.mult),
                     reads=[srcbuf, tmpbuf] + list(extra_reads), writes=[dstbuf])

        def rstd_from_ss(ss, ssb, tmpb):
            X('scalar', lambda e: e.activation(out=ss, in_=ss, func=AF.Ln, bias=eps_t[:, 0:1], scale=1.0 / D),
              reads=[b_const], writes=[ssb])
            X('scalar', lambda e: e.activation(out=ss, in_=ss, func=AF.Exp, scale=-0.5), writes=[ssb])

        def phaseA(st):
            hT = sb(st, "hT", [128, 16, 2048], BF16)
            cosT = sb(st, "cosT", [128, 2048], F32)
            sinT = sb(st, "sinT", [128, 2048], F32)
            rot = sb(st, "rot", [128, 128], BF16)
            gm = sb(st, "gm", [128, 16], F32)
            lng = sb(st, "lng", [128, 1024], F32)
            lnb = sb(st, "lnb", [128, 1024], F32)
            xs = [sb(st, f"xs{i}", [128, D], F32) for i in range(2)]
            xsb = bufs(2)
            xn2 = [sb(st, f"xn{i}", [128, D], BF16) for i in range(2)]
            xn2b = bufs(2)
            junkA = sb(st, "junkA", [128, D], BF16)
            junkAb = Buf()
            ssA2 = sb(st, "ssA2", [128, 2], F32)
            ssA2b = bufs(2)
            ssb = Buf()
            wr = [sb(st, f"wr{i}", [128, 16, 512], BF16) for i in range(3)]
            wrb = bufs(3)
            stg = [sb(st, f"stg{i}", [128, 4, 512], BF16) for i in range(2)]
            stgb = bufs(2)
            tmp = [sb(st, f"tmpA{i}", [128, 512], F32) for i in range(2)]
            tmpb = bufs(2)
            gv2 = [sb(st, f"gv{i}", [128, 1024], F32) for i in range(2)]
            gv2b = bufs(2)
            bst2 = [sb(st, f"bst{i}", [128, 2, 6], F32) for i in range(2)]
            mv2 = [sb(st, f"mv{i}", [128, 2], F32) for i in range(2)]
            mvb2 = bufs(2)
            vst = [sb(st, f"vst{i}", [128, 1024], BF16) for i in range(2)]
            vstb = bufs(2)
            hTb = Buf()
            cb = Buf()

            DMA('gpsimd', 'c0', ident_b[:], c_ident, writes=[b_const])
            DMA('sync', 'c1', ident_f[:], c_ident, writes=[b_const])
            DMA('gpsimd', 'c2', ones_b[:], c_ones, writes=[b_const])
            X('vector', lambda e: e.memset(eps_t[:], EPS), writes=[b_const])
            DMA('gpsimd', 'c3', rot[:], c_rot, writes=[cb])
            DMA('sync', 'c4', gm[:], gmix, writes=[cb])
            DMA('sync', 'c5', lng[:], ln_g_bc, writes=[cb])
            DMA('sync', 'c6', lnb[:], ln_b_bc, writes=[cb])
            ztile = sb(st, "ztile", [128, D], BF16)
            zt = ztile[:]
            zb = Buf()
            X('vector', lambda e: e.memset(zt, 0.0), writes=[zb])

            zq = list(range(NROW // 128))

            def zero_some(n):
                for _ in range(n):
                    if zq:
                        r = zq.pop(0)
                        DMA('sync', f'z{r % 4}', disp[r * 128:(r + 1) * 128, :], zt, reads=[zb])

            def zero_fill():
                DMA('sync', 'z4', ydisp[DUMP:DUMP + 128, :], zt, reads=[zb])

            units = [('k', 1024 + 512 * i) for i in range(2)] + [('q', 512 * i) for i in range(2)] + \
                    [('v', 2048 + 512 * i) for i in range(2)] + [('u', 3072 + 512 * i) for i in range(2)] + \
                    [('vg', 4096)] + [('ga', 5120 + 512 * i) for i in range(4)] + \
                    [('gs', 7168 + 512 * i) for i in range(4)]
            wslot = [0]
            pend_rope = []
            psrr = [0]
            stgrr = [0]

            def load_w(col0):
                s_ = wslot[0] % 3
                wslot[0] += 1
                DMA('gpsimd', f'w{s_}', wr[s_][:], w_in[:, col0:col0 + 512].rearrange("(k p) c -> p k c", p=128),
                    writes=[wrb[s_]])
                return s_

            for half in range(2 if 'h1' in OPTS else 1):
                T0 = half * 2048
                DMA('sync', 'c7', cosT[:], c_cos[:, T0:T0 + 2048], writes=[cb])
                DMA('sync', 'c8', sinT[:], c_sin[:, T0:T0 + 2048], writes=[cb])
                for i in range(16):
                    t0 = T0 + i * 128
                    xi = i % 2
                    DMA('sync', f'x{xi}', xs[xi][:], x[t0:t0 + 128, :], writes=[xsb[xi]])
                    xn = xn2[xi]
                    xnb = xn2b[xi]
                    X('scalar', lambda e, xi=xi: e.activation(out=junkA[:], in_=xs[xi][:], func=AF.Square,
                                                              accum_out=ssA2[:, xi:xi + 1]),
                      reads=[xsb[xi]], writes=[junkAb, ssA2b[xi]])
                    rstd_from_ss(ssA2[:, xi:xi + 1], ssA2b[xi], None)
                    X('scalar', lambda e, xi=xi, xn=xn: e.activation(out=xn[:], in_=xs[xi][:], func=AF.Copy,
                                                                     scale=ssA2[:, xi:xi + 1]),
                      reads=[xsb[xi], ssA2b[xi]], writes=[xnb])
                    for g8 in range(2):
                        pb_ = 6 + g8
                        pv = ps[pb_][:].bitcast(BF16).rearrange("p (a b) -> p a b", a=8)

                        def tr(e, g8=g8, pv=pv, xn=xn):
                            ins = None
                            for j in range(8):
                                kc = g8 * 8 + j
                                ins = e.transpose(out=pv[:, j, :], in_=xn[:, kc * 128:(kc + 1) * 128],
                                                  identity=ident_b[:])
                            return ins
                        X('tensor', tr, reads=[xnb, b_const], writes=[psb[pb_]])
                        X('vector', lambda e, g8=g8, pv=pv, i=i: e.tensor_tensor(
                            out=hT[:, g8 * 8:(g8 + 1) * 8, i * 128:(i + 1) * 128], in0=pv,
                            in1=gm[:, g8 * 8:(g8 + 1) * 8].unsqueeze(2).to_broadcast([128, 8, 128]),
                            op=ALU.mult), reads=[psb[pb_], cb], writes=[hTb])
                if half == 0:
                    zero_fill()
                for kind, col0 in units:
                    if 'p2' not in OPTS or ('only' in OPTS and kind not in OPTS):
                        continue
                    zero_some(4)
                    if kind == 'vg':
                        sl = [load_w(col0), load_w(col0 + 512)]
                        for i in range(16):
                            t0 = T0 + i * 128
                            pbs = []
                            for hh in range(2):
                                pb_ = psrr[0] % 4
                                psrr[0] += 1
                                pbs.append(pb_)

                                def mm(e, i=i, pb_=pb_, ws=sl[hh]):
                                    ins = None
                                    for kc in range(16):
                                        ins = e.matmul(ps[pb_][:], lhsT=hT[:, kc, i * 128:(i + 1) * 128],
                                                       rhs=wr[ws][:, kc, :], start=(kc == 0), stop=(kc == 15))
                                    return ins
                                X('tensor', mm, reads=[hTb, wrb[sl[hh]]], writes=[psb[pb_]])
                            gi_ = i % 2
                            gv, gvb, bst, mv, mvb = gv2[gi_], gv2b[gi_], bst2[gi_], mv2[gi_], mvb2[gi_]
                            for hh in range(2):
                                gelu_tanh(ps[pbs[hh]][:], psb[pbs[hh]], tmp[hh][:], tmpb[hh],
                                          gv[:, hh * 512:(hh + 1) * 512], gvb)
                            for hh in range(2):
                                X('vector', lambda e, hh=hh, bst=bst, gv=gv: e.bn_stats(out=bst[:, hh, :],
                                                                                        in_=gv[:, hh * 512:(hh + 1) * 512]),
                                  reads=[gvb], writes=[mvb])
                            X('vector', lambda e, bst=bst, mv=mv: e.bn_aggr(out=mv[:], in_=bst[:].rearrange("p a b -> p (a b)")),
                              writes=[mvb])
                            X('vector', lambda e, mv=mv: e.tensor_scalar(out=mv[:, 1:2], in0=mv[:, 1:2], scalar1=EPS,
                                                                         scalar2=None, op0=ALU.add), writes=[mvb])
                            X('scalar', lambda e, mv=mv: e.activation(out=mv[:, 1:2], in_=mv[:, 1:2], func=AF.Sqrt),
                              writes=[mvb])
                            X('vector', lambda e, mv=mv: e.reciprocal(out=mv[:, 1:2], in_=mv[:, 1:2]), writes=[mvb])
                            X('vector', lambda e, mv=mv, gv=gv: e.tensor_scalar(out=gv[:], in0=gv[:], scalar1=mv[:, 0:1],
                                                                                scalar2=mv[:, 1:2], op0=ALU.subtract,
                                                                                op1=ALU.mult), reads=[mvb], writes=[gvb])
                            X('gpsimd', lambda e, gv=gv: e.tensor_tensor(out=gv[:], in0=gv[:], in1=lng[:], op=ALU.mult),
                              reads=[cb], writes=[gvb])
                            vi = i % 2
                            X('gpsimd', lambda e, vi=vi, gv=gv: e.tensor_tensor(out=vst[vi][:], in0=gv[:], in1=lnb[:],
                                                                                op=ALU.add),
                              reads=[gvb, cb], writes=[vstb[vi]])
                            DMA('sync', f'vs{vi}', vgn[t0:t0 + 128, :], vst[vi][:], reads=[vstb[vi]])
                        continue
                    s_ = load_w(col0)
                    if kind == 'v':
                        vc0 = col0 - 2048
                        for i in range(16):
                            t0 = T0 + i * 128
                            pb_ = psrr[0] % 4
                            psrr[0] += 1

                            def mm(e, i=i, pb_=pb_, s_=s_):
                                ins = None
                                for kc in range(16):
                                    ins = e.matmul(ps[pb_][:], lhsT=hT[:, kc, i * 128:(i + 1) * 128],
                                                   rhs=wr[s_][:, kc, :], start=(kc == 0), stop=(kc == 15))
                                return ins
                            X('tensor', mm, reads=[hTb, wrb[s_]], writes=[psb[pb_]])
                            vi = i % 2
                            X('scalar', lambda e, vi=vi, pb_=pb_: e.activation(out=vst[vi][:, 0:512], in_=ps[pb_][:],
                                                                               func=AF.Copy),
                              reads=[psb[pb_]], writes=[vstb[vi]])
                            DMA('sync', f'vs{vi}', Vd[t0:t0 + 128, vc0:vc0 + 512], vst[vi][:, 0:512],
                                reads=[vstb[vi]])
                        continue
                    for tt in range(4):
                        t0 = T0 + tt * 512
                        si = stgrr[0] % 2
                        stgrr[0] += 1
                        for cc in range(4):
                            pb_ = psrr[0] % 4
                            psrr[0] += 1

                            def mm(e, tt=tt, cc=cc, pb_=pb_, s_=s_):
                                ins = None
                                for kc in range(16):
                                    ins = e.matmul(ps[pb_][:], lhsT=wr[s_][:, kc, cc * 128:(cc + 1) * 128],
                                                   rhs=hT[:, kc, tt * 512:(tt + 1) * 512],
                                                   start=(kc == 0), stop=(kc == 15))
                                return ins
                            X('tensor', mm, reads=[hTb, wrb[s_]], writes=[psb[pb_]])
                            dst = stg[si][:, cc, :]
                            if kind in ('q', 'k'):
                                X('scalar', lambda e, dst=dst, pb_=pb_: e.activation(out=dst, in_=ps[pb_][:],
                                                                                     func=AF.Copy),
                                  reads=[psb[pb_]], writes=[stgb[si]])
                                cs = cosT[:, tt * 512:(tt + 1) * 512]
                                sn = sinT[:, tt * 512:(tt + 1) * 512]

                                def rope_tail(dst=dst, cs=cs, sn=sn, pb_=pb_, si=si):
                                    X('tensor', lambda e, dst=dst: e.matmul(ps[4][:], lhsT=rot[:], rhs=dst,
                                                                            start=True, stop=True),
                                      reads=[stgb[si], cb], writes=[psb[4]])
                                    X('vector', lambda e, cs=cs, pb_=pb_: e.tensor_tensor(out=tmp[0][:], in0=cs,
                                                                                          in1=ps[pb_][:], op=ALU.mult),
                                      reads=[psb[pb_], cb, stgb[si]], writes=[tmpb[0]])
                                    X('vector', lambda e, sn=sn: e.tensor_tensor(out=tmp[1][:], in0=sn,
                                                                                 in1=ps[4][:], op=ALU.mult),
                                      reads=[psb[4], cb], writes=[tmpb[1]])
                                    X('vector', lambda e, dst=dst: e.tensor_tensor(out=dst, in0=tmp[0][:],
                                                                                   in1=tmp[1][:], op=ALU.add),
                                      reads=[tmpb[0], tmpb[1]], writes=[stgb[si]])
                                if pend_rope:
                                    pend_rope.pop()()
                                pend_rope.append(rope_tail)
                            elif kind == 'u':
                                ti = cc % 2
                                gelu_tanh(ps[pb_][:], psb[pb_], tmp[ti][:], tmpb[ti], dst, stgb[si])
                            else:
                                X('scalar', lambda e, dst=dst, pb_=pb_: e.activation(out=dst, in_=ps[pb_][:],
                                                                                     func=AF.Sigmoid),
                                  reads=[psb[pb_]], writes=[stgb[si]])
                        if pend_rope:
                            pend_rope.pop()()
                        if kind == 'q':
                            dd, r0_ = qT, col0
                        elif kind == 'k':
                            dd, r0_ = kT, col0 - 1024
                        elif kind == 'u':
                            dd, r0_ = uT, col0 - 3072
                        elif kind == 'ga':
                            dd, r0_ = sga, col0 - 5120
                        else:
                            dd, r0_ = sgs, col0 - 7168
                        DMA('sync', f'st{si}', dd[r0_:r0_ + 512, t0:t0 + 512].rearrange("(c p) t -> p c t", p=128),
                            stg[si][:], reads=[stgb[si]])
            zero_some(1000)

        if upto >= 'A':
            phase(phaseA)

        def phaseB(st):
            qs = [sb(st, f"qs{i}", [128, S], BF16) for i in range(3)]
            ks = [sb(st, f"ks{i}", [128, S], BF16) for i in range(3)]
            vs = [sb(st, f"vs{i}", [128, 32, 128], BF16) for i in range(3)]
            qb_, kb_, vb_ = bufs(3), bufs(3), bufs(3)
            kmf = sb(st, "kmf", [128, 16], F32)
            kmb = sb(st, "kmb", [128, 16], BF16)
            kmB = Buf()
            mneg = sb(st, "mneg", [128, 32, 16], F32)
            valid = sb(st, "valid", [128, 32, 16], F32)
            own = sb(st, "own", [128, 32, 16], F32)
            esel = sb(st, "esel", [128, 16, 128], BF16)
            caus = sb(st, "caus", [128, 4, 512], BF16)
            cb = Buf()
            gmt = sb(st, "gmt", [128, 32, 16], F32)
            gmtb = Buf()
            m8 = sb(st, "m8", [128, 32, 8], F32)
            m8b = Buf()
            alw = sb(st, "alw", [128, 32, 16], F32)
            alwb = Buf()
            Mb2 = [sb(st, f"Mb{i}", [128, 32, 16], BF16) for i in range(2)]
            Mbb = bufs(2)
            MT2 = [sb(st, f"MT{i}", [128, S], BF16) for i in range(2)]
            MT2b = bufs(2)
            for i_ in range(2):
                X('vector', lambda e, i_=i_: e.memset(MT2[i_][:], 0.0), writes=[MT2b[i_]])
            pT = [sb(st, f"pT{i}", [128, 512], BF16) for i in range(4)]
            pTb = bufs(4)
            rec = sb(st, "rec", [128, 512], F32)
            recb = Buf()
            dacc = [sb(st, f"dacc{i}", [128, 512], F32) for i in range(4)]
            daccb = bufs(4)
            ones_f = sb(st, "ones_f", [128, 128], F32)
            DMA('sync', 'c5', ones_f[:], c_ones, writes=[cb])
            ot = [sb(st, f"ot{i}", [128, 512], BF16) for i in range(2)]
            otb = bufs(2)
            DMA('sync', 'c0', mneg[:].rearrange("p a b -> p (a b)"), c_mneg, writes=[cb])
            DMA('sync', 'c1', valid[:].rearrange("p a b -> p (a b)"), c_valid, writes=[cb])
            DMA('sync', 'c2', own[:].rearrange("p a b -> p (a b)"), c_own, writes=[cb])
            DMA('gpsimd', 'c3', esel[:].rearrange("p a b -> p (a b)"), c_esel, writes=[cb])
            DMA('gpsimd', 'c4', caus[:].rearrange("p a b -> p (a b)"), c_caus, writes=[cb])
            scale = DH ** -0.5
            prr = [0]
            orr = [0]

            def load_head(h):
                s_ = h % 3
                DMA('sync', f'q{s_}', qs[s_][:], qT[h * 128:(h + 1) * 128, :], writes=[qb_[s_]])
                DMA('sync', f'k{s_}', ks[s_][:], kT[h * 128:(h + 1) * 128, :], writes=[kb_[s_]])
                DMA('sync', f'v{s_}', vs[s_][:], Vd[:, h * 128:(h + 1) * 128].rearrange("(n p) c -> p n c", p=128),
                    writes=[vb_[s_]])

            def G1(h):
                s_ = h % 3
                q_, k_ = qs[s_], ks[s_]
                mi = h % 2
                X('vector', lambda e, k_=k_: e.tensor_reduce(out=kmf[:], in_=k_[:].rearrange("p (n t) -> p n t", t=256),
                                                             axis=AX.X, op=ALU.add),
                  reads=[kb_[s_]], writes=[kmB])
                X('vector', lambda e: e.tensor_scalar(out=kmb[:], in0=kmf[:], scalar1=1.0 / 256, scalar2=None,
                                                      op0=ALU.mult), writes=[kmB])

                def gmm(e, q_=q_):
                    ins = None
                    for qi in range(32):
                        ins = e.matmul(ps[4][:, qi * 16:(qi + 1) * 16], lhsT=q_[:, qi * 128:(qi + 1) * 128],
                                       rhs=kmb[:], start=True, stop=True)
                    return ins
                X('tensor', gmm, reads=[qb_[s_], kmB], writes=[psb[4]])
                X('vector', lambda e: e.tensor_tensor(out=gmt[:], in0=mneg[:],
                                                      in1=ps[4][:].rearrange("p (a b) -> p a b", b=16), op=ALU.add),
                  reads=[psb[4], cb], writes=[gmtb])
                for qi in range(32):
                    X('vector', lambda e, qi=qi: e.max(out=m8[:, qi, :], in_=gmt[:, qi, :]),
                      reads=[gmtb], writes=[m8b])
                X('vector', lambda e: e.tensor_tensor(out=alw[:], in0=gmt[:],
                                                      in1=m8[:, :, 2:3].to_broadcast([128, 32, 16]), op=ALU.is_ge),
                  reads=[gmtb, m8b], writes=[alwb])
                X('vector', lambda e: e.tensor_tensor(out=alw[:], in0=alw[:], in1=valid[:], op=ALU.mult),
                  reads=[cb], writes=[alwb])
                X('vector', lambda e: e.tensor_tensor(out=alw[:], in0=alw[:], in1=own[:], op=ALU.add),
                  reads=[cb], writes=[alwb])
                X('vector', lambda e, mi=mi: e.tensor_scalar(out=Mb2[mi][:], in0=alw[:], scalar1=-1.0, scalar2=-NEG,
                                                             op0=ALU.add, op1=ALU.mult), reads=[alwb], writes=[Mbb[mi]])

            def G2(h):
                mi = h % 2
                for g4 in range(4):
                    pb_ = 4
                    pv = ps[pb_][0:16, :].bitcast(BF16)

                    def tr(e, g4=g4, pv=pv, mi=mi):
                        ins = None
                        for j in range(8):
                            qi = g4 * 8 + j
                            ins = e.transpose(out=pv[:, j * 128:(j + 1) * 128], in_=Mb2[mi][:, qi, :], identity=ident_b[:])
                        return ins
                    X('tensor', tr, reads=[Mbb[mi], b_const], writes=[psb[pb_]])
                    X('scalar', lambda e, g4=g4, pv=pv, mi=mi: e.activation(out=MT2[mi][0:16, g4 * 1024:(g4 + 1) * 1024],
                                                                            in_=pv, func=AF.Copy),
                      reads=[psb[pb_]], writes=[MT2b[mi]])

            load_head(0)
            load_head(1)
            G1(0)
            G2(0)
            for h in range(NH):
                s_ = h % 3
                if h + 2 < NH:
                    load_head(h + 2)
                q_, k_, v_ = qs[s_], ks[s_], vs[s_]
                MT = MT2[h % 2]
                MTb = MT2b[h % 2]
                for J in range(8):
                    if h + 1 < NH and J == 2:
                        G1(h + 1)
                    if h + 1 < NH and J == 5:
                        G2(h + 1)
                    nkt = 4 * J + 4
                    PO, PD = (2, 3) if J % 2 == 0 else (6, 7)

                    SB = (0, 1, 5)

                    def smm(kt, J=J, k_=k_, q_=q_, MT=MT, MTb=MTb):
                        pb_ = SB[kt % 3]

                        def f(e):
                            e.matmul(ps[pb_][:], lhsT=k_[:, kt * 128:(kt + 1) * 128], rhs=q_[:, J * 512:(J + 1) * 512],
                                     start=True, stop=False)
                            return e.matmul(ps[pb_][:], lhsT=esel[:, kt // 2, :], rhs=MT[:, J * 512:(J + 1) * 512],
                                            start=False, stop=True)
                        X('tensor', f, reads=[kb_[s_], qb_[s_], MTb, cb], writes=[psb[pb_]])
                    smm(0)
                    smm(1)
                    for kt in range(nkt):
                        if kt + 2 < nkt:
                            smm(kt + 2)
                        pi = prr[0] % 4
                        prr[0] += 1
                        pb_ = SB[kt % 3]
                        X('scalar', lambda e, pi=pi, pb_=pb_: e.activation(out=pT[pi][:], in_=ps[pb_][:], func=AF.Exp,
                                                                           scale=scale),
                          reads=[psb[pb_]], writes=[pTb[pi]])
                        r = kt - 4 * J
                        if r >= 0:
                            X('vector', lambda e, pi=pi, r=r: e.tensor_tensor(out=pT[pi][:], in0=pT[pi][:],
                                                                              in1=caus[:, r, :], op=ALU.mult),
                              reads=[cb], writes=[pTb[pi]])

                        if kt % 2 == 1:
                            def pv_(e, kt=kt, pi=pi, v_=v_, nkt=nkt, PO=PO, PD=PD):
                                e.matmul(ps[PO][:], lhsT=v_[:, kt, :], rhs=pT[pi][:], start=(kt == 0), stop=(kt == nkt - 1))
                                return e.matmul(ps[PD][:], lhsT=ones_b[:], rhs=pT[pi][:], start=(kt == 1), stop=False)
                            X('tensor', pv_, reads=[pTb[pi], vb_[s_], b_const], writes=[psb[PO], psb[PD]])
                        else:
                            def pv_(e, kt=kt, pi=pi, v_=v_, nkt=nkt, PO=PO):
                                return e.matmul(ps[PO][:], lhsT=v_[:, kt, :], rhs=pT[pi][:], start=(kt == 0),
                                                stop=(kt == nkt - 1))
                            X('tensor', pv_, reads=[pTb[pi], vb_[s_]], writes=[psb[PO]])
                            ai = J % 2
                            if kt == 0:
                                X('gpsimd', lambda e, pi=pi, ai=ai: e.tensor_copy(out=dacc[ai][:], in_=pT[pi][:]),
                                  reads=[pTb[pi]], writes=[daccb[ai]])
                            else:
                                X('gpsimd', lambda e, pi=pi, ai=ai: e.tensor_tensor(out=dacc[ai][:], in0=dacc[ai][:],
                                                                                    in1=pT[pi][:], op=ALU.add),
                                  reads=[pTb[pi]], writes=[daccb[ai]])
                    a0_ = J % 2
                    X('tensor', lambda e, a0_=a0_, PD=PD: e.matmul(ps[PD][:], lhsT=ones_f[:], rhs=dacc[a0_][:], start=False, stop=True),
                      reads=[daccb[a0_], cb], writes=[psb[PD]])
                    X('vector', lambda e, PD=PD: e.reciprocal(out=rec[:], in_=ps[PD][:]), reads=[psb[PD]], writes=[recb])
                    oi = orr[0] % 2
                    orr[0] += 1
                    X('vector', lambda e, oi=oi, PO=PO: e.tensor_tensor(out=ot[oi][:], in0=rec[:], in1=ps[PO][:], op=ALU.mult),
                      reads=[psb[PO], recb], writes=[otb[oi]])
                    DMA('sync', f'o{oi}', attnT[h * 128:(h + 1) * 128, J * 512:(J + 1) * 512], ot[oi][:],
                        reads=[otb[oi]])

        if upto >= 'B':
            phase(phaseB)

        def phaseC(st):
            wraw = sb(st, "wraw", [128, 8, 128], F32)
            tril = sb(st, "tril", [128, 128], F32)
            wmb = sb(st, "wmb", [128, 8, 128], BF16)
            wT = sb(st, "wT", [128, 8, 128], BF16)
            bbc = sb(st, "bbc", [128, 8, 128], F32)
            cb = Buf()
            wTb = Buf()
            vg_ = [sb(st, f"vg{i}", [128, 1024], BF16) for i in range(2)]
            u_ = [sb(st, f"u{i}", [128, 8, 128], BF16) for i in range(2)]
            vgb, ub = bufs(2), bufs(2)
            tm = sb(st, "tmC", [128, 8, 128], F32)
            tmb = Buf()
            go = [sb(st, f"go{i}", [128, 8, 128], BF16) for i in range(2)]
            gob = bufs(2)
            DMA('sync', 'c0', wraw[:], sgu_w.rearrange("g t s -> t g s"), writes=[cb])
            DMA('sync', 'c1', tril[:], c_tril, writes=[cb])
            DMA('sync', 'c2', bbc[:].rearrange("p a b -> p (a b)"), sgub_bc, writes=[cb])
            X('vector', lambda e: e.tensor_tensor(out=wmb[:], in0=wraw[:],
                                                  in1=tril[:].unsqueeze(1).to_broadcast([128, 8, 128]), op=ALU.mult),
              reads=[cb], writes=[wTb])
            pv = ps[6][:].bitcast(BF16).rearrange("p (a b) -> p a b", a=8)

            def tr(e):
                ins = None
                for g in range(8):
                    ins = e.transpose(out=pv[:, g, :], in_=wmb[:, g, :], identity=ident_b[:])
                return ins
            X('tensor', tr, reads=[wTb, b_const], writes=[psb[6]])
            X('vector', lambda e: e.tensor_copy(out=wT[:], in_=pv), reads=[psb[6]], writes=[wTb])

            def load(i):
                s_ = i % 2
                DMA('sync', f'a{s_}', vg_[s_][:], vgn[i * 128:(i + 1) * 128, :], writes=[vgb[s_]])
                DMA('sync', f'b{s_}', u_[s_][:], uT[:, i * 128:(i + 1) * 128].rearrange("(g c) t -> c g t", c=128),
                    writes=[ub[s_]])
            load(0)
            for i in range(32):
                s_ = i % 2
                if i + 1 < 32:
                    load(i + 1)
                pa, pb2 = (0, 1) if s_ == 0 else (2, 3)

                def mm(e, s_=s_, pa=pa, pb2=pb2):
                    ins = None
                    for g in range(8):
                        bank = pa if g < 4 else pb2
                        ins = e.matmul(ps[bank][:, (g % 4) * 128:(g % 4 + 1) * 128],
                                       lhsT=vg_[s_][:, g * 128:(g + 1) * 128], rhs=wT[:, g, :], start=True, stop=True)
                    return ins
                X('tensor', mm, reads=[vgb[s_], wTb], writes=[psb[pa], psb[pb2]])
                for hh, bank in enumerate((pa, pb2)):
                    X('vector', lambda e, hh=hh, bank=bank: e.tensor_tensor(
                        out=tm[:, hh * 4:(hh + 1) * 4, :], in0=bbc[:, hh * 4:(hh + 1) * 4, :],
                        in1=ps[bank][:].rearrange("p (a b) -> p a b", a=4), op=ALU.add),
                      reads=[psb[bank], cb], writes=[tmb])
                X('vector', lambda e, s_=s_: e.tensor_tensor(out=go[s_][:], in0=tm[:], in1=u_[s_][:], op=ALU.mult),
                  reads=[tmb, ub[s_]], writes=[gob[s_]])
                DMA('sync', f'g{s_}', GTd[:, i * 128:(i + 1) * 128].rearrange("(g c) t -> c g t", c=128), go[s_][:],
                    reads=[gob[s_]])


        def phaseD(st):
            wpa = sb(st, "wpa", [128, 8, D], BF16)
            wpb = sb(st, "wpb", [128, 8, D], BF16)
            wb = Buf()
            at = [sb(st, f"at{i}", [128, 8, 512], BF16) for i in range(2)]
            gt = [sb(st, f"gt{i}", [128, 8, 512], BF16) for i in range(2)]
            atb, gtb = bufs(2), bufs(2)
            ga_ = [sb(st, f"ga{i}", [128, 512], BF16) for i in range(4)]
            gs_ = [sb(st, f"gs{i}", [128, 512], BF16) for i in range(4)]
            gab, gsb = bufs(4), bufs(4)
            m1 = sb(st, "m1", [128, 512], F32)
            m2 = sb(st, "m2", [128, 512], F32)
            m1b, m2b = Buf(), Buf()
            mo = [sb(st, f"mo{i}", [128, 16, 512], BF16) for i in range(2)]
            mob = bufs(2)
            for hh in range(2):
                DMA('gpsimd', f'Dc{hh}', wpa[:, :, hh * 1024:(hh + 1) * 1024],
                    w_pa[:, hh * 1024:(hh + 1) * 1024].rearrange("(k p) c -> p k c", p=128), writes=[wb])
                DMA('gpsimd', f'Dc{2 + hh}', wpb[:, :, hh * 1024:(hh + 1) * 1024],
                    w_pb[:, hh * 1024:(hh + 1) * 1024].rearrange("(k p) c -> p k c", p=128), writes=[wb])

            def load(J):
                s_ = J % 2
                DMA('sync', f'Da{s_}', at[s_][:], attnT[:, J * 512:(J + 1) * 512].rearrange("(k p) t -> p k t", p=128),
                    writes=[atb[s_]])
                DMA('sync', f'Db{s_}', gt[s_][:], GTd[:, J * 512:(J + 1) * 512].rearrange("(k p) t -> p k t", p=128),
                    writes=[gtb[s_]])
            grr = [0]

            def loadg(J, c):
                gi = grr[0] % 4
                grr[0] += 1
                DMA('sync', f'Dga{gi}', ga_[gi][:], sga[c * 128:(c + 1) * 128, J * 512:(J + 1) * 512], writes=[gab[gi]])
                DMA('sync', f'Dgs{gi}', gs_[gi][:], sgs[c * 128:(c + 1) * 128, J * 512:(J + 1) * 512], writes=[gsb[gi]])
                return gi
            load(0)
            pend = [loadg(0, 0), loadg(0, 1)]
            for J in range(8):
                s_ = J % 2
                if J + 1 < 8:
                    load(J + 1)
                for c in range(16):
                    nxt = J * 16 + c + 2
                    gi = pend.pop(0)
                    if nxt < 128:
                        pend.append(loadg(nxt // 16, nxt % 16))
                    pa, pb2 = (0, 1) if c % 2 == 0 else (2, 3)

                    def mm(e, s_=s_, c=c, pa=pa, pb2=pb2):
                        for kc in range(8):
                            e.matmul(ps[pa][:], lhsT=wpa[:, kc, c * 128:(c + 1) * 128], rhs=at[s_][:, kc, :],
                                     start=(kc == 0), stop=(kc == 7))
                        ins = None
                        for kc in range(8):
                            ins = e.matmul(ps[pb2][:], lhsT=wpb[:, kc, c * 128:(c + 1) * 128], rhs=gt[s_][:, kc, :],
                                           start=(kc == 0), stop=(kc == 7))
                        return ins
                    X('tensor', mm, reads=[wb, atb[s_], gtb[s_]], writes=[psb[pa], psb[pb2]])
                    X('vector', lambda e, gi=gi, pa=pa: e.tensor_tensor(out=m1[:], in0=ga_[gi][:], in1=ps[pa][:],
                                                                        op=ALU.mult),
                      reads=[psb[pa], gab[gi]], writes=[m1b])
                    X('vector', lambda e, gi=gi, pb2=pb2: e.tensor_tensor(out=m2[:], in0=gs_[gi][:], in1=ps[pb2][:],
                                                                          op=ALU.mult),
                      reads=[psb[pb2], gsb[gi]], writes=[m2b])
                    X('vector', lambda e, s_=s_, c=c: e.tensor_tensor(out=mo[s_][:, c, :], in0=m1[:], in1=m2[:],
                                                                      op=ALU.add),
                      reads=[m1b, m2b], writes=[mob[s_]])
                DMA('sync', f'Dm{s_}', mTd[:, J * 512:(J + 1) * 512].rearrange("(c p) t -> p c t", p=128), mo[s_][:],
                    reads=[mob[s_]])

        def phaseCD(st):
            phaseC(st)
            phaseD(st)

        if upto >= 'D':
            phase(phaseCD)

        def phaseE(st):
            wo = sb(st, "wo", [128, 16, D], BF16)
            wob = Buf()
            wr_ = sb(st, "wrt", [128, 16, 36], F32)
            g2 = sb(st, "g2", [128, D], F32)
            rb = sb(st, "rb", [128, 36], F32)
            ltri = sb(st, "ltri", [128, 128], BF16)
            ecap = sb(st, "ecap", [128, 32], F32)
            cb = Buf()
            mt = [sb(st, f"mt{i}", [128, 16, 512], BF16) for i in range(2)]
            mtb = bufs(2)
            xs = [sb(st, f"xs{i}", [128, D], F32) for i in range(2)]
            xsb = bufs(2)
            x1s = [sb(st, f"x1s{i}", [128, D], F32) for i in range(2)]
            x1b = bufs(2)
            junk = sb(st, "junkE", [128, D], BF16)
            junkb = Buf()
            h2f = [sb(st, f"h2f{i}", [128, D], F32) for i in range(2)]
            h2fb = bufs(2)
            h2b = [sb(st, f"h2b{i}", [128, D], BF16) for i in range(3)]
            h2bb = bufs(3)
            ss2 = sb(st, "ss2", [128, 2], F32)
            ss2b = bufs(2)
            Lb = bufs(2)
            h2T = sb(st, "h2T", [128, 16, 128], F32)
            h2Tb = Buf()
            ss = sb(st, "ssE", [128, 1], F32)
            ssb = Buf()
            Lall = sb(st, "Lall", [128, 32, 36], F32)
            gmx = sb(st, "gmx", [128, 32], F32)
            Gh = sb(st, "Gh", [128, 32, 4], F32)
            dg = sb(st, "dg", [128, 32, 4], F32)
            pg = sb(st, "pg", [128, 32], F32)
            ed = sb(st, "ed", [128, 32], F32)
            den = sb(st, "den", [128, 32], F32)
            m8 = sb(st, "m8E", [128, 32, 8], F32)
            rf = sb(st, "rf", [128, 32], F32)
            h2db = bufs(32)
            rtb = Buf()
            acb = Buf()
            for q4 in range(4):
                DMA('gpsimd', f'c{q4}', wo[:, :, q4 * 512:(q4 + 1) * 512],
                    w_out[:, q4 * 512:(q4 + 1) * 512].rearrange("(k p) c -> p k c", p=128), writes=[wob])
            DMA('sync', 'c4', wr_[:], w_r.rearrange("(k p) c -> p k c", p=128), writes=[cb])
            DMA('sync', 'c5', g2[:], g2_bc, writes=[cb])
            DMA('sync', 'c6', rb[:], rb_bc, writes=[cb])
            DMA('gpsimd', 'c7', ltri[:], c_ltri, writes=[cb])
            DMA('sync', 'c8', ecap[:], c_ecap, writes=[cb])

            def load(J):
                s_ = J % 2
                DMA('sync', f'a{s_}', mt[s_][:], mTd[:, J * 512:(J + 1) * 512].rearrange("(k p) t -> p k t", p=128),
                    writes=[mtb[s_]])
            load(0)

            def S1(i):
                J, r = divmod(i, 4)
                s_ = J % 2
                t0 = i * 128
                xi = i % 2
                hi = i % 2
                bi = i % 3
                if r == 0 and J + 1 < 8:
                    load(J + 1)
                DMA('sync', f'x{xi}', xs[xi][:], x[t0:t0 + 128, :], writes=[xsb[xi]])
                for dt_ in range(4):
                    def mm(e, s_=s_, r=r, dt_=dt_):
                        ins = None
                        for c in range(16):
                            ins = e.matmul(ps[dt_][:], lhsT=mt[s_][:, c, r * 128:(r + 1) * 128],
                                           rhs=wo[:, c, dt_ * 512:(dt_ + 1) * 512], start=(c == 0), stop=(c == 15))
                        return ins
                    X('tensor', mm, reads=[mtb[s_], wob], writes=[psb[dt_]])
                    X('vector', lambda e, xi=xi, dt_=dt_: e.tensor_tensor(
                        out=x1s[xi][:, dt_ * 512:(dt_ + 1) * 512], in0=xs[xi][:, dt_ * 512:(dt_ + 1) * 512],
                        in1=ps[dt_][:], op=ALU.add),
                      reads=[psb[dt_], xsb[xi]], writes=[x1b[xi]])
                DMA('sync', f'y{xi}', x1d[t0:t0 + 128, :], x1s[xi][:], reads=[x1b[xi]])

            def S1b(i):
                t0 = i * 128
                xi = i % 2
                hi = i % 2
                bi = i % 3
                X('scalar', lambda e, xi=xi, hi=hi: e.activation(out=junk[:], in_=x1s[xi][:], func=AF.Square,
                                                                 accum_out=ss2[:, hi:hi + 1]),
                  reads=[x1b[xi]], writes=[junkb, ss2b[hi]])
                rstd_from_ss(ss2[:, hi:hi + 1], ss2b[hi], None)
                X('scalar', lambda e, xi=xi, hi=hi: e.activation(out=h2f[hi][:], in_=x1s[xi][:], func=AF.Copy,
                                                                 scale=ss2[:, hi:hi + 1]),
                  reads=[x1b[xi], ss2b[hi]], writes=[h2fb[hi]])
                X('gpsimd', lambda e, hi=hi: e.tensor_tensor(out=h2f[hi][:], in0=h2f[hi][:], in1=g2[:], op=ALU.mult),
                  reads=[cb], writes=[h2fb[hi]])
                X('scalar', lambda e, hi=hi, bi=bi: e.activation(out=h2b[bi][:], in_=h2f[hi][:], func=AF.Copy),
                  reads=[h2fb[hi]], writes=[h2bb[bi]])
                DMA('sync', f'hd{bi}', h2d[t0:t0 + 128, :], h2b[bi][:], reads=[h2bb[bi]], writes=[h2db[i]])

            def S2(i):
                hi = i % 2
                li = i % 2
                for g4 in range(4):
                    pb_ = 4 + (g4 % 2)

                    def tr(e, g4=g4, pb_=pb_, hi=hi):
                        ins = None
                        for j in range(4):
                            kc = g4 * 4 + j
                            ins = e.transpose(out=ps[pb_][:, j * 128:(j + 1) * 128],
                                              in_=h2f[hi][:, kc * 128:(kc + 1) * 128], identity=ident_f[:])
                        return ins
                    X('tensor', tr, reads=[h2fb[hi], b_const], writes=[psb[pb_]])
                    X('scalar', lambda e, g4=g4, pb_=pb_: e.activation(
                        out=h2T[:, g4 * 4:(g4 + 1) * 4, :], in_=ps[pb_][:].rearrange("p (a b) -> p a b", a=4),
                        func=AF.Copy), reads=[psb[pb_]], writes=[h2Tb])

                def lmm(e):
                    ins = None
                    for kc in range(16):
                        ins = e.matmul(ps[6][:, 0:36], lhsT=h2T[:, kc, :], rhs=wr_[:, kc, :], start=(kc == 0),
                                       stop=(kc == 15))
                    return ins
                X('tensor', lmm, reads=[h2Tb, cb], writes=[psb[6]])
                X('vector', lambda e, i=i: e.tensor_tensor(out=Lall[:, i, :], in0=rb[:], in1=ps[6][:, 0:36], op=ALU.add),
                  reads=[psb[6], cb], writes=[Lb[li]])

            S1(0)
            S1b(0)
            for i in range(32):
                if i + 1 < 32:
                    S1(i + 1)
                S2(i)
                if i + 1 < 32:
                    S1b(i + 1)
            v3 = lambda ap_: ap_.rearrange("p (t e) -> p t e", e=32)
            Lm = v3(xs[0][:, 0:1024])
            A0 = v3(xs[0][:, 1024:2048])
            A1 = v3(xs[1][:, 0:1024])
            posE = v3(xs[1][:, 1024:2048])
            okm = v3(x1s[0][:, 0:1024])
            t32 = v3(x1s[0][:, 1024:2048])
            Ab = v3(junk[:, 0:1024])
            X('vector', lambda e: e.memset(gmx[:], 0.0), writes=[rtb, xsb[0], xsb[1], x1b[0], x1b[1], junkb])
            V_ = lambda fn, reads=(), writes=(): X('vector', fn, reads=list(reads), writes=[rtb] + list(writes))
            Lg = Lall[:, :, 0:4]
            Le4 = Lall[:, :, 4:36].rearrange("p t (a b) -> p t a b", a=4)
            V_(lambda e: e.tensor_reduce(out=gmx[:], in_=Lg, axis=AX.X, op=ALU.max), reads=Lb)
            V_(lambda e: e.tensor_tensor(out=Gh[:], in0=Lg, in1=gmx[:].unsqueeze(2).to_broadcast([128, 32, 4]),
                                         op=ALU.is_ge))
            V_(lambda e: e.tensor_tensor(out=dg[:], in0=Lg, in1=gmx[:].unsqueeze(2).to_broadcast([128, 32, 4]),
                                         op=ALU.subtract))
            X('scalar', lambda e: e.activation(out=dg[:], in_=dg[:], func=AF.Exp), writes=[rtb])
            V_(lambda e: e.tensor_reduce(out=pg[:], in_=dg[:], axis=AX.X, op=ALU.add))
            V_(lambda e: e.reciprocal(out=pg[:], in_=pg[:]))
            V_(lambda e: e.tensor_scalar(out=Gh[:], in0=Gh[:], scalar1=-1.0, scalar2=1e30, op0=ALU.add, op1=ALU.mult))
            V_(lambda e: e.tensor_tensor(out=Lm.rearrange("p t (a b) -> p t a b", a=4), in0=Le4,
                                         in1=Gh[:].unsqueeze(3).to_broadcast([128, 32, 4, 8]), op=ALU.add))
            for i in range(32):
                V_(lambda e, i=i: e.max(out=m8[:, i, :], in_=Lm[:, i, :]))
            V_(lambda e: e.tensor_tensor(out=A0, in0=Lm, in1=m8[:, :, 0:1].to_broadcast([128, 32, 32]),
                                         op=ALU.is_equal))
            V_(lambda e: e.tensor_tensor(out=A1, in0=Lm, in1=m8[:, :, 1:2].to_broadcast([128, 32, 32]),
                                         op=ALU.is_equal))
            V_(lambda e: e.tensor_tensor(out=ed[:].unsqueeze(2), in0=m8[:, :, 1:2], in1=m8[:, :, 0:1], op=ALU.subtract))
            X('scalar', lambda e: e.activation(out=ed[:], in_=ed[:], func=AF.Exp), writes=[rtb])
            V_(lambda e: e.tensor_scalar(out=den[:], in0=ed[:], scalar1=1.0, scalar2=None, op0=ALU.add))
            V_(lambda e: e.reciprocal(out=den[:], in_=den[:]))
            V_(lambda e: e.tensor_tensor(out=W0[:], in0=pg[:], in1=den[:], op=ALU.mult), writes=[b_rt])
            V_(lambda e: e.tensor_tensor(out=W1[:], in0=W0[:], in1=ed[:], op=ALU.mult), writes=[b_rt])
            V_(lambda e: e.tensor_tensor(out=Ab, in0=A0, in1=A1, op=ALU.add))
            for hf in range(2):
                def pmm(e, hf=hf):
                    ins = None
                    for ii in range(16):
                        i = hf * 16 + ii
                        o_ = ps[6 + hf][:, ii * 32:(ii + 1) * 32]
                        ins = e.matmul(o_, lhsT=ltri[:], rhs=Ab[:, i, :], start=True, stop=(i == 0))
                        for j in range(i):
                            ins = e.matmul(o_, lhsT=ones_b[:], rhs=Ab[:, j, :], start=False, stop=(j == i - 1))
                    return ins
                X('tensor', pmm, reads=[rtb, cb, b_const], writes=[psb[6 + hf]])
                hs = slice(hf * 16, (hf + 1) * 16)
                pv3 = ps[6 + hf][:].rearrange("p (t e) -> p t e", e=32)
                V_(lambda e, hs=hs, pv3=pv3: e.tensor_scalar(out=okm[:, hs, :], in0=pv3, scalar1=float(CAP), scalar2=None,
                                                             op0=ALU.is_lt), reads=[psb[6 + hf]])
                V_(lambda e, hs=hs, pv3=pv3: e.tensor_tensor(out=posE[:, hs, :],
                                                             in0=ecap[:].unsqueeze(1).to_broadcast([128, 16, 32]),
                                                             in1=pv3, op=ALU.add), reads=[psb[6 + hf], cb])
            V_(lambda e: e.scalar_tensor_tensor(out=posE, in0=posE, scalar=-float(DUMP), in1=okm,
                                                op0=ALU.add, op1=ALU.mult))
            for sl_, A_, R_ in ((0, A0, R0), (1, A1, R1)):
                V_(lambda e, A_=A_: e.tensor_tensor(out=t32, in0=A_, in1=posE, op=ALU.mult))
                V_(lambda e: e.tensor_reduce(out=rf[:], in_=t32, axis=AX.X, op=ALU.add))
                V_(lambda e: e.tensor_scalar(out=rf[:], in0=rf[:], scalar1=float(DUMP), scalar2=None, op0=ALU.add))
                V_(lambda e, R_=R_: e.tensor_copy(out=R_[:], in_=rf[:]), writes=[b_rt])
            for i in range(32):
                bi = i % 3
                DMA('sync', f'hb{bi}', h2b[bi][:], h2d[i * 128:(i + 1) * 128, :], reads=[h2db[i]], writes=[h2bb[bi]])
                for sl_, R_ in ((0, R0), (1, R1)):
                    deps = _deps([h2bb[bi], b_rt], [])
                    tok = P.dma('gpsimd', f'sc{bi}{sl_}',
                                lambda e, R_=R_, i=i, bi=bi: e.indirect_dma_start(
                                    out=disp, out_offset=bass.IndirectOffsetOnAxis(ap=R_[:, i:i + 1], axis=0),
                                    in_=h2b[bi][:], in_offset=None), deps)
                    _upd(tok, [h2bb[bi], b_rt], [])
            if debug:
                X('vector', lambda e: e.tensor_copy(out=h2f[0][:, 0:32], in_=R0[:]), reads=[b_rt], writes=[h2fb[0]])
                X('vector', lambda e: e.tensor_copy(out=h2f[0][:, 32:64], in_=R1[:]), reads=[b_rt], writes=[h2fb[0]])
                X('vector', lambda e: e.tensor_copy(out=h2f[0][:, 64:96], in_=W0[:]), reads=[b_rt], writes=[h2fb[0]])
                X('vector', lambda e: e.tensor_copy(out=h2f[0][:, 96:128], in_=W1[:]), reads=[b_rt], writes=[h2fb[0]])
                DMA('sync', 'dbg', rtab, h2f[0][:, 0:128], reads=[h2fb[0]])

        if upto >= 'E':
            phase(phaseE)

        def phaseF(st):
            NR = 8
            ring = [sb(st, f"rg{i}", [128, 4096], BF16) for i in range(NR)]
            ringb = bufs(NR)
            xb = [sb(st, f"xb{i}", [128, NT, D], BF16) for i in range(2)]
            xbb = bufs(2)
            xbT = sb(st, "xbT", [128, 16, CAP], BF16)
            xbTb = Buf()
            hid = sb(st, "hid", [128, 8, CAP], BF16)
            hidb = bufs(8)
            sg = [sb(st, f"sg{i}", [128, CAP], F32) for i in range(2)]
            sgb = bufs(2)
            seq = []
            for e_ in range(NE):
                for j in range(4):
                    seq.append((e_, 'g', j))
                    seq.append((e_, 'u', j))
                for dt_ in range(4):
                    seq.append((e_, 'd', dt_))
            slot_of = {}
            nxt = [0]

            def issue_load():
                if nxt[0] >= len(seq):
                    return
                n = nxt[0]
                nxt[0] += 1
                e_, kind, j = seq[n]
                s_ = n % NR
                slot_of[(e_, kind, j)] = s_
                if kind == 'd':
                    src = wd[e_][:, j * 512:(j + 1) * 512].rearrange("(k p) c -> p k c", p=128)
                    dst = ring[s_][:].rearrange("p (k c) -> p k c", k=8)
                else:
                    wsrc = wg if kind == 'g' else wu
                    src = wsrc[e_][:, j * 256:(j + 1) * 256].rearrange("(k p) c -> p k c", p=128)
                    dst = ring[s_][:].rearrange("p (k c) -> p k c", k=16)
                DMA('gpsimd', f'r{s_}', dst, src, writes=[ringb[s_]])

            def load_x(e_):
                s_ = e_ % 2
                DMA('sync', f'x{s_}', xb[s_][:], disp[e_ * CAP:(e_ + 1) * CAP, :].rearrange("(n p) d -> p n d", p=128),
                    writes=[xbb[s_]])
            load_x(0)
            for _ in range(NR):
                issue_load()
            prr = [0]
            yrr = [0]
            for e_ in range(NE):
                xs_ = e_ % 2
                if e_ + 1 < NE:
                    load_x(e_ + 1)
                for k2 in range(8):
                    pb_ = 6 + (k2 % 2)
                    pv = ps[pb_][:].bitcast(BF16)[:, 0:2 * CAP].rearrange("p (a b) -> p a b", a=2)

                    def tr(e, k2=k2, pv=pv, xs_=xs_):
                        ins = None
                        for a in range(2):
                            kc = k2 * 2 + a
                            for n in range(NT):
                                ins = e.transpose(out=pv[:, a, n * 128:(n + 1) * 128],
                                                  in_=xb[xs_][:, n, kc * 128:(kc + 1) * 128], identity=ident_b[:])
                        return ins
                    X('tensor', tr, reads=[xbb[xs_], b_const], writes=[psb[pb_]])
                    X('vector', lambda e, k2=k2, pv=pv: e.tensor_copy(out=xbT[:, k2 * 2:(k2 + 1) * 2, :], in_=pv),
                      reads=[psb[pb_]], writes=[xbTb])
                for j in range(4):
                    sg_ = slot_of[(e_, 'g', j)]
                    su_ = slot_of[(e_, 'u', j)]
                    wgv = ring[sg_][:].rearrange("p (k c) -> p k c", k=16)
                    wuv = ring[su_][:].rearrange("p (k c) -> p k c", k=16)
                    for a in range(2):
                        fc = j * 2 + a
                        pg = (prr[0] % 2) * 2
                        prr[0] += 1
                        pu = pg + 1

                        def mm(e, a=a, wgv=wgv, wuv=wuv, pg=pg, pu=pu):
                            for kc in range(16):
                                e.matmul(ps[pg][:, 0:CAP], lhsT=wgv[:, kc, a * 128:(a + 1) * 128], rhs=xbT[:, kc, :],
                                         start=(kc == 0), stop=(kc == 15))
                            ins = None
                            for kc in range(16):
                                ins = e.matmul(ps[pu][:, 0:CAP], lhsT=wuv[:, kc, a * 128:(a + 1) * 128], rhs=xbT[:, kc, :],
                                               start=(kc == 0), stop=(kc == 15))
                            return ins
                        X('tensor', mm, reads=[xbTb, ringb[sg_], ringb[su_]], writes=[psb[pg], psb[pu]])
                        si = fc % 2
                        X('scalar', lambda e, si=si, pg=pg: e.activation(out=sg[si][:], in_=ps[pg][:, 0:CAP], func=AF.Silu),
                          reads=[psb[pg]], writes=[sgb[si]])
                        X('vector', lambda e, si=si, pu=pu, fc=fc: e.tensor_tensor(out=hid[:, fc, :], in0=sg[si][:],
                                                                                   in1=ps[pu][:, 0:CAP], op=ALU.mult),
                          reads=[sgb[si], psb[pu]], writes=[hidb[fc]])
                    issue_load()
                    issue_load()
                for dt_ in range(4):
                    sd_ = slot_of[(e_, 'd', dt_)]
                    wdv = ring[sd_][:].rearrange("p (k c) -> p k c", k=8)
                    for n in range(NT):
                        pb_ = 4 + (prr[0] % 2)
                        prr[0] += 1

                        def mm(e, n=n, wdv=wdv, pb_=pb_):
                            ins = None
                            for fc in range(8):
                                ins = e.matmul(ps[pb_][:], lhsT=hid[:, fc, n * 128:(n + 1) * 128], rhs=wdv[:, fc, :],
                                               start=(fc == 0), stop=(fc == 7))
                            return ins
                        X('tensor', mm, reads=hidb + [ringb[sd_]], writes=[psb[pb_]])
                        X('scalar' if (n % 2) else 'vector',
                          (lambda e, pb_=pb_, n=n, dt_=dt_: e.activation(out=yst[n][:, dt_ * 512:(dt_ + 1) * 512],
                                                                         in_=ps[pb_][:], func=AF.Copy)) if (n % 2) else
                          (lambda e, pb_=pb_, n=n, dt_=dt_: e.tensor_copy(out=yst[n][:, dt_ * 512:(dt_ + 1) * 512],
                                                                          in_=ps[pb_][:])),
                          reads=[psb[pb_]], writes=[ystb[n]])
                    issue_load()
                for n in range(NT):
                    r0_ = e_ * CAP + n * 128
                    DMA('sync', f'y{n}', ydisp[r0_:r0_ + 128, :], yst[n][:], reads=[ystb[n]])

        yst = None
        ystb = None

        def phaseF_wrap(st):
            nonlocal yst, ystb
            yst = [sb(st, f"yst{i}", [128, D], BF16) for i in range(NT)]
            ystb = bufs(NT)
            phaseF(st)

        if upto >= 'F':
            phase(phaseF_wrap)

        def phaseG(st):
            gf = sb(st, "gf", [128, D], F32)
            cb = Buf()
            y0 = [sb(st, f"y0{i}", [128, D], BF16) for i in range(3)]
            y1 = [sb(st, f"y1{i}", [128, D], BF16) for i in range(3)]
            x1t = [sb(st, f"x1t{i}", [128, D], F32) for i in range(3)]
            y0b, y1b, x1tb = bufs(3), bufs(3), bufs(3)
            acc2 = [sb(st, f"acc{i}", [128, D], F32) for i in range(2)]
            acc2b = bufs(2)
            ssg = sb(st, "ssg2", [128, 2], F32)
            ssgb = bufs(2)
            t0g = [sb(st, f"t0g{i}", [128, D], F32) for i in range(2)]
            t1g = [sb(st, f"t1g{i}", [128, D], F32) for i in range(2)]
            t0gb, t1gb = bufs(2), bufs(2)
            junk = sb(st, "junkG", [128, D], BF16)
            junkb = Buf()
            ss = sb(st, "ssG", [128, 1], F32)
            ssb = Buf()
            ob = [sb(st, f"ob{i}", [128, D], F32) for i in range(2)]
            obb = bufs(2)
            DMA('sync', 'c0', gf[:], gf_bc, writes=[cb])

            def load(i):
                s_ = i % 3
                for nm, yt, ytb, R_ in (('g0', y0, y0b, R0), ('g1', y1, y1b, R1)):
                    deps = _deps([b_rt], [ytb[s_]])
                    tok = P.dma('gpsimd', f'{nm}{s_}',
                                lambda e, yt=yt, R_=R_, i=i, s_=s_: e.indirect_dma_start(
                                    out=yt[s_][:], out_offset=None, in_=ydisp,
                                    in_offset=bass.IndirectOffsetOnAxis(ap=R_[:, i:i + 1], axis=0)), deps)
                    _upd(tok, [b_rt], [ytb[s_]])
                DMA('sync', f'x{s_}', x1t[s_][:], x1d[i * 128:(i + 1) * 128, :], writes=[x1tb[s_]])
            load(0)
            load(1)
            for i in range(32):
                s_ = i % 2
                l_ = i % 3
                if i + 2 < 32:
                    load(i + 2)
                X('scalar', lambda e, s_=s_, i=i, l_=l_: e.activation(out=t0g[s_][:], in_=y0[l_][:], func=AF.Copy,
                                                               scale=W0[:, i:i + 1]),
                  reads=[y0b[l_], b_rt], writes=[t0gb[s_]])
                X('scalar', lambda e, s_=s_, i=i, l_=l_: e.activation(out=t1g[s_][:], in_=y1[l_][:], func=AF.Copy,
                                                               scale=W1[:, i:i + 1]),
                  reads=[y1b[l_], b_rt], writes=[t1gb[s_]])
                X('vector', lambda e, s_=s_, l_=l_: e.tensor_tensor(out=acc2[s_][:], in0=t0g[s_][:], in1=x1t[l_][:], op=ALU.add),
                  reads=[t0gb[s_], x1tb[l_]], writes=[acc2b[s_]])
                X('vector', lambda e, s_=s_: e.tensor_tensor(out=acc2[s_][:], in0=acc2[s_][:], in1=t1g[s_][:], op=ALU.add),
                  reads=[t1gb[s_]], writes=[acc2b[s_]])
                X('scalar', lambda e, s_=s_: e.activation(out=junk[:], in_=acc2[s_][:], func=AF.Square,
                                                          accum_out=ssg[:, s_:s_ + 1]),
                  reads=[acc2b[s_]], writes=[junkb, ssgb[s_]])
                rstd_from_ss(ssg[:, s_:s_ + 1], ssgb[s_], None)
                X('scalar', lambda e, s_=s_: e.activation(out=acc2[s_][:], in_=acc2[s_][:], func=AF.Copy,
                                                          scale=ssg[:, s_:s_ + 1]),
                  reads=[ssgb[s_]], writes=[acc2b[s_]])
                X('vector', lambda e, s_=s_: e.tensor_tensor(out=ob[s_][:], in0=acc2[s_][:], in1=gf[:], op=ALU.mult),
                  reads=[acc2b[s_], cb], writes=[obb[s_]])
                DMA('sync', f'o{s_}', out[i * 128:(i + 1) * 128, :], ob[s_][:], reads=[obb[s_]])

        if upto >= 'G':
            phase(phaseG)
    return nc


def make_consts():
    c = {}
    c["c_ident"] = np.eye(128, dtype=np.float32)
    tp = np.arange(128)
    c["c_ltri"] = (tp[:, None] < tp[None, :]).astype(np.float32)
    c["c_ones"] = np.ones((128, 128), np.float32)
    half = 16
    inv = (500000.0 ** (-np.arange(half, dtype=np.float32) * 2.0 / 32)).astype(np.float32)
    ang = np.arange(S, dtype=np.float32)[None, :] * inv[:, None]
    c["c_cos"] = np.concatenate([np.cos(ang), np.cos(ang), np.ones((96, S))], 0).astype(np.float32)
    c["c_sin"] = np.concatenate([np.sin(ang), np.sin(ang), np.zeros((96, S))], 0).astype(np.float32)
    R = np.zeros((128, 128), np.float32)
    for j in range(16):
        R[j + 16, j] = -1.0
        R[j, j + 16] = 1.0
    c["c_rot"] = R
    n = np.arange(16)[None, :]
    blk = (np.arange(32) // 2)[:, None]
    mneg = np.where(n >= blk, -1e30, 0.0).astype(np.float32)
    valid = (n < blk).astype(np.float32)
    own = (n == blk).astype(np.float32)
    c["c_mneg"] = np.ascontiguousarray(np.broadcast_to(mneg.reshape(1, 512), (128, 512)))
    c["c_valid"] = np.ascontiguousarray(np.broadcast_to(valid.reshape(1, 512), (128, 512)))
    c["c_own"] = np.ascontiguousarray(np.broadcast_to(own.reshape(1, 512), (128, 512)))
    es = np.zeros((128, 16, 128), np.float32)
    for i in range(16):
        es[i, i, :] = 1.0
    c["c_esel"] = es.reshape(128, 2048)
    kl = np.arange(128)[:, None]
    ii = np.arange(512)[None, :]
    ca = np.ones((128, 4, 512), np.float32)
    ca[:, 0, :] = np.where(ii < 256, (kl <= ii), 1.0)
    ca[:, 1, :] = np.where(ii < 256, (128 + kl <= ii), 1.0)
    ca[:, 2, :] = np.where(ii >= 256, (kl <= ii - 256), 1.0)
    ca[:, 3, :] = np.where(ii >= 256, (128 + kl <= ii - 256), 1.0)
    c["c_caus"] = ca.reshape(128, 2048)
    c["c_tril"] = (tp[None, :] <= tp[:, None]).astype(np.float32)
    c["c_ecap"] = np.ascontiguousarray(np.broadcast_to((np.arange(32, dtype=np.float32) * CAP)[None, :], (128, 32)))
    return c


def make_shared(inp):
    f = lambda a: np.ascontiguousarray(np.asarray(a, dtype=np.float32))
    bc = lambda v, n: np.ascontiguousarray(np.broadcast_to(f(v).reshape(1, n), (128, n)))
    sh = {}
    sh["w_in"] = f(inp["w_in"][0])
    sh["sgu_w"] = f(inp["sgu_w"][0])
    sh["w_pa"] = f(inp["w_proj_attn"][0])
    sh["w_pb"] = f(inp["w_proj_sgu"][0])
    sh["w_out"] = f(inp["w_out"][0])
    sh["w_r"] = np.ascontiguousarray(np.concatenate([f(inp["w_router_group"][0]), f(inp["w_router_expert"][0])], axis=1))
    sh["wg"] = f(inp["w_exp_gate"][0])
    sh["wu"] = f(inp["w_exp_up"][0])
    sh["wd"] = f(inp["w_exp_down"][0])
    sh["gmix"] = np.ascontiguousarray(f(inp["norm_mix_g"][0]).reshape(16, 128).T)
    sh["ln_g_bc"] = bc(inp["sgu_ln_g"][0], 1024)
    sh["ln_b_bc"] = bc(inp["sgu_ln_b"][0], 1024)
    sh["sgub_bc"] = bc(f(inp["sgu_b"][0]).reshape(-1), 1024)
    sh["g2_bc"] = bc(inp["norm_ffn_g"][0], D)
    sh["gf_bc"] = bc(inp["norm_final_g"], D)
    sh["rb_bc"] = bc(np.concatenate([f(inp["b_router_group"][0]), f(inp["b_router_expert"][0])]), 36)
    sh.update(make_consts())
    return sh


_NC_CACHE = {}


def kernel(**inputs):
    xfull = np.asarray(inputs["x"], dtype=np.float32)
    B = xfull.shape[0]
    sh = make_shared(inputs)
    if "nc" not in _NC_CACHE:
        _NC_CACHE["nc"] = build()
    nc = _NC_CACHE["nc"]
    in_maps = []
    for c in range(B):
        m = dict(sh)
        m["x"] = np.ascontiguousarray(xfull[c])
        in_maps.append(m)
    res = run_bass_kernel_spmd(nc, in_maps, core_ids=list(range(B)))
    return np.stack([np.asarray(r["out"]) for r in res.results], axis=0).astype(np.float32)
```

```python
import numpy as np
from contextlib import ExitStack
import concourse.bass as bass
import concourse.mybir as mybir
from concourse.bass_utils import run_bass_kernel_spmd

F32 = mybir.dt.float32
BF16 = mybir.dt.bfloat16
I32 = mybir.dt.int32
AF = mybir.ActivationFunctionType
ALU = mybir.AluOpType
AX = mybir.AxisListType

S = 4096
D = 2048
NH = 8
DH = 128
DIN = 9216
NE = 32
DFF = 1024
CAP = 512
NT = CAP // 128
NROW = NE * CAP + 128
DUMP = NE * CAP
EPS = 1e-6
NEG = -30000.0
ENG = ['tensor', 'vector', 'scalar', 'gpsimd', 'sync']
import os
OPTS = set(os.environ.get('KOPTS', 'zero,p2,h1').split(','))


class Prog:
    def __init__(self, nc, es):
        self.nc = nc
        self.es = es
        self.q = {e: [] for e in ENG}
        self.sems = {}
        self.count = {}
        self.seen = {e: {} for e in ENG}
        for e in ENG:
            self._sem('E_' + e)

    def _sem(self, key):
        if key not in self.sems:
            self.sems[key] = self.es.enter_context(self.nc.semaphore(key))
            self.count[key] = 0
        return self.sems[key]

    def _waits(self, eng, deps):
        waits = {}
        for d in deps:
            if d is None:
                continue
            k, n = d
            if n > self.seen[eng].get(k, 0):
                waits[k] = max(waits.get(k, 0), n)
        for k, n in waits.items():
            self.seen[eng][k] = n
        return list(waits.items())

    def op(self, eng, fn, deps=()):
        w = self._waits(eng, deps)
        key = 'E_' + eng
        self.count[key] += 1
        self.q[eng].append((fn, w, key, 1))
        return (key, self.count[key])

    def dma(self, eng, semkey, fn, deps=()):
        semkey = eng[0] + '_' + semkey
        self._sem(semkey)
        w = self._waits(eng, deps)
        self.count[semkey] += 16
        self.q[eng].append((fn, w, semkey, 16))
        return (semkey, self.count[semkey])

    def barrier(self):
        toks = [(k, c) for k, c in self.count.items() if c > 0]
        for e in ENG:
            w = self._waits(e, toks)
            if w:
                self.q[e].append((None, w, None, 0))

    def emit(self, block):
        sems = self.sems
        q = self.q
        self.q = {e: [] for e in ENG}

        def run(engname):
            def body(e):
                for fn, w, key, inc in q[engname]:
                    for k, n in w:
                        e.wait_ge(sems[k], n)
                    if fn is not None:
                        ins = fn(e)
                        ins.then_inc(sems[key], inc)
            return body
        block.tensor(run('tensor'))
        block.vector(run('vector'))
        block.scalar(run('scalar'))
        block.gpsimd(run('gpsimd'))
        block.sync(run('sync'))


class Buf:
    def __init__(self):
        self.w = None
        self.r = []


def _deps(reads, writes):
    deps = []
    for b in reads:
        deps.append(b.w)
    for b in writes:
        deps.append(b.w)
        deps.extend(b.r)
    return deps


def _upd(tok, reads, writes):
    for b in reads:
        b.r.append(tok)
    for b in writes:
        b.w = tok
        b.r = []


class K:
    def __init__(self, nc, P):
        self.nc = nc
        self.P = P

    def X(self, eng, fn, reads=(), writes=()):
        deps = _deps(reads, writes)
        if eng == 'tensor':
            deps = [d for d in deps if d is not None and d[0] != 'E_tensor']
        tok = self.P.op(eng, fn, deps)
        _upd(tok, reads, writes)
        return tok

    def DMA(self, eng, semkey, out, in_, reads=(), writes=()):
        deps = _deps(reads, writes)
        tok = self.P.dma(eng, semkey, lambda e, out=out, in_=in_: e.dma_start(out=out, in_=in_), deps)
        _upd(tok, reads, writes)
        return tok


def bufs(n):
    return [Buf() for _ in range(n)]


def build(debug=False, upto='G'):
    nc = bass.Bass("TRN2", target_bir_lowering=False)

    def din(name, shape, dt=F32):
        return nc.dram_tensor(name, list(shape), dt, kind="ExternalInput").ap()

    def dscr(name, shape, dt):
        kind = "ExternalOutput" if (debug and name in debug) else "Internal"
        return nc.dram_tensor(name, list(shape), dt, kind=kind).ap()

    x = din("x", [S, D])
    w_in = din("w_in", [D, DIN])
    sgu_w = din("sgu_w", [8, 128, 128])
    w_pa = din("w_pa", [1024, D])
    w_pb = din("w_pb", [1024, D])
    w_out = din("w_out", [D, D])
    w_r = din("w_r", [D, 36])
    wg = din("wg", [NE, D, DFF])
    wu = din("wu", [NE, D, DFF])
    wd = din("wd", [NE, DFF, D])
    gmix = din("gmix", [128, 16])
    ln_g_bc = din("ln_g_bc", [128, 1024])
    ln_b_bc = din("ln_b_bc", [128, 1024])
    sgub_bc = din("sgub_bc", [128, 1024])
    g2_bc = din("g2_bc", [128, D])
    gf_bc = din("gf_bc", [128, D])
    rb_bc = din("rb_bc", [128, 36])
    c_ident = din("c_ident", [128, 128])
    c_ltri = din("c_ltri", [128, 128])
    c_ones = din("c_ones", [128, 128])
    c_cos = din("c_cos", [128, S])
    c_sin = din("c_sin", [128, S])
    c_rot = din("c_rot", [128, 128])
    c_mneg = din("c_mneg", [128, 512])
    c_valid = din("c_valid", [128, 512])
    c_own = din("c_own", [128, 512])
    c_esel = din("c_esel", [128, 16 * 128])
    c_caus = din("c_caus", [128, 4 * 512])
    c_tril = din("c_tril", [128, 128])
    c_ecap = din("c_ecap", [128, 32])

    out = nc.dram_tensor("out", [S, D], F32, kind="ExternalOutput").ap()

    qT = dscr("qT", [NH * 128, S], BF16)
    kT = dscr("kT", [NH * 128, S], BF16)
    Vd = dscr("Vd", [S, 1024], BF16)
    uT = dscr("uT", [1024, S], BF16)
    vgn = dscr("vgn", [S, 1024], BF16)
    sga = dscr("sga", [D, S], BF16)
    sgs = dscr("sgs", [D, S], BF16)
    attnT = dscr("attnT", [1024, S], BF16)
    GTd = dscr("GTd", [1024, S], BF16)
    mTd = dscr("mTd", [D, S], BF16)
    x1d = dscr("x1d", [S, D], F32)
    h2d = dscr("h2d", [S, D], BF16)
    disp = dscr("disp", [NROW, D], BF16)
    ydisp = dscr("ydisp", [NROW, D], BF16)
    rtab = dscr("rtab", [128, 128], F32)

    with ExitStack() as es:
        P = Prog(nc, es)
        k = K(nc, P)
        X, DMA = k.X, k.DMA
        ps = [es.enter_context(nc.psum_tensor(f"ps{i}", [128, 512], F32)) for i in range(8)]
        psb = bufs(8)

        uniq = [0]

        def sb(st, name, shape, dt):
            uniq[0] += 1
            return st.enter_context(nc.sbuf_tensor(f"{name}_{uniq[0]}", list(shape), dt))

        ident_b = sb(es, "ident_b", [128, 128], BF16)
        ident_f = sb(es, "ident_f", [128, 128], F32)
        ones_b = sb(es, "ones_b", [128, 128], BF16)
        eps_t = sb(es, "eps_t", [128, 1], F32)
        R0 = sb(es, "R0", [128, 32], I32)
        R1 = sb(es, "R1", [128, 32], I32)
        W0 = sb(es, "W0", [128, 32], F32)
        W1 = sb(es, "W1", [128, 32], F32)
        b_const = Buf()
        b_rt = Buf()
        rt_t = bufs(32)

        def phase(fn):
            with ExitStack() as st:
                block = st.enter_context(nc.Block())
                fn(st)
                P.barrier()
                P.emit(block)

        def gelu_tanh(src_ps, srcbuf, tmp, tmpbuf, dst, dstbuf, extra_reads=()):
            X('scalar', lambda e: e.activation(out=tmp, in_=src_ps, func=AF.Square),
              reads=[srcbuf], writes=[tmpbuf])
            X('vector', lambda e: e.tensor_scalar(out=tmp, in0=tmp, scalar1=0.044715, scalar2=1.0,
                                                  op0=ALU.mult, op1=ALU.add),
              reads=[], writes=[tmpbuf])
            X('vector', lambda e: e.tensor_tensor(out=tmp, in0=tmp, in1=src_ps, op=ALU.mult),
              reads=[srcbuf], writes=[tmpbuf])
            X('scalar', lambda e: e.activation(out=tmp, in_=tmp, func=AF.Sigmoid, scale=1.5957691216057308),
              reads=[], writes=[tmpbuf])
            return X('vector', lambda e: e.tensor_tensor(out=dst, in0=tmp, in1=src_ps, op=ALU.mult),
                     reads=[srcbuf, tmpbuf] + list(extra_reads), writes=[dstbuf])

        def rstd_from_ss(ss, ssb, tmpb):
            X('scalar', lambda e: e.activation(out=ss, in_=ss, func=AF.Ln, bias=eps_t[:, 0:1], scale=1.0 / D),
              reads=[b_const], writes=[ssb])
            X('scalar', lambda e: e.activation(out=ss, in_=ss, func=AF.Exp, scale=-0.5), writes=[ssb])

        def phaseA(st):
            hT = sb(st, "hT", [128, 16, 2048], BF16)
            cosT = sb(st, "cosT", [128, 2048], F32)
            sinT = sb(st, "sinT", [128, 2048], F32)
            rot = sb(st, "rot", [128, 128], BF16)
            gm = sb(st, "gm", [128, 16], F32)
            lng = sb(st, "lng", [128, 1024], F32)
            lnb = sb(st, "lnb", [128, 1024], F32)
            xs = [sb(st, f"xs{i}", [128, D], F32) for i in range(2)]
            xsb = bufs(2)
            xn2 = [sb(st, f"xn{i}", [128, D], BF16) for i in range(2)]
            xn2b = bufs(2)
            junkA = sb(st, "junkA", [128, D], BF16)
            junkAb = Buf()
            ssA2 = sb(st, "ssA2", [128, 2], F32)
            ssA2b = bufs(2)
            ssb = Buf()
            wr = [sb(st, f"wr{i}", [128, 16, 512], BF16) for i in range(3)]
            wrb = bufs(3)
            stg = [sb(st, f"stg{i}", [128, 4, 512], BF16) for i in range(2)]
            stgb = bufs(2)
            tmp = [sb(st, f"tmpA{i}", [128, 512], F32) for i in range(2)]
            tmpb = bufs(2)
            gv2 = [sb(st, f"gv{i}", [128, 1024], F32) for i in range(2)]
            gv2b = bufs(2)
            bst2 = [sb(st, f"bst{i}", [128, 2, 6], F32) for i in range(2)]
            mv2 = [sb(st, f"mv{i}", [128, 2], F32) for i in range(2)]
            mvb2 = bufs(2)
            vst = [sb(st, f"vst{i}", [128, 1024], BF16) for i in range(2)]
            vstb = bufs(2)
            hTb = Buf()
            cb = Buf()

            DMA('gpsimd', 'c0', ident_b[:], c_ident, writes=[b_const])
            DMA('sync', 'c1', ident_f[:], c_ident, writes=[b_const])
            DMA('gpsimd', 'c2', ones_b[:], c_ones, writes=[b_const])
            X('vector', lambda e: e.memset(eps_t[:], EPS), writes=[b_const])
            DMA('gpsimd', 'c3', rot[:], c_rot, writes=[cb])
            DMA('sync', 'c4', gm[:], gmix, writes=[cb])
            DMA('sync', 'c5', lng[:], ln_g_bc, writes=[cb])
            DMA('sync', 'c6', lnb[:], ln_b_bc, writes=[cb])
            ztile = sb(st, "ztile", [128, D], BF16)
            zt = ztile[:]
            zb = Buf()
            X('vector', lambda e: e.memset(zt, 0.0), writes=[zb])

            zq = list(range(NROW // 128))

            def zero_some(n):
                for _ in range(n):
                    if zq:
                        r = zq.pop(0)
                        DMA('sync', f'z{r % 4}', disp[r * 128:(r + 1) * 128, :], zt, reads=[zb])

            def zero_fill():
                DMA('sync', 'z4', ydisp[DUMP:DUMP + 128, :], zt, reads=[zb])

            units = [('k', 1024 + 512 * i) for i in range(2)] + [('q', 512 * i) for i in range(2)] + \
                    [('v', 2048 + 512 * i) for i in range(2)] + [('u', 3072 + 512 * i) for i in range(2)] + \
                    [('vg', 4096)] + [('ga', 5120 + 512 * i) for i in range(4)] + \
                    [('gs', 7168 + 512 * i) for i in range(4)]
            wslot = [0]
            pend_rope = []
            psrr = [0]
            stgrr = [0]

            def load_w(col0):
                s_ = wslot[0] % 3
                wslot[0] += 1
                DMA('gpsimd', f'w{s_}', wr[s_][:], w_in[:, col0:col0 + 512].rearrange("(k p) c -> p k c", p=128),
                    writes=[wrb[s_]])
                return s_

            for half in range(2 if 'h1' in OPTS else 1):
                T0 = half * 2048
                DMA('sync', 'c7', cosT[:], c_cos[:, T0:T0 + 2048], writes=[cb])
                DMA('sync', 'c8', sinT[:], c_sin[:, T0:T0 + 2048], writes=[cb])
                for i in range(16):
                    t0 = T0 + i * 128
                    xi = i % 2
                    DMA('sync', f'x{xi}', xs[xi][:], x[t0:t0 + 128, :], writes=[xsb[xi]])
                    xn = xn2[xi]
                    xnb = xn2b[xi]
                    X('scalar', lambda e, xi=xi: e.activation(out=junkA[:], in_=xs[xi][:], func=AF.Square,
                                                              accum_out=ssA2[:, xi:xi + 1]),
                      reads=[xsb[xi]], writes=[junkAb, ssA2b[xi]])
                    rstd_from_ss(ssA2[:, xi:xi + 1], ssA2b[xi], None)
                    X('scalar', lambda e, xi=xi, xn=xn: e.activation(out=xn[:], in_=xs[xi][:], func=AF.Copy,
                                                                     scale=ssA2[:, xi:xi + 1]),
                      reads=[xsb[xi], ssA2b[xi]], writes=[xnb])
                    for g8 in range(2):
                        pb_ = 6 + g8
                        pv = ps[pb_][:].bitcast(BF16).rearrange("p (a b) -> p a b", a=8)

                        def tr(e, g8=g8, pv=pv, xn=xn):
                            ins = None
                            for j in range(8):
                                kc = g8 * 8 + j
                                ins = e.transpose(out=pv[:, j, :], in_=xn[:, kc * 128:(kc + 1) * 128],
                                                  identity=ident_b[:])
                            return ins
                        X('tensor', tr, reads=[xnb, b_const], writes=[psb[pb_]])
                        X('vector', lambda e, g8=g8, pv=pv, i=i: e.tensor_tensor(
                            out=hT[:, g8 * 8:(g8 + 1) * 8, i * 128:(i + 1) * 128], in0=pv,
                            in1=gm[:, g8 * 8:(g8 + 1) * 8].unsqueeze(2).to_broadcast([128, 8, 128]),
                            op=ALU.mult), reads=[psb[pb_], cb], writes=[hTb])
                if half == 0:
                    zero_fill()
                for kind, col0 in units:
                    if 'p2' not in OPTS or ('only' in OPTS and kind not in OPTS):
                        continue
                    zero_some(4)
                    if kind == 'vg':
                        sl = [load_w(col0), load_w(col0 + 512)]
                        for i in range(16):
                            t0 = T0 + i * 128
                            pbs = []
                            for hh in range(2):
                                pb_ = (0, 1, 2, 3, 5)[psrr[0] % 5]
                                psrr[0] += 1
                                pbs.append(pb_)

                                def mm(e, i=i, pb_=pb_, ws=sl[hh]):
                                    ins = None
                                    for kc in range(16):
                                        ins = e.matmul(ps[pb_][:], lhsT=hT[:, kc, i * 128:(i + 1) * 128],
                                                       rhs=wr[ws][:, kc, :], start=(kc == 0), stop=(kc == 15))
                                    return ins
                                X('tensor', mm, reads=[hTb, wrb[sl[hh]]], writes=[psb[pb_]])
                            gi_ = i % 2
                            gv, gvb, bst, mv, mvb = gv2[gi_], gv2b[gi_], bst2[gi_], mv2[gi_], mvb2[gi_]
                            for hh in range(2):
                                gelu_tanh(ps[pbs[hh]][:], psb[pbs[hh]], tmp[hh][:], tmpb[hh],
                                          gv[:, hh * 512:(hh + 1) * 512], gvb)
                            for hh in range(2):
                                X('vector', lambda e, hh=hh, bst=bst, gv=gv: e.bn_stats(out=bst[:, hh, :],
                                                                                        in_=gv[:, hh * 512:(hh + 1) * 512]),
                                  reads=[gvb], writes=[mvb])
                            X('vector', lambda e, bst=bst, mv=mv: e.bn_aggr(out=mv[:], in_=bst[:].rearrange("p a b -> p (a b)")),
                              writes=[mvb])
                            X('vector', lambda e, mv=mv: e.tensor_scalar(out=mv[:, 1:2], in0=mv[:, 1:2], scalar1=EPS,
                                                                         scalar2=None, op0=ALU.add), writes=[mvb])
                            X('scalar', lambda e, mv=mv: e.activation(out=mv[:, 1:2], in_=mv[:, 1:2], func=AF.Sqrt),
                              writes=[mvb])
                            X('vector', lambda e, mv=mv: e.reciprocal(out=mv[:, 1:2], in_=mv[:, 1:2]), writes=[mvb])
                            X('vector', lambda e, mv=mv, gv=gv: e.tensor_scalar(out=gv[:], in0=gv[:], scalar1=mv[:, 0:1],
                                                                                scalar2=mv[:, 1:2], op0=ALU.subtract,
                                                                                op1=ALU.mult), reads=[mvb], writes=[gvb])
                            X('gpsimd', lambda e, gv=gv: e.tensor_tensor(out=gv[:], in0=gv[:], in1=lng[:], op=ALU.mult),
                              reads=[cb], writes=[gvb])
                            vi = i % 2
                            X('gpsimd', lambda e, vi=vi, gv=gv: e.tensor_tensor(out=vst[vi][:], in0=gv[:], in1=lnb[:],
                                                                                op=ALU.add),
                              reads=[gvb, cb], writes=[vstb[vi]])
                            DMA('sync', f'vs{vi}', vgn[t0:t0 + 128, :], vst[vi][:], reads=[vstb[vi]])
                        continue
                    s_ = load_w(col0)
                    if kind == 'v':
                        vc0 = col0 - 2048
                        for i in range(16):
                            t0 = T0 + i * 128
                            pb_ = (0, 1, 2, 3, 5)[psrr[0] % 5]
                            psrr[0] += 1

                            def mm(e, i=i, pb_=pb_, s_=s_):
                                ins = None
                                for kc in range(16):
                                    ins = e.matmul(ps[pb_][:], lhsT=hT[:, kc, i * 128:(i + 1) * 128],
                                                   rhs=wr[s_][:, kc, :], start=(kc == 0), stop=(kc == 15))
                                return ins
                            X('tensor', mm, reads=[hTb, wrb[s_]], writes=[psb[pb_]])
                            vi = i % 2
                            X('scalar', lambda e, vi=vi, pb_=pb_: e.activation(out=vst[vi][:, 0:512], in_=ps[pb_][:],
                                                                               func=AF.Copy),
                              reads=[psb[pb_]], writes=[vstb[vi]])
                            DMA('sync', f'vs{vi}', Vd[t0:t0 + 128, vc0:vc0 + 512], vst[vi][:, 0:512],
                                reads=[vstb[vi]])
                        continue
                    for tt in range(4):
                        t0 = T0 + tt * 512
                        si = stgrr[0] % 2
                        stgrr[0] += 1
                        for cc in range(4):
                            pb_ = (0, 1, 2, 3, 5)[psrr[0] % 5]
                            psrr[0] += 1

                            def mm(e, tt=tt, cc=cc, pb_=pb_, s_=s_):
                                ins = None
                                for kc in range(16):
                                    ins = e.matmul(ps[pb_][:], lhsT=wr[s_][:, kc, cc * 128:(cc + 1) * 128],
                                                   rhs=hT[:, kc, tt * 512:(tt + 1) * 512],
                                                   start=(kc == 0), stop=(kc == 15))
                                return ins
                            X('tensor', mm, reads=[hTb, wrb[s_]], writes=[psb[pb_]])
                            dst = stg[si][:, cc, :]
                            if kind in ('q', 'k'):
                                X('scalar', lambda e, dst=dst, pb_=pb_: e.activation(out=dst, in_=ps[pb_][:],
                                                                                     func=AF.Copy),
                                  reads=[psb[pb_]], writes=[stgb[si]])
                                cs = cosT[:, tt * 512:(tt + 1) * 512]
                                sn = sinT[:, tt * 512:(tt + 1) * 512]

                                def rope_tail(dst=dst, cs=cs, sn=sn, pb_=pb_, si=si):
                                    X('tensor', lambda e, dst=dst: e.matmul(ps[4][:], lhsT=rot[:], rhs=dst,
                                                                            start=True, stop=True),
                                      reads=[stgb[si], cb], writes=[psb[4]])
                                    X('vector', lambda e, cs=cs, pb_=pb_: e.tensor_tensor(out=tmp[0][:], in0=cs,
                                                                                          in1=ps[pb_][:], op=ALU.mult),
                                      reads=[psb[pb_], cb, stgb[si]], writes=[tmpb[0]])
                                    X('vector', lambda e, sn=sn: e.tensor_tensor(out=tmp[1][:], in0=sn,
                                                                                 in1=ps[4][:], op=ALU.mult),
                                      reads=[psb[4], cb], writes=[tmpb[1]])
                                    X('vector', lambda e, dst=dst: e.tensor_tensor(out=dst, in0=tmp[0][:],
                                                                                   in1=tmp[1][:], op=ALU.add),
                                      reads=[tmpb[0], tmpb[1]], writes=[stgb[si]])
                                if pend_rope:
                                    pend_rope.pop()()
                                pend_rope.append(rope_tail)
                            elif kind == 'u':
                                ti = cc % 2
                                gelu_tanh(ps[pb_][:], psb[pb_], tmp[ti][:], tmpb[ti], dst, stgb[si])
                            else:
                                X('scalar', lambda e, dst=dst, pb_=pb_: e.activation(out=dst, in_=ps[pb_][:],
                                                                                     func=AF.Sigmoid),
                                  reads=[psb[pb_]], writes=[stgb[si]])
                        if pend_rope:
                            pend_rope.pop()()
                        if kind == 'q':
                            dd, r0_ = qT, col0
                        elif kind == 'k':
                            dd, r0_ = kT, col0 - 1024
                        elif kind == 'u':
                            dd, r0_ = uT, col0 - 3072
                        elif kind == 'ga':
                            dd, r0_ = sga, col0 - 5120
                        else:
                            dd, r0_ = sgs, col0 - 7168
                        DMA('sync', f'st{si}', dd[r0_:r0_ + 512, t0:t0 + 512].rearrange("(c p) t -> p c t", p=128),
                            stg[si][:], reads=[stgb[si]])
            zero_some(1000)

        if upto >= 'A':
            phase(phaseA)

        def phaseB(st):
            qs = [sb(st, f"qs{i}", [128, S], BF16) for i in range(3)]
            ks = [sb(st, f"ks{i}", [128, S], BF16) for i in range(3)]
            vs = [sb(st, f"vs{i}", [128, 32, 128], BF16) for i in range(3)]
            qb_, kb_, vb_ = bufs(3), bufs(3), bufs(3)
            kmf = sb(st, "kmf", [128, 16], F32)
            kmb = sb(st, "kmb", [128, 16], BF16)
            kmB = Buf()
            mneg = sb(st, "mneg", [128, 32, 16], F32)
            valid = sb(st, "valid", [128, 32, 16], F32)
            own = sb(st, "own", [128, 32, 16], F32)
            esel = sb(st, "esel", [128, 16, 128], BF16)
            caus = sb(st, "caus", [128, 4, 512], BF16)
            cb = Buf()
            gmt = sb(st, "gmt", [128, 32, 16], F32)
            gmtb = Buf()
            m8 = sb(st, "m8", [128, 32, 8], F32)
            m8b = Buf()
            alw = sb(st, "alw", [128, 32, 16], F32)
            alwb = Buf()
            Mb2 = [sb(st, f"Mb{i}", [128, 32, 16], BF16) for i in range(2)]
            Mbb = bufs(2)
            MT2 = [sb(st, f"MT{i}", [128, S], BF16) for i in range(2)]
            MT2b = bufs(2)
            for i_ in range(2):
                X('vector', lambda e, i_=i_: e.memset(MT2[i_][:], 0.0), writes=[MT2b[i_]])
            pT = [sb(st, f"pT{i}", [128, 512], BF16) for i in range(4)]
            pTb = bufs(4)
            rec = sb(st, "rec", [128, 512], F32)
            recb = Buf()
            dacc = [sb(st, f"dacc{i}", [128, 512], F32) for i in range(4)]
            daccb = bufs(4)
            ones_f = sb(st, "ones_f", [128, 128], F32)
            DMA('sync', 'c5', ones_f[:], c_ones, writes=[cb])
            ot = [sb(st, f"ot{i}", [128, 512], BF16) for i in range(2)]
            otb = bufs(2)
            DMA('sync', 'c0', mneg[:].rearrange("p a b -> p (a b)"), c_mneg, writes=[cb])
            DMA('sync', 'c1', valid[:].rearrange("p a b -> p (a b)"), c_valid, writes=[cb])
            DMA('sync', 'c2', own[:].rearrange("p a b -> p (a b)"), c_own, writes=[cb])
            DMA('gpsimd', 'c3', esel[:].rearrange("p a b -> p (a b)"), c_esel, writes=[cb])
            DMA('gpsimd', 'c4', caus[:].rearrange("p a b -> p (a b)"), c_caus, writes=[cb])
            scale = DH ** -0.5
            prr = [0]
            orr = [0]

            def load_head(h):
                s_ = h % 3
                DMA('sync', f'q{s_}', qs[s_][:], qT[h * 128:(h + 1) * 128, :], writes=[qb_[s_]])
                DMA('sync', f'k{s_}', ks[s_][:], kT[h * 128:(h + 1) * 128, :], writes=[kb_[s_]])
                DMA('sync', f'v{s_}', vs[s_][:], Vd[:, h * 128:(h + 1) * 128].rearrange("(n p) c -> p n c", p=128),
                    writes=[vb_[s_]])

            def G1(h):
                s_ = h % 3
                q_, k_ = qs[s_], ks[s_]
                mi = h % 2
                X('vector', lambda e, k_=k_: e.tensor_reduce(out=kmf[:], in_=k_[:].rearrange("p (n t) -> p n t", t=256),
                                                             axis=AX.X, op=ALU.add),
                  reads=[kb_[s_]], writes=[kmB])
                X('vector', lambda e: e.tensor_scalar(out=kmb[:], in0=kmf[:], scalar1=1.0 / 256, scalar2=None,
                                                      op0=ALU.mult), writes=[kmB])

                def gmm(e, q_=q_):
                    ins = None
                    for qi in range(32):
                        ins = e.matmul(ps[4][:, qi * 16:(qi + 1) * 16], lhsT=q_[:, qi * 128:(qi + 1) * 128],
                                       rhs=kmb[:], start=True, stop=True)
                    return ins
                X('tensor', gmm, reads=[qb_[s_], kmB], writes=[psb[4]])
                X('vector', lambda e: e.tensor_tensor(out=gmt[:], in0=mneg[:],
                                                      in1=ps[4][:].rearrange("p (a b) -> p a b", b=16), op=ALU.add),
                  reads=[psb[4], cb], writes=[gmtb])
                for qi in range(32):
                    X('vector', lambda e, qi=qi: e.max(out=m8[:, qi, :], in_=gmt[:, qi, :]),
                      reads=[gmtb], writes=[m8b])
                X('vector', lambda e: e.tensor_tensor(out=alw[:], in0=gmt[:],
                                                      in1=m8[:, :, 2:3].to_broadcast([128, 32, 16]), op=ALU.is_ge),
                  reads=[gmtb, m8b], writes=[alwb])
                X('vector', lambda e: e.tensor_tensor(out=alw[:], in0=alw[:], in1=valid[:], op=ALU.mult),
                  reads=[cb], writes=[alwb])
                X('vector', lambda e: e.tensor_tensor(out=alw[:], in0=alw[:], in1=own[:], op=ALU.add),
                  reads=[cb], writes=[alwb])
                X('vector', lambda e, mi=mi: e.tensor_scalar(out=Mb2[mi][:], in0=alw[:], scalar1=-1.0, scalar2=-NEG,
                                                             op0=ALU.add, op1=ALU.mult), reads=[alwb], writes=[Mbb[mi]])

            def G2(h):
                mi = h % 2
                for g4 in range(4):
                    pb_ = 4
                    pv = ps[pb_][0:16, :].bitcast(BF16)

                    def tr(e, g4=g4, pv=pv, mi=mi):
                        ins = None
                        for j in range(8):
                            qi = g4 * 8 + j
                            ins = e.transpose(out=pv[:, j * 128:(j + 1) * 128], in_=Mb2[mi][:, qi, :], identity=ident_b[:])
                        return ins
                    X('tensor', tr, reads=[Mbb[mi], b_const], writes=[psb[pb_]])
                    X('scalar', lambda e, g4=g4, pv=pv, mi=mi: e.activation(out=MT2[mi][0:16, g4 * 1024:(g4 + 1) * 1024],
                                                                            in_=pv, func=AF.Copy),
                      reads=[psb[pb_]], writes=[MT2b[mi]])

            load_head(0)
            load_head(1)
            G1(0)
            G2(0)
            for h in range(NH):
                s_ = h % 3
                if h + 2 < NH:
                    load_head(h + 2)
                q_, k_, v_ = qs[s_], ks[s_], vs[s_]
                MT = MT2[h % 2]
                MTb = MT2b[h % 2]
                for J in range(8):
                    if h + 1 < NH and J == 2:
                        G1(h + 1)
                    if h + 1 < NH and J == 5:
                        G2(h + 1)
                    nkt = 4 * J + 4
                    PO, PD = (2, 3) if J % 2 == 0 else (6, 7)

                    SB = (0, 1, 5)

                    def smm(kt, J=J, k_=k_, q_=q_, MT=MT, MTb=MTb):
                        pb_ = SB[kt % 3]

                        def f(e):
                            e.matmul(ps[pb_][:], lhsT=k_[:, kt * 128:(kt + 1) * 128], rhs=q_[:, J * 512:(J + 1) * 512],
                                     start=True, stop=False)
                            return e.matmul(ps[pb_][:], lhsT=esel[:, kt // 2, :], rhs=MT[:, J * 512:(J + 1) * 512],
                                            start=False, stop=True)
                        X('tensor', f, reads=[kb_[s_], qb_[s_], MTb, cb], writes=[psb[pb_]])
                    smm(0)
                    smm(1)
                    for kt in range(nkt):
                        if kt + 2 < nkt:
                            smm(kt + 2)
                        pi = prr[0] % 4
                        prr[0] += 1
                        pb_ = SB[kt % 3]
                        X('scalar', lambda e, pi=pi, pb_=pb_: e.activation(out=pT[pi][:], in_=ps[pb_][:], func=AF.Exp,
                                                                           scale=scale),
                          reads=[psb[pb_]], writes=[pTb[pi]])
                        r = kt - 4 * J
                        if r >= 0:
                            X('vector', lambda e, pi=pi, r=r: e.tensor_tensor(out=pT[pi][:], in0=pT[pi][:],
                                                                              in1=caus[:, r, :], op=ALU.mult),
                              reads=[cb], writes=[pTb[pi]])

                        if kt % 2 == 1:
                            def pv_(e, kt=kt, pi=pi, v_=v_, nkt=nkt, PO=PO, PD=PD):
                                e.matmul(ps[PO][:], lhsT=v_[:, kt, :], rhs=pT[pi][:], start=(kt == 0), stop=(kt == nkt - 1))
                                return e.matmul(ps[PD][:], lhsT=ones_b[:], rhs=pT[pi][:], start=(kt == 1), stop=False)
                            X('tensor', pv_, reads=[pTb[pi], vb_[s_], b_const], writes=[psb[PO], psb[PD]])
                        else:
                            def pv_(e, kt=kt, pi=pi, v_=v_, nkt=nkt, PO=PO):
                                return e.matmul(ps[PO][:], lhsT=v_[:, kt, :], rhs=pT[pi][:], start=(kt == 0),
                                                stop=(kt == nkt - 1))
                            X('tensor', pv_, reads=[pTb[pi], vb_[s_]], writes=[psb[PO]])
                            ai = J % 2
                            if kt == 0:
                                X('gpsimd', lambda e, pi=pi, ai=ai: e.tensor_copy(out=dacc[ai][:], in_=pT[pi][:]),
                                  reads=[pTb[pi]], writes=[daccb[ai]])
                            else:
                                X('gpsimd', lambda e, pi=pi, ai=ai: e.tensor_tensor(out=dacc[ai][:], in0=dacc[ai][:],
                                                                                    in1=pT[pi][:], op=ALU.add),
                                  reads=[pTb[pi]], writes=[daccb[ai]])
                    a0_ = J % 2
                    X('tensor', lambda e, a0_=a0_, PD=PD: e.matmul(ps[PD][:], lhsT=ones_f[:], rhs=dacc[a0_][:], start=False, stop=True),
                      reads=[daccb[a0_], cb], writes=[psb[PD]])
                    X('vector', lambda e, PD=PD: e.reciprocal(out=rec[:], in_=ps[PD][:]), reads=[psb[PD]], writes=[recb])
                    oi = orr[0] % 2
                    orr[0] += 1
                    X('vector', lambda e, oi=oi, PO=PO: e.tensor_tensor(out=ot[oi][:], in0=rec[:], in1=ps[PO][:], op=ALU.mult),
                      reads=[psb[PO], recb], writes=[otb[oi]])
                    DMA('sync', f'o{oi}', attnT[h * 128:(h + 1) * 128, J * 512:(J + 1) * 512], ot[oi][:],
                        reads=[otb[oi]])

        if upto >= 'B':
            phase(phaseB)

        def phaseC(st):
            wraw = sb(st, "wraw", [128, 8, 128], F32)
            tril = sb(st, "tril", [128, 128], F32)
            wmb = sb(st, "wmb", [128, 8, 128], BF16)
            wT = sb(st, "wT", [128, 8, 128], BF16)
            bbc = sb(st, "bbc", [128, 8, 128], F32)
            cb = Buf()
            wTb = Buf()
            vg_ = [sb(st, f"vg{i}", [128, 1024], BF16) for i in range(2)]
            u_ = [sb(st, f"u{i}", [128, 8, 128], BF16) for i in range(2)]
            vgb, ub = bufs(2), bufs(2)
            tm = sb(st, "tmC", [128, 8, 128], F32)
            tmb = Buf()
            go = [sb(st, f"go{i}", [128, 8, 128], BF16) for i in range(2)]
            gob = bufs(2)
            DMA('sync', 'c0', wraw[:], sgu_w.rearrange("g t s -> t g s"), writes=[cb])
            DMA('sync', 'c1', tril[:], c_tril, writes=[cb])
            DMA('sync', 'c2', bbc[:].rearrange("p a b -> p (a b)"), sgub_bc, writes=[cb])
            X('vector', lambda e: e.tensor_tensor(out=wmb[:], in0=wraw[:],
                                                  in1=tril[:].unsqueeze(1).to_broadcast([128, 8, 128]), op=ALU.mult),
              reads=[cb], writes=[wTb])
            pv = ps[6][:].bitcast(BF16).rearrange("p (a b) -> p a b", a=8)

            def tr(e):
                ins = None
                for g in range(8):
                    ins = e.transpose(out=pv[:, g, :], in_=wmb[:, g, :], identity=ident_b[:])
                return ins
            X('tensor', tr, reads=[wTb, b_const], writes=[psb[6]])
            X('vector', lambda e: e.tensor_copy(out=wT[:], in_=pv), reads=[psb[6]], writes=[wTb])

            def load(i):
                s_ = i % 2
                DMA('sync', f'a{s_}', vg_[s_][:], vgn[i * 128:(i + 1) * 128, :], writes=[vgb[s_]])
                DMA('sync', f'b{s_}', u_[s_][:], uT[:, i * 128:(i + 1) * 128].rearrange("(g c) t -> c g t", c=128),
                    writes=[ub[s_]])
            load(0)
            for i in range(32):
                s_ = i % 2
                if i + 1 < 32:
                    load(i + 1)
                pa, pb2 = (0, 1) if s_ == 0 else (2, 3)

                def mm(e, s_=s_, pa=pa, pb2=pb2):
                    ins = None
                    for g in range(8):
                        bank = pa if g < 4 else pb2
                        ins = e.matmul(ps[bank][:, (g % 4) * 128:(g % 4 + 1) * 128],
                                       lhsT=vg_[s_][:, g * 128:(g + 1) * 128], rhs=wT[:, g, :], start=True, stop=True)
                    return ins
                X('tensor', mm, reads=[vgb[s_], wTb], writes=[psb[pa], psb[pb2]])
                for hh, bank in enumerate((pa, pb2)):
                    X('vector', lambda e, hh=hh, bank=bank: e.tensor_tensor(
                        out=tm[:, hh * 4:(hh + 1) * 4, :], in0=bbc[:, hh * 4:(hh + 1) * 4, :],
                        in1=ps[bank][:].rearrange("p (a b) -> p a b", a=4), op=ALU.add),
                      reads=[psb[bank], cb], writes=[tmb])
                X('vector', lambda e, s_=s_: e.tensor_tensor(out=go[s_][:], in0=tm[:], in1=u_[s_][:], op=ALU.mult),
                  reads=[tmb, ub[s_]], writes=[gob[s_]])
                DMA('sync', f'g{s_}', GTd[:, i * 128:(i + 1) * 128].rearrange("(g c) t -> c g t", c=128), go[s_][:],
                    reads=[gob[s_]])


        def phaseD(st):
            wpa = sb(st, "wpa", [128, 8, D], BF16)
            wpb = sb(st, "wpb", [128, 8, D], BF16)
            wb = Buf()
            at = [sb(st, f"at{i}", [128, 8, 512], BF16) for i in range(2)]
            gt = [sb(st, f"gt{i}", [128, 8, 512], BF16) for i in range(2)]
            atb, gtb = bufs(2), bufs(2)
            ga_ = [sb(st, f"ga{i}", [128, 512], BF16) for i in range(4)]
            gs_ = [sb(st, f"gs{i}", [128, 512], BF16) for i in range(4)]
            gab, gsb = bufs(4), bufs(4)
            m1 = sb(st, "m1", [128, 512], F32)
            m2 = sb(st, "m2", [128, 512], F32)
            m1b, m2b = Buf(), Buf()
            mo = [sb(st, f"mo{i}", [128, 16, 512], BF16) for i in range(2)]
            mob = bufs(2)
            for hh in range(2):
                DMA('gpsimd', f'Dc{hh}', wpa[:, :, hh * 1024:(hh + 1) * 1024],
                    w_pa[:, hh * 1024:(hh + 1) * 1024].rearrange("(k p) c -> p k c", p=128), writes=[wb])
                DMA('gpsimd', f'Dc{2 + hh}', wpb[:, :, hh * 1024:(hh + 1) * 1024],
                    w_pb[:, hh * 1024:(hh + 1) * 1024].rearrange("(k p) c -> p k c", p=128), writes=[wb])

            def load(J):
                s_ = J % 2
                DMA('sync', f'Da{s_}', at[s_][:], attnT[:, J * 512:(J + 1) * 512].rearrange("(k p) t -> p k t", p=128),
                    writes=[atb[s_]])
                DMA('sync', f'Db{s_}', gt[s_][:], GTd[:, J * 512:(J + 1) * 512].rearrange("(k p) t -> p k t", p=128),
                    writes=[gtb[s_]])
            grr = [0]

            def loadg(J, c):
                gi = grr[0] % 4
                grr[0] += 1
                DMA('sync', f'Dga{gi}', ga_[gi][:], sga[c * 128:(c + 1) * 128, J * 512:(J + 1) * 512], writes=[gab[gi]])
                DMA('sync', f'Dgs{gi}', gs_[gi][:], sgs[c * 128:(c + 1) * 128, J * 512:(J + 1) * 512], writes=[gsb[gi]])
                return gi
            load(0)
            pend = [loadg(0, 0), loadg(0, 1)]
            for J in range(8):
                s_ = J % 2
                if J + 1 < 8:
                    load(J + 1)
                for c in range(16):
                    nxt = J * 16 + c + 2
                    gi = pend.pop(0)
                    if nxt < 128:
                        pend.append(loadg(nxt // 16, nxt % 16))
                    pa, pb2 = (0, 1) if c % 2 == 0 else (2, 3)

                    def mm(e, s_=s_, c=c, pa=pa, pb2=pb2):
                        for kc in range(8):
                            e.matmul(ps[pa][:], lhsT=wpa[:, kc, c * 128:(c + 1) * 128], rhs=at[s_][:, kc, :],
                                     start=(kc == 0), stop=(kc == 7))
                        ins = None
                        for kc in range(8):
                            ins = e.matmul(ps[pb2][:], lhsT=wpb[:, kc, c * 128:(c + 1) * 128], rhs=gt[s_][:, kc, :],
                                           start=(kc == 0), stop=(kc == 7))
                        return ins
                    X('tensor', mm, reads=[wb, atb[s_], gtb[s_]], writes=[psb[pa], psb[pb2]])
                    X('vector', lambda e, gi=gi, pa=pa: e.tensor_tensor(out=m1[:], in0=ga_[gi][:], in1=ps[pa][:],
                                                                        op=ALU.mult),
                      reads=[psb[pa], gab[gi]], writes=[m1b])
                    X('vector', lambda e, gi=gi, pb2=pb2: e.tensor_tensor(out=m2[:], in0=gs_[gi][:], in1=ps[pb2][:],
                                                                          op=ALU.mult),
                      reads=[psb[pb2], gsb[gi]], writes=[m2b])
                    X('vector', lambda e, s_=s_, c=c: e.tensor_tensor(out=mo[s_][:, c, :], in0=m1[:], in1=m2[:],
                                                                      op=ALU.add),
                      reads=[m1b, m2b], writes=[mob[s_]])
                DMA('sync', f'Dm{s_}', mTd[:, J * 512:(J + 1) * 512].rearrange("(c p) t -> p c t", p=128), mo[s_][:],
                    reads=[mob[s_]])

        def phaseCD(st):
            phaseC(st)
            phaseD(st)

        if upto >= 'D':
            phase(phaseCD)

        def phaseE(st):
            wo = sb(st, "wo", [128, 16, D], BF16)
            wob = Buf()
            wr_ = sb(st, "wrt", [128, 16, 36], F32)
            g2 = sb(st, "g2", [128, D], F32)
            rb = sb(st, "rb", [128, 36], F32)
            ltri = sb(st, "ltri", [128, 128], BF16)
            ecap = sb(st, "ecap", [128, 32], F32)
            cb = Buf()
            mt = [sb(st, f"mt{i}", [128, 16, 512], BF16) for i in range(2)]
            mtb = bufs(2)
            xs = [sb(st, f"xs{i}", [128, D], F32) for i in range(2)]
            xsb = bufs(2)
            x1s = [sb(st, f"x1s{i}", [128, D], F32) for i in range(2)]
            x1b = bufs(2)
            junk = sb(st, "junkE", [128, D], BF16)
            junkb = Buf()
            h2f = [sb(st, f"h2f{i}", [128, D], F32) for i in range(2)]
            h2fb = bufs(2)
            h2b = [sb(st, f"h2b{i}", [128, D], BF16) for i in range(3)]
            h2bb = bufs(3)
            ss2 = sb(st, "ss2", [128, 2], F32)
            ss2b = bufs(2)
            Lb = bufs(2)
            h2T = sb(st, "h2T", [128, 16, 128], F32)
            h2Tb = Buf()
            ss = sb(st, "ssE", [128, 1], F32)
            ssb = Buf()
            Lall = sb(st, "Lall", [128, 32, 36], F32)
            gmx = sb(st, "gmx", [128, 32], F32)
            Gh = sb(st, "Gh", [128, 32, 4], F32)
            dg = sb(st, "dg", [128, 32, 4], F32)
            pg = sb(st, "pg", [128, 32], F32)
            ed = sb(st, "ed", [128, 32], F32)
            den = sb(st, "den", [128, 32], F32)
            m8 = sb(st, "m8E", [128, 32, 8], F32)
            rf = sb(st, "rf", [128, 32], F32)
            h2db = bufs(32)
            rtb = Buf()
            acb = Buf()
            for q4 in range(4):
                DMA('gpsimd', f'c{q4}', wo[:, :, q4 * 512:(q4 + 1) * 512],
                    w_out[:, q4 * 512:(q4 + 1) * 512].rearrange("(k p) c -> p k c", p=128), writes=[wob])
            DMA('sync', 'c4', wr_[:], w_r.rearrange("(k p) c -> p k c", p=128), writes=[cb])
            DMA('sync', 'c5', g2[:], g2_bc, writes=[cb])
            DMA('sync', 'c6', rb[:], rb_bc, writes=[cb])
            DMA('gpsimd', 'c7', ltri[:], c_ltri, writes=[cb])
            DMA('sync', 'c8', ecap[:], c_ecap, writes=[cb])

            def load(J):
                s_ = J % 2
                DMA('sync', f'a{s_}', mt[s_][:], mTd[:, J * 512:(J + 1) * 512].rearrange("(k p) t -> p k t", p=128),
                    writes=[mtb[s_]])
            load(0)

            def S1(i):
                J, r = divmod(i, 4)
                s_ = J % 2
                t0 = i * 128
                xi = i % 2
                hi = i % 2
                bi = i % 3
                if r == 0 and J + 1 < 8:
                    load(J + 1)
                DMA('sync', f'x{xi}', xs[xi][:], x[t0:t0 + 128, :], writes=[xsb[xi]])
                for dt_ in range(4):
                    def mm(e, s_=s_, r=r, dt_=dt_):
                        ins = None
                        for c in range(16):
                            ins = e.matmul(ps[dt_][:], lhsT=mt[s_][:, c, r * 128:(r + 1) * 128],
                                           rhs=wo[:, c, dt_ * 512:(dt_ + 1) * 512], start=(c == 0), stop=(c == 15))
                        return ins
                    X('tensor', mm, reads=[mtb[s_], wob], writes=[psb[dt_]])
                    X('vector', lambda e, xi=xi, dt_=dt_: e.tensor_tensor(
                        out=x1s[xi][:, dt_ * 512:(dt_ + 1) * 512], in0=xs[xi][:, dt_ * 512:(dt_ + 1) * 512],
                        in1=ps[dt_][:], op=ALU.add),
                      reads=[psb[dt_], xsb[xi]], writes=[x1b[xi]])
                DMA('sync', f'y{xi}', x1d[t0:t0 + 128, :], x1s[xi][:], reads=[x1b[xi]])

            def S1b(i):
                t0 = i * 128
                xi = i % 2
                hi = i % 2
                bi = i % 3
                X('scalar', lambda e, xi=xi, hi=hi: e.activation(out=junk[:], in_=x1s[xi][:], func=AF.Square,
                                                                 accum_out=ss2[:, hi:hi + 1]),
                  reads=[x1b[xi]], writes=[junkb, ss2b[hi]])
                rstd_from_ss(ss2[:, hi:hi + 1], ss2b[hi], None)
                X('scalar', lambda e, xi=xi, hi=hi: e.activation(out=h2f[hi][:], in_=x1s[xi][:], func=AF.Copy,
                                                                 scale=ss2[:, hi:hi + 1]),
                  reads=[x1b[xi], ss2b[hi]], writes=[h2fb[hi]])
                X('gpsimd', lambda e, hi=hi: e.tensor_tensor(out=h2f[hi][:], in0=h2f[hi][:], in1=g2[:], op=ALU.mult),
                  reads=[cb], writes=[h2fb[hi]])
                X('scalar', lambda e, hi=hi, bi=bi: e.activation(out=h2b[bi][:], in_=h2f[hi][:], func=AF.Copy),
                  reads=[h2fb[hi]], writes=[h2bb[bi]])
                DMA('sync', f'hd{bi}', h2d[t0:t0 + 128, :], h2b[bi][:], reads=[h2bb[bi]], writes=[h2db[i]])

            def S2(i):
                hi = i % 2
                li = i % 2
                for g4 in range(4):
                    pb_ = 4 + (g4 % 2)

                    def tr(e, g4=g4, pb_=pb_, hi=hi):
                        ins = None
                        for j in range(4):
                            kc = g4 * 4 + j
                            ins = e.transpose(out=ps[pb_][:, j * 128:(j + 1) * 128],
                                              in_=h2f[hi][:, kc * 128:(kc + 1) * 128], identity=ident_f[:])
                        return ins
                    X('tensor', tr, reads=[h2fb[hi], b_const], writes=[psb[pb_]])
                    X('scalar', lambda e, g4=g4, pb_=pb_: e.activation(
                        out=h2T[:, g4 * 4:(g4 + 1) * 4, :], in_=ps[pb_][:].rearrange("p (a b) -> p a b", a=4),
                        func=AF.Copy), reads=[psb[pb_]], writes=[h2Tb])

                def lmm(e):
                    ins = None
                    for kc in range(16):
                        ins = e.matmul(ps[6][:, 0:36], lhsT=h2T[:, kc, :], rhs=wr_[:, kc, :], start=(kc == 0),
                                       stop=(kc == 15))
                    return ins
                X('tensor', lmm, reads=[h2Tb, cb], writes=[psb[6]])
                X('vector', lambda e, i=i: e.tensor_tensor(out=Lall[:, i, :], in0=rb[:], in1=ps[6][:, 0:36], op=ALU.add),
                  reads=[psb[6], cb], writes=[Lb[li]])

            S1(0)
            S1b(0)
            for i in range(32):
                if i + 1 < 32:
                    S1(i + 1)
                S2(i)
                if i + 1 < 32:
                    S1b(i + 1)
            v3 = lambda ap_: ap_.rearrange("p (t e) -> p t e", e=32)
            Lm = v3(xs[0][:, 0:1024])
            A0 = v3(xs[0][:, 1024:2048])
            A1 = v3(xs[1][:, 0:1024])
            posE = v3(xs[1][:, 1024:2048])
            okm = v3(x1s[0][:, 0:1024])
            t32 = v3(x1s[0][:, 1024:2048])
            Ab = v3(junk[:, 0:1024])
            X('vector', lambda e: e.memset(gmx[:], 0.0), writes=[rtb, xsb[0], xsb[1], x1b[0], x1b[1], junkb])
            V_ = lambda fn, reads=(), writes=(): X('vector', fn, reads=list(reads), writes=[rtb] + list(writes))
            Lg = Lall[:, :, 0:4]
            Le4 = Lall[:, :, 4:36].rearrange("p t (a b) -> p t a b", a=4)
            V_(lambda e: e.tensor_reduce(out=gmx[:], in_=Lg, axis=AX.X, op=ALU.max), reads=Lb)
            V_(lambda e: e.tensor_tensor(out=Gh[:], in0=Lg, in1=gmx[:].unsqueeze(2).to_broadcast([128, 32, 4]),
                                         op=ALU.is_ge))
            V_(lambda e: e.tensor_tensor(out=dg[:], in0=Lg, in1=gmx[:].unsqueeze(2).to_broadcast([128, 32, 4]),
                                         op=ALU.subtract))
            X('scalar', lambda e: e.activation(out=dg[:], in_=dg[:], func=AF.Exp), writes=[rtb])
            V_(lambda e: e.tensor_reduce(out=pg[:], in_=dg[:], axis=AX.X, op=ALU.add))
            V_(lambda e: e.reciprocal(out=pg[:], in_=pg[:]))
            V_(lambda e: e.tensor_scalar(out=Gh[:], in0=Gh[:], scalar1=-1.0, scalar2=1e30, op0=ALU.add, op1=ALU.mult))
            V_(lambda e: e.tensor_tensor(out=Lm.rearrange("p t (a b) -> p t a b", a=4), in0=Le4,
                                         in1=Gh[:].unsqueeze(3).to_broadcast([128, 32, 4, 8]), op=ALU.add))
            for i in range(32):
                V_(lambda e, i=i: e.max(out=m8[:, i, :], in_=Lm[:, i, :]))
            V_(lambda e: e.tensor_tensor(out=A0, in0=Lm, in1=m8[:, :, 0:1].to_broadcast([128, 32, 32]),
                                         op=ALU.is_equal))
            V_(lambda e: e.tensor_tensor(out=A1, in0=Lm, in1=m8[:, :, 1:2].to_broadcast([128, 32, 32]),
                                         op=ALU.is_equal))
            V_(lambda e: e.tensor_tensor(out=ed[:].unsqueeze(2), in0=m8[:, :, 1:2], in1=m8[:, :, 0:1], op=ALU.subtract))
            X('scalar', lambda e: e.activation(out=ed[:], in_=ed[:], func=AF.Exp), writes=[rtb])
            V_(lambda e: e.tensor_scalar(out=den[:], in0=ed[:], scalar1=1.0, scalar2=None, op0=ALU.add))
            V_(lambda e: e.reciprocal(out=den[:], in_=den[:]))
            V_(lambda e: e.tensor_tensor(out=W0[:], in0=pg[:], in1=den[:], op=ALU.mult), writes=[b_rt])
            V_(lambda e: e.tensor_tensor(out=W1[:], in0=W0[:], in1=ed[:], op=ALU.mult), writes=[b_rt])
            V_(lambda e: e.tensor_tensor(out=Ab, in0=A0, in1=A1, op=ALU.add))
            for hf in range(2):
                def pmm(e, hf=hf):
                    ins = None
                    for ii in range(16):
                        i = hf * 16 + ii
                        o_ = ps[6 + hf][:, ii * 32:(ii + 1) * 32]
                        ins = e.matmul(o_, lhsT=ltri[:], rhs=Ab[:, i, :], start=True, stop=(i == 0))
                        for j in range(i):
                            ins = e.matmul(o_, lhsT=ones_b[:], rhs=Ab[:, j, :], start=False, stop=(j == i - 1))
                    return ins
                X('tensor', pmm, reads=[rtb, cb, b_const], writes=[psb[6 + hf]])
                hs = slice(hf * 16, (hf + 1) * 16)
                pv3 = ps[6 + hf][:].rearrange("p (t e) -> p t e", e=32)
                V_(lambda e, hs=hs, pv3=pv3: e.tensor_scalar(out=okm[:, hs, :], in0=pv3, scalar1=float(CAP), scalar2=None,
                                                             op0=ALU.is_lt), reads=[psb[6 + hf]])
                V_(lambda e, hs=hs, pv3=pv3: e.tensor_tensor(out=posE[:, hs, :],
                                                             in0=ecap[:].unsqueeze(1).to_broadcast([128, 16, 32]),
                                                             in1=pv3, op=ALU.add), reads=[psb[6 + hf], cb])
            V_(lambda e: e.scalar_tensor_tensor(out=posE, in0=posE, scalar=-float(DUMP), in1=okm,
                                                op0=ALU.add, op1=ALU.mult))
            for sl_, A_, R_ in ((0, A0, R0), (1, A1, R1)):
                V_(lambda e, A_=A_: e.tensor_tensor(out=t32, in0=A_, in1=posE, op=ALU.mult))
                V_(lambda e: e.tensor_reduce(out=rf[:], in_=t32, axis=AX.X, op=ALU.add))
                V_(lambda e: e.tensor_scalar(out=rf[:], in0=rf[:], scalar1=float(DUMP), scalar2=None, op0=ALU.add))
                V_(lambda e, R_=R_: e.tensor_copy(out=R_[:], in_=rf[:]), writes=[b_rt])
            for i in range(32):
                bi = i % 3
                DMA('sync', f'hb{bi}', h2b[bi][:], h2d[i * 128:(i + 1) * 128, :], reads=[h2db[i]], writes=[h2bb[bi]])
                for sl_, R_ in ((0, R0), (1, R1)):
                    deps = _deps([h2bb[bi], b_rt], [])
                    tok = P.dma('gpsimd', f'sc{bi}{sl_}',
                                lambda e, R_=R_, i=i, bi=bi: e.indirect_dma_start(
                                    out=disp, out_offset=bass.IndirectOffsetOnAxis(ap=R_[:, i:i + 1], axis=0),
                                    in_=h2b[bi][:], in_offset=None), deps)
                    _upd(tok, [h2bb[bi], b_rt], [])
            if debug:
                X('vector', lambda e: e.tensor_copy(out=h2f[0][:, 0:32], in_=R0[:]), reads=[b_rt], writes=[h2fb[0]])
                X('vector', lambda e: e.tensor_copy(out=h2f[0][:, 32:64], in_=R1[:]), reads=[b_rt], writes=[h2fb[0]])
                X('vector', lambda e: e.tensor_copy(out=h2f[0][:, 64:96], in_=W0[:]), reads=[b_rt], writes=[h2fb[0]])
                X('vector', lambda e: e.tensor_copy(out=h2f[0][:, 96:128], in_=W1[:]), reads=[b_rt], writes=[h2fb[0]])
                DMA('sync', 'dbg', rtab, h2f[0][:, 0:128], reads=[h2fb[0]])

        if upto >= 'E':
            phase(phaseE)

        def phaseF(st):
            NR = 8
            ring = [sb(st, f"rg{i}", [128, 4096], BF16) for i in range(NR)]
            ringb = bufs(NR)
            xb = [sb(st, f"xb{i}", [128, NT, D], BF16) for i in range(2)]
            xbb = bufs(2)
            xbT = sb(st, "xbT", [128, 16, CAP], BF16)
            xbTb = Buf()
            hid = sb(st, "hid", [128, 8, CAP], BF16)
            hidb = bufs(8)
            sg = [sb(st, f"sg{i}", [128, CAP], F32) for i in range(2)]
            sgb = bufs(2)
            seq = []
            for e_ in range(NE):
                for j in range(4):
                    seq.append((e_, 'g', j))
                    seq.append((e_, 'u', j))
                for dt_ in range(4):
                    seq.append((e_, 'd', dt_))
            slot_of = {}
            nxt = [0]

            def issue_load():
                if nxt[0] >= len(seq):
                    return
                n = nxt[0]
                nxt[0] += 1
                e_, kind, j = seq[n]
                s_ = n % NR
                slot_of[(e_, kind, j)] = s_
                if kind == 'd':
                    src = wd[e_][:, j * 512:(j + 1) * 512].rearrange("(k p) c -> p k c", p=128)
                    dst = ring[s_][:].rearrange("p (k c) -> p k c", k=8)
                else:
                    wsrc = wg if kind == 'g' else wu
                    src = wsrc[e_][:, j * 256:(j + 1) * 256].rearrange("(k p) c -> p k c", p=128)
                    dst = ring[s_][:].rearrange("p (k c) -> p k c", k=16)
                DMA('gpsimd', f'r{s_}', dst, src, writes=[ringb[s_]])

            def load_x(e_):
                s_ = e_ % 2
                DMA('sync', f'x{s_}', xb[s_][:], disp[e_ * CAP:(e_ + 1) * CAP, :].rearrange("(n p) d -> p n d", p=128),
                    writes=[xbb[s_]])
            load_x(0)
            for _ in range(NR):
                issue_load()
            prr = [0]
            yrr = [0]
            for e_ in range(NE):
                xs_ = e_ % 2
                if e_ + 1 < NE:
                    load_x(e_ + 1)
                for k2 in range(8):
                    pb_ = 6 + (k2 % 2)
                    pv = ps[pb_][:].bitcast(BF16)[:, 0:2 * CAP].rearrange("p (a b) -> p a b", a=2)

                    def tr(e, k2=k2, pv=pv, xs_=xs_):
                        ins = None
                        for a in range(2):
                            kc = k2 * 2 + a
                            for n in range(NT):
                                ins = e.transpose(out=pv[:, a, n * 128:(n + 1) * 128],
                                                  in_=xb[xs_][:, n, kc * 128:(kc + 1) * 128], identity=ident_b[:])
                        return ins
                    X('tensor', tr, reads=[xbb[xs_], b_const], writes=[psb[pb_]])
                    X('vector', lambda e, k2=k2, pv=pv: e.tensor_copy(out=xbT[:, k2 * 2:(k2 + 1) * 2, :], in_=pv),
                      reads=[psb[pb_]], writes=[xbTb])
                for j in range(4):
                    sg_ = slot_of[(e_, 'g', j)]
                    su_ = slot_of[(e_, 'u', j)]
                    wgv = ring[sg_][:].rearrange("p (k c) -> p k c", k=16)
                    wuv = ring[su_][:].rearrange("p (k c) -> p k c", k=16)
                    for a in range(2):
                        fc = j * 2 + a
                        pg = (prr[0] % 2) * 2
                        prr[0] += 1
                        pu = pg + 1

                        def mm(e, a=a, wgv=wgv, wuv=wuv, pg=pg, pu=pu):
                            for kc in range(16):
                                e.matmul(ps[pg][:, 0:CAP], lhsT=wgv[:, kc, a * 128:(a + 1) * 128], rhs=xbT[:, kc, :],
                                         start=(kc == 0), stop=(kc == 15))
                            ins = None
                            for kc in range(16):
                                ins = e.matmul(ps[pu][:, 0:CAP], lhsT=wuv[:, kc, a * 128:(a + 1) * 128], rhs=xbT[:, kc, :],
                                               start=(kc == 0), stop=(kc == 15))
                            return ins
                        X('tensor', mm, reads=[xbTb, ringb[sg_], ringb[su_]], writes=[psb[pg], psb[pu]])
                        si = fc % 2
                        X('scalar', lambda e, si=si, pg=pg: e.activation(out=sg[si][:], in_=ps[pg][:, 0:CAP], func=AF.Silu),
                          reads=[psb[pg]], writes=[sgb[si]])
                        X('vector', lambda e, si=si, pu=pu, fc=fc: e.tensor_tensor(out=hid[:, fc, :], in0=sg[si][:],
                                                                                   in1=ps[pu][:, 0:CAP], op=ALU.mult),
                          reads=[sgb[si], psb[pu]], writes=[hidb[fc]])
                    issue_load()
                    issue_load()
                for dt_ in range(4):
                    sd_ = slot_of[(e_, 'd', dt_)]
                    wdv = ring[sd_][:].rearrange("p (k c) -> p k c", k=8)
                    for n in range(NT):
                        pb_ = 4 + (prr[0] % 2)
                        prr[0] += 1

                        def mm(e, n=n, wdv=wdv, pb_=pb_):
                            ins = None
                            for fc in range(8):
                                ins = e.matmul(ps[pb_][:], lhsT=hid[:, fc, n * 128:(n + 1) * 128], rhs=wdv[:, fc, :],
                                               start=(fc == 0), stop=(fc == 7))
                            return ins
                        X('tensor', mm, reads=hidb + [ringb[sd_]], writes=[psb[pb_]])
                        X('scalar' if (n % 2) else 'vector',
                          (lambda e, pb_=pb_, n=n, dt_=dt_: e.activation(out=yst[n][:, dt_ * 512:(dt_ + 1) * 512],
                                                                         in_=ps[pb_][:], func=AF.Copy)) if (n % 2) else
                          (lambda e, pb_=pb_, n=n, dt_=dt_: e.tensor_copy(out=yst[n][:, dt_ * 512:(dt_ + 1) * 512],
                                                                          in_=ps[pb_][:])),
                          reads=[psb[pb_]], writes=[ystb[n]])
                    issue_load()
                for n in range(NT):
                    r0_ = e_ * CAP + n * 128
                    DMA('sync', f'y{n}', ydisp[r0_:r0_ + 128, :], yst[n][:], reads=[ystb[n]])

        yst = None
        ystb = None

        def phaseF_wrap(st):
            nonlocal yst, ystb
            yst = [sb(st, f"yst{i}", [128, D], BF16) for i in range(NT)]
            ystb = bufs(NT)
            phaseF(st)

        if upto >= 'F':
            phase(phaseF_wrap)

        def phaseG(st):
            gf = sb(st, "gf", [128, D], F32)
            cb = Buf()
            y0 = [sb(st, f"y0{i}", [128, D], BF16) for i in range(3)]
            y1 = [sb(st, f"y1{i}", [128, D], BF16) for i in range(3)]
            x1t = [sb(st, f"x1t{i}", [128, D], F32) for i in range(3)]
            y0b, y1b, x1tb = bufs(3), bufs(3), bufs(3)
            acc2 = [sb(st, f"acc{i}", [128, D], F32) for i in range(2)]
            acc2b = bufs(2)
            ssg = sb(st, "ssg2", [128, 2], F32)
            ssgb = bufs(2)
            t0g = [sb(st, f"t0g{i}", [128, D], F32) for i in range(2)]
            t1g = [sb(st, f"t1g{i}", [128, D], F32) for i in range(2)]
            t0gb, t1gb = bufs(2), bufs(2)
            junk = sb(st, "junkG", [128, D], BF16)
            junkb = Buf()
            ss = sb(st, "ssG", [128, 1], F32)
            ssb = Buf()
            ob = [sb(st, f"ob{i}", [128, D], F32) for i in range(2)]
            obb = bufs(2)
            DMA('sync', 'c0', gf[:], gf_bc, writes=[cb])

            def load(i):
                s_ = i % 3
                for nm, yt, ytb, R_ in (('g0', y0, y0b, R0), ('g1', y1, y1b, R1)):
                    deps = _deps([b_rt], [ytb[s_]])
                    tok = P.dma('gpsimd', f'{nm}{s_}',
                                lambda e, yt=yt, R_=R_, i=i, s_=s_: e.indirect_dma_start(
                                    out=yt[s_][:], out_offset=None, in_=ydisp,
                                    in_offset=bass.IndirectOffsetOnAxis(ap=R_[:, i:i + 1], axis=0)), deps)
                    _upd(tok, [b_rt], [ytb[s_]])
                DMA('sync', f'x{s_}', x1t[s_][:], x1d[i * 128:(i + 1) * 128, :], writes=[x1tb[s_]])
            def Ga(i):
                s_ = i % 2
                l_ = i % 3
                X('scalar', lambda e, s_=s_, i=i, l_=l_: e.activation(out=t0g[s_][:], in_=y0[l_][:], func=AF.Copy,
                                                               scale=W0[:, i:i + 1]),
                  reads=[y0b[l_], b_rt], writes=[t0gb[s_]])
                X('scalar', lambda e, s_=s_, i=i, l_=l_: e.activation(out=t1g[s_][:], in_=y1[l_][:], func=AF.Copy,
                                                               scale=W1[:, i:i + 1]),
                  reads=[y1b[l_], b_rt], writes=[t1gb[s_]])
                X('vector', lambda e, s_=s_, l_=l_: e.tensor_tensor(out=acc2[s_][:], in0=t0g[s_][:], in1=x1t[l_][:], op=ALU.add),
                  reads=[t0gb[s_], x1tb[l_]], writes=[acc2b[s_]])
                X('vector', lambda e, s_=s_: e.tensor_tensor(out=acc2[s_][:], in0=acc2[s_][:], in1=t1g[s_][:], op=ALU.add),
                  reads=[t1gb[s_]], writes=[acc2b[s_]])

            def Gb(i):
                s_ = i % 2
                X('scalar', lambda e, s_=s_: e.activation(out=junk[:], in_=acc2[s_][:], func=AF.Square,
                                                          accum_out=ssg[:, s_:s_ + 1]),
                  reads=[acc2b[s_]], writes=[junkb, ssgb[s_]])
                rstd_from_ss(ssg[:, s_:s_ + 1], ssgb[s_], None)
                X('scalar', lambda e, s_=s_: e.activation(out=acc2[s_][:], in_=acc2[s_][:], func=AF.Copy,
                                                          scale=ssg[:, s_:s_ + 1]),
                  reads=[ssgb[s_]], writes=[acc2b[s_]])
                X('vector', lambda e, s_=s_: e.tensor_tensor(out=ob[s_][:], in0=acc2[s_][:], in1=gf[:], op=ALU.mult),
                  reads=[acc2b[s_], cb], writes=[obb[s_]])
                DMA('sync', f'o{s_}', out[i * 128:(i + 1) * 128, :], ob[s_][:], reads=[obb[s_]])


            load(0)
            load(1)
            Ga(0)
            for i in range(32):
                if i + 2 < 32:
                    load(i + 2)
                if i + 1 < 32:
                    Ga(i + 1)
                Gb(i)

        if upto >= 'G':
            phase(phaseG)
    return nc


def make_consts():
    c = {}
    c["c_ident"] = np.eye(128, dtype=np.float32)
    tp = np.arange(128)
    c["c_ltri"] = (tp[:, None] < tp[None, :]).astype(np.float32)
    c["c_ones"] = np.ones((128, 128), np.float32)
    half = 16
    inv = (500000.0 ** (-np.arange(half, dtype=np.float32) * 2.0 / 32)).astype(np.float32)
    ang = np.arange(S, dtype=np.float32)[None, :] * inv[:, None]
    c["c_cos"] = np.concatenate([np.cos(ang), np.cos(ang), np.ones((96, S))], 0).astype(np.float32)
    c["c_sin"] = np.concatenate([np.sin(ang), np.sin(ang), np.zeros((96, S))], 0).astype(np.float32)
    R = np.zeros((128, 128), np.float32)
    for j in range(16):
        R[j + 16, j] = -1.0
        R[j, j + 16] = 1.0
    c["c_rot"] = R
    n = np.arange(16)[None, :]
    blk = (np.arange(32) // 2)[:, None]
    mneg = np.where(n >= blk, -1e30, 0.0).astype(np.float32)
    valid = (n < blk).astype(np.float32)
    own = (n == blk).astype(np.float32)
    c["c_mneg"] = np.ascontiguousarray(np.broadcast_to(mneg.reshape(1, 512), (128, 512)))
    c["c_valid"] = np.ascontiguousarray(np.broadcast_to(valid.reshape(1, 512), (128, 512)))
    c["c_own"] = np.ascontiguousarray(np.broadcast_to(own.reshape(1, 512), (128, 512)))
    es = np.zeros((128, 16, 128), np.float32)
    for i in range(16):
        es[i, i, :] = 1.0
    c["c_esel"] = es.reshape(128, 2048)
    kl = np.arange(128)[:, None]
    ii = np.arange(512)[None, :]
    ca = np.ones((128, 4, 512), np.float32)
    ca[:, 0, :] = np.where(ii < 256, (kl <= ii), 1.0)
    ca[:, 1, :] = np.where(ii < 256, (128 + kl <= ii), 1.0)
    ca[:, 2, :] = np.where(ii >= 256, (kl <= ii - 256), 1.0)
    ca[:, 3, :] = np.where(ii >= 256, (128 + kl <= ii - 256), 1.0)
    c["c_caus"] = ca.reshape(128, 2048)
    c["c_tril"] = (tp[None, :] <= tp[:, None]).astype(np.float32)
    c["c_ecap"] = np.ascontiguousarray(np.broadcast_to((np.arange(32, dtype=np.float32) * CAP)[None, :], (128, 32)))
    return c


def make_shared(inp):
    f = lambda a: np.ascontiguousarray(np.asarray(a, dtype=np.float32))
    bc = lambda v, n: np.ascontiguousarray(np.broadcast_to(f(v).reshape(1, n), (128, n)))
    sh = {}
    sh["w_in"] = f(inp["w_in"][0])
    sh["sgu_w"] = f(inp["sgu_w"][0])
    sh["w_pa"] = f(inp["w_proj_attn"][0])
    sh["w_pb"] = f(inp["w_proj_sgu"][0])
    sh["w_out"] = f(inp["w_out"][0])
    sh["w_r"] = np.ascontiguousarray(np.concatenate([f(inp["w_router_group"][0]), f(inp["w_router_expert"][0])], axis=1))
    sh["wg"] = f(inp["w_exp_gate"][0])
    sh["wu"] = f(inp["w_exp_up"][0])
    sh["wd"] = f(inp["w_exp_down"][0])
    sh["gmix"] = np.ascontiguousarray(f(inp["norm_mix_g"][0]).reshape(16, 128).T)
    sh["ln_g_bc"] = bc(inp["sgu_ln_g"][0], 1024)
    sh["ln_b_bc"] = bc(inp["sgu_ln_b"][0], 1024)
    sh["sgub_bc"] = bc(f(inp["sgu_b"][0]).reshape(-1), 1024)
    sh["g2_bc"] = bc(inp["norm_ffn_g"][0], D)
    sh["gf_bc"] = bc(inp["norm_final_g"], D)
    sh["rb_bc"] = bc(np.concatenate([f(inp["b_router_group"][0]), f(inp["b_router_expert"][0])]), 36)
    sh.update(make_consts())
    return sh


_NC_CACHE = {}


def kernel(**inputs):
    xfull = np.asarray(inputs["x"], dtype=np.float32)
    B = xfull.shape[0]
    sh = make_shared(inputs)
    if "nc" not in _NC_CACHE:
        _NC_CACHE["nc"] = build()
    nc = _NC_CACHE["nc"]
    in_maps = []
    for c in range(B):
        m = dict(sh)
        m["x"] = np.ascontiguousarray(xfull[c])
        in_maps.append(m)
    res = run_bass_kernel_spmd(nc, in_maps, core_ids=list(range(B)))
    return np.stack([np.asarray(r["out"]) for r in res.results], axis=0).astype(np.float32)
```
